# Optimizing a Trainium2 kernel written in Bass

```python
import math
import jax, jax.numpy as jnp
from jax import lax
import numpy as np

D_MODEL = 1024
BATCH = 8
SEQ = 2048
DEPTH = 2

N_A = DEPTH // 2
N_B = DEPTH - N_A
N_DENSE = (DEPTH + 1) // 2
N_MOE = DEPTH // 2

CONV_WIDTH = 3
HEAD_DIM = 64
N_HEADS = D_MODEL // (2 * HEAD_DIM)
V_DIM = 2 * HEAD_DIM
D_FF = 256 * ((8 * D_MODEL // 3 + 255) // 256)
N_EXPERTS = 8
TOP_K = 2
D_FF_EXPERT = 7 * D_MODEL // 2
Q_BLOCK = 128
EPS = 1e-6

kernel_name = "yoco_shortconv_diffattn_moe"


def rmsnorm(x, g):
    xf = x.astype(jnp.float32)
    y = xf * lax.rsqrt(jnp.mean(xf * xf, axis=-1, keepdims=True) + EPS)
    return (y * g.astype(jnp.float32)).astype(x.dtype)


def lambda_init(layer_idx_1based):
    return 0.8 - 0.6 * math.exp(-0.3 * (layer_idx_1based - 1))


def swiglu(h, w_gu, w_down):
    gu = h @ w_gu
    g, u = jnp.split(gu, 2, axis=-1)
    return (jax.nn.silu(g) * u) @ w_down


def short_conv_mixer(h, w_in, conv_w, w_out):
    S = h.shape[1]
    bcv = h @ w_in
    b, c, v = jnp.split(bcv, 3, axis=-1)
    u = c * v
    up = jnp.pad(u, ((0, 0), (CONV_WIDTH - 1, 0), (0, 0)))
    z = up[:, 0:S] * conv_w[0]
    for j in range(1, CONV_WIDTH):
        z = z + up[:, j:j + S] * conv_w[j]
    return (b * z) @ w_out


def shared_kv(stream, ln_kv, w_kv, k_norm):
    Bsz, S, _ = stream.shape
    h = rmsnorm(stream, ln_kv)
    kv = h @ w_kv
    k = kv[..., :2 * N_HEADS * HEAD_DIM].reshape(Bsz, S, N_HEADS, 2, HEAD_DIM)
    k = rmsnorm(k, k_norm)
    v = kv[..., 2 * N_HEADS * HEAD_DIM:].reshape(Bsz, S, N_HEADS, V_DIM)
    return k, v


def diff_attention(h, k, v, w_q, q_norm, lam_params, sub_norm, w_o, lam_init):
    Bsz, S, _ = h.shape
    scale = HEAD_DIM ** -0.5
    q = (h @ w_q).reshape(Bsz, S, N_HEADS, 2, HEAD_DIM)
    q = rmsnorm(q, q_norm) * scale
    lp = lam_params.astype(jnp.float32)
    lam = (jnp.exp(jnp.sum(lp[0] * lp[1])) - jnp.exp(jnp.sum(lp[2] * lp[3])) + lam_init)
    n_blk = S // Q_BLOCK
    qb = q.reshape(Bsz, n_blk, Q_BLOCK, N_HEADS, 2, HEAD_DIM).transpose(1, 0, 2, 3, 4, 5)
    kpos = jnp.arange(S)

    def block(args):
        qi, bi = args
        s = jnp.einsum('bqhcd,bkhcd->bhcqk', qi, k).astype(jnp.float32)
        qpos = bi * Q_BLOCK + jnp.arange(Q_BLOCK)
        mask = kpos[None, :] <= qpos[:, None]
        s = jnp.where(mask, s, -jnp.inf)
        p = jax.nn.softmax(s, axis=-1)
        a = p[:, :, 0] - lam * p[:, :, 1]
        return jnp.einsum('bhqk,bkhe->bqhe', a.astype(v.dtype), v)

    o = lax.map(block, (qb, jnp.arange(n_blk)))
    o = o.transpose(1, 0, 2, 3, 4).reshape(Bsz, S, N_HEADS, V_DIM)
    o = rmsnorm(o, sub_norm) * (1.0 - lam_init)
    return o.reshape(Bsz, S, N_HEADS * V_DIM) @ w_o


def moe_swiglu(h, w_router, w_gu_e, w_down_e):
    logits = (h @ w_router).astype(jnp.float32)
    top_v, top_i = lax.top_k(logits, TOP_K)
    gates = jax.nn.softmax(top_v, axis=-1)
    combine = jnp.sum(jax.nn.one_hot(top_i, N_EXPERTS, dtype=jnp.float32) * gates[..., None], axis=-2)
    combine = combine.astype(h.dtype)
    out = jnp.zeros_like(h)
    for e in range(N_EXPERTS):
        out = out + combine[..., e:e + 1] * swiglu(h, w_gu_e[e], w_down_e[e])
    return out


def setup_inputs(seed: int = 0) -> dict:
    key = jax.random.key(seed)
    ks = jax.random.split(key, 24)
    D = D_MODEL
    f32 = jnp.float32

    def nrm(k, shape, scale):
        return jax.random.normal(k, shape, f32) * scale

    def gain(k, shape):
        return 1.0 + 0.02 * jax.random.normal(k, shape, f32)

    kv_width = 2 * N_HEADS * HEAD_DIM + N_HEADS * V_DIM
    return {
        "x": jax.random.normal(ks[0], (BATCH, SEQ, D), f32),
        "ln_mix": gain(ks[1], (DEPTH, D)),
        "ln_ffn": gain(ks[2], (DEPTH, D)),
        "conv_w_in": nrm(ks[3], (N_A, D, 3 * D), D ** -0.5),
        "conv_w": nrm(ks[4], (N_A, CONV_WIDTH, D), CONV_WIDTH ** -0.5),
        "conv_w_out": nrm(ks[5], (N_A, D, D), D ** -0.5),
        "ln_kv": gain(ks[6], (D,)),
        "w_kv": nrm(ks[7], (D, kv_width), D ** -0.5),
        "k_norm": gain(ks[8], (HEAD_DIM,)),
        "attn_w_q": nrm(ks[9], (N_B, D, 2 * N_HEADS * HEAD_DIM), D ** -0.5),
        "q_norm": gain(ks[10], (N_B, HEAD_DIM)),
        "lam_params": nrm(ks[11], (N_B, 4, HEAD_DIM), 0.1),
        "sub_norm": gain(ks[12], (N_B, V_DIM)),
        "attn_w_o": nrm(ks[13], (N_B, N_HEADS * V_DIM, D), (N_HEADS * V_DIM) ** -0.5),
        "ffn_w_gu": nrm(ks[14], (N_DENSE, D, 2 * D_FF), D ** -0.5),
        "ffn_w_down": nrm(ks[15], (N_DENSE, D_FF, D), D_FF ** -0.5),
        "router_w": nrm(ks[16], (N_MOE, D, N_EXPERTS), D ** -0.5),
        "moe_w_gu": nrm(ks[17], (N_MOE, N_EXPERTS, D, 2 * D_FF_EXPERT), D ** -0.5),
        "moe_w_down": nrm(ks[18], (N_MOE, N_EXPERTS, D_FF_EXPERT, D), D_FF_EXPERT ** -0.5),
    }


def reference(x, ln_mix, ln_ffn, conv_w_in, conv_w, conv_w_out, ln_kv, w_kv, k_norm,
              attn_w_q, q_norm, lam_params, sub_norm, attn_w_o, ffn_w_gu, ffn_w_down,
              router_w, moe_w_gu, moe_w_down):
    k_sh = None
    v_sh = None
    for i in range(DEPTH):
        h = rmsnorm(x, ln_mix[i])
        if i < N_A:
            x = x + short_conv_mixer(h, conv_w_in[i], conv_w[i], conv_w_out[i])
        else:
            j = i - N_A
            x = x + diff_attention(h, k_sh, v_sh, attn_w_q[j], q_norm[j], lam_params[j],
                                   sub_norm[j], attn_w_o[j], lambda_init(i + 1))
        h = rmsnorm(x, ln_ffn[i])
        if i % 2 == 0:
            x = x + swiglu(h, ffn_w_gu[i // 2], ffn_w_down[i // 2])
        else:
            x = x + moe_swiglu(h, router_w[i // 2], moe_w_gu[i // 2], moe_w_down[i // 2])
        if i == N_A - 1:
            k_sh, v_sh = shared_kv(x, ln_kv, w_kv, k_norm)
    return x
```

```python
import math
import re
import numpy as np
import concourse.bass as bass
import concourse.mybir as mybir
from concourse.bass_utils import run_bass_kernel_spmd

F32 = mybir.dt.float32
BF16 = mybir.dt.bfloat16
AF = mybir.ActivationFunctionType
ALU = mybir.AluOpType
AX = mybir.AxisListType

D = 1024
S = 2048
KT = 8
NTC = 4
NT = 16
DFF = 2816
NFF = 22
NE = 8
DFE = 3584
NFE = 28
EPS = 1e-6
LAM_INIT = 0.8 - 0.6 * math.exp(-0.3 * 1.0)
NS = 4
SLOT = 4096

PC_LNMIX0, PC_LNFFN0, PC_LNKV, PC_LNMIX1, PC_LNFFN1 = 0, 8, 16, 24, 32
PC_CW = 40
PC_GK, PC_GQ, PC_GS = 64, 65, 66
PC_IOTA = 67
NPC = 83
NTILE = 15
NSLOT = NTILE * 512


class Buf:
    __slots__ = ("name", "lw", "rd")

    def __init__(self, name):
        self.name = name
        self.lw = None
        self.rd = {}


class Prog:
    def __init__(self, nc):
        self.nc = nc
        self.ops = []
        self.plan = False
        self.E = {"pe": nc.tensor, "act": nc.scalar, "dve": nc.vector,
                  "pool": nc.gpsimd, "sp": nc.sync}

    def op(self, eng, fn, r=(), w=(), dma=False):
        if self.plan:
            return
        self.ops.append([eng, fn, tuple(r), tuple(w), dma, None, False, 0])

    def matmul(self, out, lhsT, rhs, start, stop, r, w):
        nc = self.nc
        self.op("pe", lambda: nc.tensor.matmul(out, lhsT, rhs, start=start, stop=stop), r, w)

    def transpose(self, out, in_, ident, r, w):
        nc = self.nc
        self.op("pe", lambda: nc.tensor.transpose(out, in_, ident), r, w)

    def act(self, out, in_, func, r, w, bias=None, scale=None):
        nc = self.nc
        kw = {}
        if bias is not None:
            kw["bias"] = bias
        if scale is not None:
            kw["scale"] = scale
        self.op("act", lambda: nc.scalar.activation(out=out, in_=in_, func=func, **kw), r, w)

    def copy(self, eng, out, in_, r, w):
        nc = self.nc
        if eng == "act":
            self.op("act", lambda: nc.scalar.copy(out=out, in_=in_), r, w)
        else:
            e = self.E[eng]
            self.op(eng, lambda: e.tensor_copy(out=out, in_=in_), r, w)

    def tt(self, eng, out, in0, in1, op, r, w):
        e = self.E[eng]
        self.op(eng, lambda: e.tensor_tensor(out=out, in0=in0, in1=in1, op=op), r, w)

    def ts(self, eng, out, in0, s1, s2, op0, op1, r, w):
        e = self.E[eng]
        if s2 is None:
            self.op(eng, lambda: e.tensor_scalar(out=out, in0=in0, scalar1=s1, scalar2=None, op0=op0), r, w)
        else:
            self.op(eng, lambda: e.tensor_scalar(out=out, in0=in0, scalar1=s1, scalar2=s2, op0=op0, op1=op1), r, w)

    def stt(self, eng, out, in0, scalar, in1, op0, op1, r, w):
        e = self.E[eng]
        self.op(eng, lambda: e.scalar_tensor_tensor(out=out, in0=in0, scalar=scalar, in1=in1, op0=op0, op1=op1), r, w)

    def recip(self, out, in_, r, w):
        nc = self.nc
        self.op("dve", lambda: nc.vector.reciprocal(out=out, in_=in_), r, w)

    def reduce(self, out, in_, op, r, w):
        nc = self.nc
        self.op("dve", lambda: nc.vector.tensor_reduce(out=out, in_=in_, axis=AX.X, op=op), r, w)

    def memset(self, eng, ap, val, w):
        e = self.E[eng]
        self.op(eng, lambda: e.memset(ap, val), (), w)

    def dma(self, q, out, in_, r, w):
        e = self.E[q]
        self.op(q, lambda: e.dma_start(out=out, in_=in_), r, w, dma=True)

    @staticmethod
    def _ckey(o):
        if o[4]:
            b = o[3][0] if o[3] else o[2][0]
            return ("dma", o[0], b.name)
        return o[0]

    def finalize(self, sem_alloc):
        ops = self.ops
        ck = self._ckey
        for i, o in enumerate(ops):
            deps = {}

            def add(j):
                k = ck(ops[j])
                if deps.get(k, -1) < j:
                    deps[k] = j
            for b in o[2]:
                if b.lw is not None:
                    add(b.lw)
            for b in o[3]:
                if b.lw is not None:
                    add(b.lw)
                for j in b.rd.values():
                    add(j)
            if o[0] == "pe" and not o[4]:
                deps.pop("pe", None)
            o[5] = list(deps.values())
            for j in o[5]:
                ops[j][6] = True
            k = ck(o)
            for b in o[2]:
                b.rd[k] = i
            for b in o[3]:
                b.lw = i
                b.rd = {}
        sems = {}
        cnt = {}
        waited = {}
        for o in ops:
            eng = o[0]
            e = self.E[eng]
            need = {}
            for j in o[5]:
                d = ops[j]
                k = ck(d)
                if need.get(k, 0) < d[7]:
                    need[k] = d[7]
            wd = waited.setdefault(eng, {})
            for k, v in need.items():
                if wd.get(k, 0) >= v:
                    continue
                e.wait_ge(sems[k], v)
                wd[k] = v
            ins = o[1]()
            k = ck(o)
            if o[4]:
                if k not in sems:
                    sems[k] = sem_alloc("s_" + "_".join(k))
                    cnt[k] = 0
                cnt[k] += 16
                ins.then_inc(sems[k], 16)
                o[7] = cnt[k]
            elif o[6]:
                if k not in sems:
                    sems[k] = sem_alloc("s_" + k)
                    cnt[k] = 0
                cnt[k] += 1
                ins.then_inc(sems[k], 1)
                o[7] = cnt[k]
        for k, s in sems.items():
            if isinstance(k, tuple):
                self.nc.sync.wait_ge(s, cnt[k])
        return len(ops)


class WStream:
    def __init__(self, P, view, ring_bufs, blocks=None, first_extra=None):
        self.P = P
        self.view = view
        self.bufs = ring_bufs
        self.first_extra = dict(first_extra or {})
        self.ns = NS
        self.base = 0
        self.plan = blocks is None
        self.blocks = [] if blocks is None else blocks
        self.next_get = 0
        self.next_issue = 0
        self.n_released = 0
        self.unheld = False

    def _issue(self):
        i = self.next_issue
        s = (i - self.base) % self.ns
        blk = self.blocks[i]
        if isinstance(blk, dict):
            if blk.get("pre") is not None:
                fn, reads = blk["pre"]
                self.P.op("pool", fn, reads, ())
            parts = blk["parts"]
        else:
            parts = blk
        for part in parts:
            off, n, split, src = part[:4]
            prefn = part[4] if len(part) > 4 else None
            dst = self.view(s, off, off + n).rearrange("p (a b) -> p a b", a=split)
            wbufs = (self.bufs[s],) + tuple(self.first_extra.pop(s, ()))
            if prefn is None:
                self.P.dma("pool", dst, src, (), wbufs)
            else:
                self.P.op("pool", (lambda prefn=prefn, dst=dst, src=src: self._dyn_dma(prefn, dst, src)),
                          (), wbufs, dma=True)
        self.next_issue += 1

    def _dyn_dma(self, prefn, dst, srcfn):
        g = self.P.nc.gpsimd
        rt = prefn()
        ins = g.dma_start(out=dst, in_=srcfn())
        m = re.search(r"R\[(Pool_tmp_(\d+))\]", str(ins.ins))
        if m:
            RH = type(rt)
            n = int(m.group(2))
            names = [m.group(1)] + [f"Pool_{rt.name}_snap_{n - k}" for k in range(1, 5)]
            for nm in names:
                try:
                    g.free_register(RH(nm, rt.engine))
                except ValueError:
                    pass
        return ins

    def _fill(self):
        while self.next_issue < len(self.blocks) and self.next_issue - self.n_released < self.ns:
            blk = self.blocks[self.next_issue]
            if isinstance(blk, dict) and blk.get("hold") and not self.unheld:
                return
            self._issue()

    def start(self):
        if self.plan:
            return
        self._fill()

    def go(self):
        if self.plan:
            self.go_at = self.next_get
            return
        assert self.next_issue == self.n_released == self.next_get, "ring must be drained when go() is called"
        self.unheld = True
        self.base = self.next_issue
        self.ns = len(self.bufs)
        self._fill()

    def get(self, parts):
        i = self.next_get
        self.next_get += 1
        if self.plan:
            self.blocks.append(parts)
            return 0, self.bufs[0]
        sl = (i - self.base) % self.ns
        return sl, self.bufs[sl]

    def release(self):
        if self.plan:
            return
        self.n_released += 1
        self._fill()


def build(stage=99):
    nc = bass.Bass("TRN2", target_bir_lowering=False)
    P = Prog(nc)

    def din(name, shape):
        return nc.dram_tensor(name, shape, F32, kind="ExternalInput").ap()

    x_d = din("x", [S, D])
    params_d = din("params", [128, NPC])
    lam_d = din("lam", [1, 256])
    win_d = din("conv_w_in", [D, 3 * D])
    wout_d = din("conv_w_out", [D, D])
    wkv_d = din("w_kv", [D, 2 * D])
    wq_d = din("attn_w_q", [D, D])
    wo_d = din("attn_w_o", [D, D])
    wgu_d = din("ffn_w_gu", [D, 2 * DFF])
    wdn_d = din("ffn_w_down", [DFF, D])
    rw_d = din("router_w", [D, NE])
    if stage >= 5:
        mgu_t = nc.dram_tensor("moe_w_gu", [NE, D, 2 * DFE], F32, kind="ExternalInput")
        mdn_t = nc.dram_tensor("moe_w_down", [NE, DFE, D], F32, kind="ExternalInput")
    else:
        mgu_t = nc.dram_tensor("moe_w_gu", [NE, 1, 1], F32, kind="ExternalInput")
        mdn_t = nc.dram_tensor("moe_w_down", [NE, 1, 1], F32, kind="ExternalInput")
    mgu_d = mgu_t.ap()
    mdn_d = mdn_t.ap()
    tri_d = din("tri", [128, 128])
    HG = nc.dram_tensor("hg_scratch", [NSLOT, D], BF16, kind="Internal").ap()
    YD = nc.dram_tensor("y_scratch", [NSLOT, D], F32, kind="Internal").ap()
    ident_d = din("ident", [128, 128])
    masks_d = din("masks", [128, 4 * 512])
    out_d = nc.dram_tensor("out", [S, D], F32, kind="ExternalOutput").ap()

    xT = nc.alloc_sbuf_tensor("xT", [128, KT, S], F32)
    hT = nc.alloc_sbuf_tensor("hT", [128, KT, S], BF16)
    AB = nc.alloc_sbuf_tensor("actbuf", [128, KT, S], BF16)
    ring = nc.alloc_sbuf_tensor("wring", [128, NS, SLOT], BF16)
    SCR = nc.alloc_sbuf_tensor("scr", [128, 10240], F32)
    prm = nc.alloc_sbuf_tensor("prm", [128, NPC], F32)
    ident = nc.alloc_sbuf_tensor("ident_sb", [128, 128], F32)
    masks = nc.alloc_sbuf_tensor("masks_sb", [128, 4, 512], BF16)
    ones_f = nc.alloc_sbuf_tensor("ones_f", [8, 128], F32)
    tri_bf = nc.alloc_sbuf_tensor("tri_bf", [128, 128], BF16)
    ident_bf = nc.alloc_sbuf_tensor("ident_bf", [128, 128], BF16)
    rw = nc.alloc_sbuf_tensor("rw_sb", [128, KT, NE], F32)
    ones_bf = nc.alloc_sbuf_tensor("ones_bf", [128, 128], BF16)
    blk_bf = nc.alloc_sbuf_tensor("blk_bf", [128, 128], BF16)
    small = nc.alloc_sbuf_tensor("small", [128, 64], F32)

    xB = [[Buf(f"x{k}_{t}") for t in range(NTC)] for k in range(KT)]
    hB = [[Buf(f"h{k}_{t}") for t in range(NTC)] for k in range(KT)]
    aB = [[Buf(f"a{k}_{t}") for t in range(NTC)] for k in range(KT)]
    wB = [Buf(f"w{s}") for s in range(NS + 3)]
    ABf = AB[:].rearrange("p k t -> p (k t)")

    def RW(sid, lo, hi):
        if sid < NS:
            return ring[:, sid, lo:hi]
        o = SLOT * (sid - NS + 1)
        return ABf[:, o + lo:o + hi]
    sB = [Buf(f"scr{i}") for i in range(40)]
    cB = Buf("consts")
    hgB = [Buf(f"hg_dram{i}") for i in range(16)]
    ydB = Buf("y_dram")
    smB = Buf("small")
    psB = [Buf(f"ps{i}") for i in range(8)]
    ps = [nc.alloc_psum_tensor(f"ps{i}", [128, 512], F32) for i in range(8)]

    def scr(kb_off, kb, dtype=F32):
        lo = int(kb_off * 256)
        hi = int((kb_off + kb) * 256)
        ap = SCR[:, lo:hi]
        if dtype == BF16:
            ap = ap.bitcast(BF16)
        b0 = int(math.floor(kb_off))
        b1 = int(math.ceil(kb_off + kb))
        return ap, sB[b0:b1]

    def tcs(t):
        return slice(t * 512, (t + 1) * 512)

    def pcol(c, n=1):
        return prm[:, c:c + n]

    class Rot:
        def __init__(self, idx):
            self.idx = idx
            self.i = 0

        def next(self):
            b = self.idx[self.i % len(self.idx)]
            self.i += 1
            return ps[b], psB[b]

    regs = {}
    for p_ in range(2):
        regs[p_] = (nc.gpsimd.alloc_register(f"re{p_}"), nc.gpsimd.alloc_register(f"rgu{p_}"),
                    nc.gpsimd.alloc_register(f"rdn{p_}"))
    tmpr = [nc.gpsimd.alloc_register(f"rtmp{i}") for i in range(4)]

    def body(P, W):
        P.dma("pool", prm[:], params_d[:], (), (cB,))
        P.dma("pool", ident[:], ident_d[:], (), (cB,))
        P.dma("pool", masks[:].rearrange("p a b -> p (a b)"), masks_d[:], (), (cB,))
        P.dma("pool", tri_bf[:], tri_d[:], (), (cB,))
        P.dma("pool", ident_bf[:], ident_d[:], (), (cB,))
        P.dma("pool", rw[:], rw_d.rearrange("(kt p) e -> p kt e", p=128), (), (cB,))
        W.start()
        P.memset("dve", ones_bf[:], 1.0, (cB,))
        P.memset("dve", blk_bf[:], 0.0, (cB,))
        P.memset("dve", blk_bf[0:64, 0:64], 1.0, (cB,))
        P.memset("dve", blk_bf[64:128, 64:128], 1.0, (cB,))
        P.memset("dve", ones_f[:], 1.0, (cB,))

        rot = Rot([0, 1, 2, 3])
        for g in range(NTC):
            stg, stgB = scr(16 * (g % 2), 16)
            stg3 = stg.rearrange("p (t d) -> p t d", t=4)
            P.dma("sp", stg3, x_d[g * 512:(g + 1) * 512, :].rearrange("(t p) d -> p t d", p=128), (), stgB)
            for kt in range(KT):
                pt, ptB = rot.next()
                for t in range(4):
                    P.transpose(pt[:, t * 128:(t + 1) * 128], stg3[:, t, kt * 128:(kt + 1) * 128], ident[:],
                                list(stgB) + [cB], (ptB,))
                P.copy("act" if kt % 2 else "dve", xT[:, kt, tcs(g)], pt[:], (ptB,), (xB[kt][g],))

        if stage >= 5:
            zsrc = ABf[:, 12288:16384].rearrange("p (s f) -> p s f", s=4)
            zB = [aB[k][t] for k in (6, 7) for t in range(NTC)]
            P.memset("dve", ABf[:, 12288:16384], 0.0, zB)
            for t in range(NTILE):
                P.dma("sp", HG[t * 512:(t + 1) * 512, :].rearrange("(s p) d -> p s d", p=128), zsrc, zB, (hgB[t % 16],))

        def rmsnorm(gcol, post_rs=None, dst=None, post_tc=None):
            banks = [4, 5, 6, 7]
            for tc in range(NTC):
                sq, sqB = scr(8 * (tc % 2), 8, BF16)
                sq3 = sq.rearrange("p (k t) -> p k t", k=KT)
                P.act(sq3, xT[:, :, tcs(tc)], AF.Square, [xB[k][tc] for k in range(KT)], sqB)
                for kt in range(KT):
                    P.matmul(ps[banks[tc]][:], ones_bf[:], sq3[:, kt, :], kt == 0, kt == KT - 1,
                             list(sqB) + [cB], (psB[banks[tc]],))
            for tc in range(NTC):
                sd, sdB = scr(16 + 4 * (tc % 2), 2)
                rs, rsB = scr(18 + 4 * (tc % 2), 2)
                P.act(sd, ps[banks[tc]][:], AF.Ln, (psB[banks[tc]], smB), sdB, bias=small[:, 0:1], scale=1.0 / D)
                P.act(rs, sd, AF.Exp, sdB, rsB, scale=-0.5)
                if post_rs is not None:
                    post_rs(tc, rs, rsB)
                for kt in range(KT):
                    if dst is None:
                        o_ap, o_b = hT[:, kt, tcs(tc)], (hB[kt][tc],)
                    else:
                        o_ap, o_b = dst(kt, tc)
                    if gcol is None:
                        P.tt("dve", o_ap, xT[:, kt, tcs(tc)], rs, ALU.mult,
                             [xB[kt][tc]] + list(rsB), o_b)
                    else:
                        P.stt("dve", o_ap, xT[:, kt, tcs(tc)], pcol(gcol + kt), rs,
                              ALU.mult, ALU.mult, [xB[kt][tc], cB] + list(rsB), o_b)
                if post_tc is not None:
                    post_tc(tc)

        P.memset("dve", small[:, 0:1], EPS, (smB,))

        def add_residual(m, tc, pt, ptB):
            P.tt("dve", xT[:, m, tcs(tc)], xT[:, m, tcs(tc)], pt[:], ALU.add, (xB[m][tc], ptB), (xB[m][tc],))

        def linear_fm(inp, inB, nk, wslot, woff, wcols, wbuf, m_list, rot, epilogue):
            for m in m_list:
                for tc in range(NTC):
                    pt, ptB = rot.next()
                    for k in range(nk):
                        c0 = woff + k * wcols + m * 128
                        P.matmul(pt[:], RW(wslot, c0, c0 + 128), inp[:, k, tcs(tc)], k == 0, k == nk - 1,
                                 (wbuf, inB[k][tc]), (ptB,))
                    epilogue(m, tc, pt, ptB)

        def wcolblock(src2d, c0, ncols):
            return [(0, KT * ncols, KT, src2d[:, c0:c0 + ncols].rearrange("(kt p) f -> p kt f", p=128))]

        def wrowblock(src2d, r0, nch):
            return [(0, nch * D, nch, src2d[r0:r0 + nch * 128, :].rearrange("(c p) f -> p c f", p=128))]

        if stage >= 2:
            rmsnorm(PC_LNMIX0)
            rot = Rot([0, 1, 2, 3, 4, 5])
            for G in range(2):
                sb_, bb_ = W.get(wcolblock(win_d, G * 512, 512))
                sc_, bc_ = W.get(wcolblock(win_d, D + G * 512, 512))
                sv_, bv_ = W.get(wcolblock(win_d, 2 * D + G * 512, 512))
                for jj in range(4):
                    j = 4 * G + jj
                    ub, ubB = scr(8 * (j % 2), 8)
                    bbuf, bbB = scr(16 + 8 * (j % 2), 8)
                    for tc in range(NTC):
                        pc, pcB = rot.next()
                        pv, pvB = rot.next()
                        pb, pbB = rot.next()
                        for (pt, ptB, slot, wb) in ((pc, pcB, sc_, bc_), (pv, pvB, sv_, bv_), (pb, pbB, sb_, bb_)):
                            for kt in range(KT):
                                c0 = kt * 512 + jj * 128
                                P.matmul(pt[:], RW(slot, c0, c0 + 128), hT[:, kt, tcs(tc)], kt == 0, kt == KT - 1,
                                         (wb, hB[kt][tc]), (ptB,))
                        csb, csbB = scr(32 + 2 * (tc % 2), 2)
                        z, zB = scr(36 + 2 * (tc % 2), 2)
                        P.copy("act", csb, pc[:], (pcB,), csbB)
                        P.tt("dve", ub[:, tcs(tc)], csb, pv[:], ALU.mult, list(csbB) + [pvB], ubB)
                        P.copy("act", bbuf[:, tcs(tc)], pb[:], (pbB,), bbB)
                        lo = tc * 512
                        P.ts("dve", z, ub[:, lo:lo + 512], pcol(PC_CW + 16 + j), None, ALU.mult, None,
                             list(ubB) + [cB], zB)
                        for sh, tap in ((1, 1), (2, 0)):
                            a = sh if tc == 0 else 0
                            P.stt("dve", z[:, a:512], ub[:, lo + a - sh:lo + 512 - sh], pcol(PC_CW + 8 * tap + j),
                                  z[:, a:512], ALU.mult, ALU.add, list(ubB) + list(zB) + [cB], zB)
                        P.tt("dve", AB[:, j, tcs(tc)], bbuf[:, tcs(tc)], z, ALU.mult, list(bbB) + list(zB), (aB[j][tc],))
                W.release(); W.release(); W.release()
            rot = Rot([0, 1, 2, 3, 4, 5, 6, 7])
            for half in range(2):
                so_, bo_ = W.get(wcolblock(wout_d, half * 512, 512))
                linear_fm(AB, aB, KT, so_, 0, 512, bo_, range(4), rot,
                          lambda m, tc, pt, ptB, half=half: add_residual(4 * half + m, tc, pt, ptB))
                W.release()

        def ffn(gu_d, dn_d, nchunks, dff, cT=None):
            rot_gu = Rot([0, 1, 2, 3])
            rot_dn = Rot([4, 5, 6, 7])
            ngrp = (nchunks + 3) // 4
            for gi in range(ngrp):
                c0 = gi * 4
                nch = min(4, nchunks - c0)
                sg_, bg_ = W.get(wcolblock(gu_d, c0 * 128, nch * 128))
                su_, bu_ = W.get(wcolblock(gu_d, dff + c0 * 128, nch * 128))
                sd_, bd_ = W.get(wrowblock(dn_d, c0 * 128, nch))
                base = 4 * (gi % 2)
                for jj in range(nch):
                    a = base + jj
                    for tc in range(NTC):
                        pg, pgB = rot_gu.next()
                        pu, puB = rot_gu.next()
                        for (pt, ptB, slot, wb) in ((pg, pgB, sg_, bg_), (pu, puB, su_, bu_)):
                            for kt in range(KT):
                                cc = kt * nch * 128 + jj * 128
                                P.matmul(pt[:], RW(slot, cc, cc + 128), hT[:, kt, tcs(tc)], kt == 0, kt == KT - 1,
                                         (wb, hB[kt][tc]), (ptB,))
                        sl, slB = scr(32 + 2 * (tc % 2), 2)
                        P.act(sl, pg[:], AF.Silu, (pgB,), slB)
                        if cT is not None:
                            P.tt("dve", sl, sl, cT[0][:, tcs(tc)], ALU.mult, list(slB) + list(cT[1]), slB)
                        P.tt("dve", AB[:, a, tcs(tc)], sl, pu[:], ALU.mult, list(slB) + [puB], (aB[a][tc],))
                W.release(); W.release()
                for m in range(KT):
                    for tc in range(NTC):
                        pt, ptB = rot_dn.next()
                        for jj in range(nch):
                            cc = jj * D + m * 128
                            P.matmul(pt[:], RW(sd_, cc, cc + 128), AB[:, base + jj, tcs(tc)], jj == 0, jj == nch - 1,
                                     (bd_, aB[base + jj][tc]), (ptB,))
                        add_residual(m, tc, pt, ptB)
                W.release()

        if stage >= 3:
            rmsnorm(PC_LNFFN0)
            ffn(wgu_d, wdn_d, NFF, DFF)

        if stage >= 4:
            rmsnorm(None)
            tmp, tmpB = scr(0, 1)
            lamp, lampB = scr(1, 1)
            P.dma("sp", lamp, lam_d.broadcast_to([128, 256]), (), lampB)
            P.tt("dve", tmp[:, 0:64], lamp[:, 0:64], lamp[:, 64:128], ALU.mult, lampB, tmpB)
            P.reduce(small[:, 8:9], tmp[:, 0:64], ALU.add, tmpB, (smB,))
            P.tt("dve", tmp[:, 64:128], lamp[:, 128:192], lamp[:, 192:256], ALU.mult, lampB, tmpB)
            P.reduce(small[:, 9:10], tmp[:, 64:128], ALU.add, tmpB, (smB,))
            P.act(small[:, 10:12], small[:, 8:10], AF.Exp, (smB,), (smB,))
            P.stt("dve", small[:, 1:2], small[:, 11:12], -LAM_INIT, small[:, 10:11], ALU.add, ALU.subtract, (smB,), (smB,))
            P.ts("dve", small[:, 2:3], pcol(PC_GQ), 0.125, None, ALU.mult, None, (cB,), (smB,))
            P.ts("dve", small[:, 3:4], pcol(PC_GS), 1.0 - LAM_INIT, None, ALU.mult, None, (cB,), (smB,))

            kT0, kT0B = scr(0, 4, BF16)
            kT1, kT1B = scr(4, 4, BF16)
            qTh, qThB = scr(8, 4, BF16)
            Vh, VhB = scr(12, 4, BF16)
            Vh3 = Vh.rearrange("p (t e) -> p t e", t=NT)
            P.ts("dve", masks[:].rearrange("p a b -> p (a b)"), masks[:].rearrange("p a b -> p (a b)"), 30000.0, -30000.0,
                 ALU.mult, ALU.add, (cB,), (cB,))
            P.memset("dve", kT0[64:128, :], 0.0, kT0B)
            P.memset("dve", kT1[0:64, :], 0.0, kT1B)
            rot_s = Rot([0, 1, 2, 3])
            rot_p = rot_s
            gkv3 = prm[:, PC_LNKV:PC_LNKV + KT].unsqueeze(2)
            gm13 = prm[:, PC_LNMIX1:PC_LNMIX1 + KT].unsqueeze(2)
            estep = [0]
            for h in range(8):
                parts = [
                    (0, KT * 128, KT, wkv_d[:, h * 128:(h + 1) * 128].rearrange("(kt p) f -> p kt f", p=128)),
                    (KT * 128, KT * 128, KT, wkv_d[:, D + h * 128:D + (h + 1) * 128].rearrange("(kt p) f -> p kt f", p=128)),
                    (2 * KT * 128, KT * 128, KT, wq_d[:, h * 128:(h + 1) * 128].rearrange("(kt p) f -> p kt f", p=128)),
                ]
                sw_, bw_ = W.get(parts)
                for part, g3 in ((0, gkv3), (1, gkv3), (2, gm13)):
                    wv = RW(sw_, part * 1024, (part + 1) * 1024).rearrange("p (k f) -> p k f", k=KT)
                    P.tt("dve", wv, wv, g3.broadcast_to([128, KT, 128]), ALU.mult, (bw_, cB), (bw_,))
                fin_prev = [None]
                for which in range(2):
                    woff = 0 if which == 0 else 2048
                    for tc in range(NTC):
                        pt, ptB = rot_p.next()
                        for kt in range(KT):
                            c0 = woff + kt * 128
                            P.matmul(pt[:], RW(sw_, c0, c0 + 128), hT[:, kt, tcs(tc)], kt == 0, kt == KT - 1,
                                     (bw_, hB[kt][tc]), (ptB,))
                        slot2 = (4 * which + tc) % 2
                        kqc, kqcB = scr(30 + 2 * slot2, 2)
                        sqh, sqhB = scr(24 + slot2, 1, BF16)
                        P.copy("dve", kqc, pt[:], (ptB,), kqcB)
                        P.tt("dve", sqh, kqc, kqc, ALU.mult, kqcB, sqhB)

                        def fin(which=which, tc=tc, kqc=kqc, kqcB=kqcB, sqh=sqh, sqhB=sqhB):
                            pst, pstB = rot_p.next()
                            P.matmul(pst[:], blk_bf[:], sqh, True, True, list(sqhB) + [cB], (pstB,))
                            lnt, lntB = scr(26, 2)
                            rsh, rshB = scr(28, 2)
                            P.act(lnt, pst[:], AF.Ln, (pstB, smB), lntB, bias=small[:, 0:1], scale=1.0 / 64)
                            P.act(rsh, lnt, AF.Exp, lntB, rshB, scale=-0.5)
                            if which == 0:
                                P.stt("dve", kT0[0:64, tcs(tc)], kqc[0:64, :], prm[0:64, PC_GK:PC_GK + 1], rsh[0:64, :],
                                      ALU.mult, ALU.mult, list(kqcB) + list(rshB) + [cB], kT0B)
                                P.stt("dve", kT1[64:128, tcs(tc)], kqc[64:128, :], prm[64:128, PC_GK:PC_GK + 1], rsh[64:128, :],
                                      ALU.mult, ALU.mult, list(kqcB) + list(rshB) + [cB], kT1B)
                            else:
                                P.stt("dve", qTh[:, tcs(tc)], kqc, small[:, 2:3], rsh,
                                      ALU.mult, ALU.mult, list(kqcB) + list(rshB) + [smB], qThB)
                        if fin_prev[0] is not None:
                            fin_prev[0]()
                        fin_prev[0] = fin
                for tg in range(4):
                    pt, ptB = rot_p.next()
                    for t in range(4):
                        tok0 = (4 * tg + t) * 128
                        for kt in range(KT):
                            c0 = 1024 + kt * 128
                            P.matmul(pt[:, t * 128:(t + 1) * 128], hT[:, kt, tok0:tok0 + 128], RW(sw_, c0, c0 + 128),
                                     kt == 0, kt == KT - 1, (bw_, hB[kt][tg]), (ptB,))
                    P.copy("dve" if tg % 2 else "act", Vh[:, tg * 512:(tg + 1) * 512], pt[:], (ptB,), VhB)
                    if tg == 0:
                        fin_prev[0]()
                W.release()
                LA = 3
                pending = []
                for qc in range(NTC):
                    nk = 4 * qc + 4
                    seq = [(c, ki) for ki in range(nk) for c in (0, 1)]
                    ets = {}
                    acc = [(ps[4], psB[4], ps[6], psB[6]), (ps[5], psB[5], ps[7], psB[7])]
                    for step in range(len(seq) + LA):
                        if step < len(seq):
                            c, ki = seq[step]
                            kTc, kTcB = (kT0, kT0B) if c == 0 else (kT1, kT1B)
                            d = ki - 4 * qc
                            q0 = max(d, 0) * 128
                            pst, pstB = rot_s.next()
                            P.matmul(pst[:, q0:512], kTc[:, ki * 128:(ki + 1) * 128], qTh[:, qc * 512 + q0:(qc + 1) * 512],
                                     True, d < 0, list(kTcB) + list(qThB), (pstB,))
                            if d >= 0:
                                P.matmul(pst[:, q0:512], ident_bf[:], masks[:, d, q0:512], False, True, (cB,), (pstB,))
                            et, etB = scr(16 + (estep[0] % 8), 1, BF16)
                            estep[0] += 1
                            P.act(et[:, q0:512], pst[:, q0:512], AF.Exp, (pstB,), etB)
                            ets[step] = (et, etB, c, ki, q0)
                            if pending and step >= 1:
                                pending.pop(0)()
                        if step >= LA:
                            et, etB, c, ki, q0 = ets[step - LA]
                            po, poB, pz, pzB = acc[c]
                            P.matmul(po[:, q0:512], Vh3[:, ki, :], et[:, q0:512], ki == 0, ki == nk - 1,
                                     list(VhB) + list(etB), (poB,))
                            P.matmul(pz[:, q0:512], ones_bf[:], et[:, q0:512], ki == 0, ki == nk - 1,
                                     list(etB) + [cB], (pzB,))
                    o_aps = []
                    rzs = []
                    for c in range(2):
                        po, poB, pz, pzB = acc[c]
                        rz, rzB = scr(30 + 2 * c, 2)
                        oc, ocB = scr(36 + 2 * c, 2)
                        P.act(rz, pz[:], AF.Ln, (pzB,), rzB)
                        P.copy("dve", oc, po[:], (poB,), ocB)
                        o_aps.append((oc, ocB))
                        rzs.append((rz, rzB))
                    def tail(h=h, qc=qc, rzs=rzs, o_aps=o_aps):
                        (o0, o0B), (o1, o1B) = o_aps
                        osq, osqB = scr(24, 1, BF16)
                        oln, olnB = scr(26, 2)
                        ors, orsB = scr(28, 2)
                        st = {}

                        def stat_mm():
                            st["p"] = rot_s.next()
                            P.matmul(st["p"][0][:], ones_bf[:], osq, True, True, list(osqB) + [cB], (st["p"][1],))
                        ops_ = []
                        for c in range(2):
                            rz, rzB = rzs[c]
                            oc, ocB = o_aps[c]
                            ops_.append(lambda rz=rz, rzB=rzB: P.act(rz, rz, AF.Exp, rzB, rzB, scale=-1.0))
                            ops_.append(lambda oc=oc, ocB=ocB, rz=rz, rzB=rzB: P.tt("dve", oc, oc, rz, ALU.mult, list(ocB) + list(rzB), ocB))
                        ops_.append(lambda: P.stt("dve", o0, o1, small[:, 1:2], o0, ALU.mult, ALU.add,
                                                  list(o0B) + list(o1B) + [smB], o0B))
                        ops_.append(lambda: P.tt("dve", osq, o0, o0, ALU.mult, o0B, osqB))
                        ops_.append(stat_mm)
                        ops_.append(lambda: P.act(oln, st["p"][0][:], AF.Ln, (st["p"][1], smB), olnB, bias=small[:, 0:1], scale=1.0 / 128))
                        ops_.append(lambda: P.act(ors, oln, AF.Exp, olnB, orsB, scale=-0.5))
                        ops_.append(lambda: P.stt("dve", AB[:, h, tcs(qc)], o0, small[:, 3:4], ors, ALU.mult, ALU.mult,
                                                  list(o0B) + list(orsB) + [smB], (aB[h][qc],)))
                        return ops_
                    pending.extend(tail())
                while pending:
                    pending.pop(0)()
            rot = Rot([0, 1, 2, 3, 4, 5, 6, 7])
            for half in range(2):
                so_, bo_ = W.get(wcolblock(wo_d, half * 512, 512))
                linear_fm(AB, aB, KT, so_, 0, 512, bo_, range(4), rot,
                          lambda m, tc, pt, ptB, half=half: add_residual(4 * half + m, tc, pt, ptB))
                W.release()

        if stage >= 5:
            rtr_ap, rtr_bufs = scr(24, 8)
            rtr = rtr_ap.rearrange("p (a b) -> p a b", a=16)
            rtB = rtr_bufs[0]

            def R(i):
                return rtr[:, i, :]

            def R3(i):
                return rtr[:, i, :].rearrange("p (t e) -> p t e", t=NT)
            rstd_tok = rtr[:, 6, 0:16]
            m1 = rtr[:, 6, 16:32]
            m2 = rtr[:, 6, 32:48]
            den = rtr[:, 6, 48:64]
            rden = rtr[:, 6, 64:80]
            P1p = rtr[:, 6, 80:96]
            P2 = rtr[:, 6, 96:112]
            sp = rtr[:, 6, 112:128]
            gw = rtr[:, 7, 0:KT * NE].rearrange("p (k e) -> p k e", k=KT)
            ne_ = rtr[:, 7, 64:72]
            ntl = rtr[:, 7, 72:80]
            tb = rtr[:, 7, 80:88]
            base = rtr[:, 7, 88:96]
            P1 = rtr[:, 7, 96:112]
            G1 = rtr[:, 11, 0:16]
            G2 = rtr[:, 11, 16:32]
            etf = rtr[:, 11, 32:48]
            idx1 = rtr[:, 12, 0:16].bitcast(mybir.dt.int32)
            idx2 = rtr[:, 12, 16:32].bitcast(mybir.dt.int32)
            eid = rtr[:, 12, 32:48].bitcast(mybir.dt.int32)
            selb = rtr[:, 13, 0:64].bitcast(BF16)
            P.memset("dve", rtr_ap, 0.0, rtr_bufs)

            htok = hT[:].rearrange("p k t -> p (k t)").rearrange("p (a b) -> p a b", a=NT)

            def htokB(tt):
                return [hB[tt // 2][2 * (tt % 2)], hB[tt // 2][2 * (tt % 2) + 1]]
            hnc, hncB = scr(32, 8, BF16)
            hnc3 = hnc.rearrange("p (k t) -> p k t", k=KT)
            rot_t = Rot([0, 1, 2])

            def post_rs(tc, rs, rsB):
                pt, ptB = ps[3], psB[3]
                for t in range(4):
                    P.transpose(pt[:, t * 128:(t + 1) * 128], rs[:, t * 128:(t + 1) * 128], ident[:], list(rsB) + [cB], (ptB,))
                P.copy("dve", rstd_tok[:, 4 * tc:4 * tc + 4], pt[:].rearrange("p (t c) -> p t c", c=128)[:, :, 0], (ptB,), (rtB,))

            def post_tc(tc):
                for t in range(4):
                    tt = 4 * tc + t
                    pt, ptB = rot_t.next()
                    ptb = pt[:].bitcast(BF16)
                    for kt in range(KT):
                        P.transpose(ptb[:, kt * 128:(kt + 1) * 128], hnc3[:, kt, t * 128:(t + 1) * 128], ident_bf[:],
                                    list(hncB) + [cB], (ptB,))
                    P.copy("act" if t % 2 else "dve", htok[:, tt, :], ptb, (ptB,), htokB(tt))
            rmsnorm(PC_LNFFN1, post_rs, dst=lambda kt, tc: (hnc3[:, kt, :], hncB), post_tc=post_tc)

            P.tt("dve", gw, rw[:], prm[:, PC_LNFFN1:PC_LNFFN1 + KT].unsqueeze(2).broadcast_to([128, KT, NE]), ALU.mult,
                 (cB, rtB), (rtB,))
            pl, plB = ps[0], psB[0]
            for t in range(NT):
                for kt in range(KT):
                    P.matmul(pl[:, t * NE:(t + 1) * NE], xT[:, kt, t * 128:(t + 1) * 128], gw[:, kt, :], kt == 0, kt == KT - 1,
                             (xB[kt][t // 4], rtB), (plB,))
            bc = lambda v: v.unsqueeze(2).broadcast_to([128, NT, NE])
            DV = lambda *a: P.tt("dve", *a, (rtB,), (rtB,))
            P.tt("dve", R3(0), pl[:, 0:NT * NE].rearrange("p (t e) -> p t e", t=NT), bc(rstd_tok), ALU.mult, (plB, rtB), (rtB,))
            P.reduce(m1, R3(0), ALU.max, (rtB,), (rtB,))
            DV(R3(1), R3(0), bc(m1), ALU.is_equal)
            P.stt("dve", R(2), R(1), -1e30, R(0), ALU.mult, ALU.add, (rtB,), (rtB,))
            P.reduce(m2, R3(2), ALU.max, (rtB,), (rtB,))
            DV(R3(3), R3(0), bc(m2), ALU.is_ge)
            DV(R3(4), R3(0), bc(m1), ALU.subtract)
            P.act(R(4), R(4), AF.Exp, (rtB,), (rtB,))
            DV(R(4), R(4), R(3), ALU.mult)
            P.reduce(den, R3(4), ALU.add, (rtB,), (rtB,))
            P.recip(rden, den, (rtB,), (rtB,))
            DV(R3(5), R3(4), bc(rden), ALU.mult)
            P.copy("dve", selb, R(3), (rtB,), (rtB,))
            pa, paB = ps[1], psB[1]
            pb, pbB = ps[2], psB[2]
            P.matmul(pa[:, 0:128], tri_bf[:], selb, True, True, (rtB, cB), (paB,))
            P.matmul(pb[:, 0:128], ones_bf[:], selb, True, True, (rtB, cB), (pbB,))
            P.copy("dve", R(8), pa[:, 0:128], (paB,), (rtB,))
            P.copy("dve", R(9), pb[:, 0:128], (pbB,), (rtB,))
            src_, dst_ = 9, 10
            for sh in (1, 2, 4, 8):
                P.copy("dve", R3(dst_)[:, 0:sh, :], R3(src_)[:, 0:sh, :], (rtB,), (rtB,))
                DV(R3(dst_)[:, sh:NT, :], R3(src_)[:, sh:NT, :], R3(src_)[:, 0:NT - sh, :], ALU.add)
                src_, dst_ = dst_, (14 if dst_ == 10 else 10)
            P.copy("dve", ne_, R3(src_)[:, NT - 1, :], (rtB,), (rtB,))
            if src_ != 10:
                DV(R(10), R(src_), R(9), ALU.subtract)
            else:
                DV(R(14), R(10), R(9), ALU.subtract)
                P.copy("dve", R(10), R(14), (rtB,), (rtB,))
            P.ts("dve", ntl, ne_, 0.0, None, ALU.is_gt, None, (rtB,), (rtB,))
            for thr in (512.0, 1024.0, 1536.0):
                P.stt("dve", ntl, ne_, thr, ntl, ALU.is_gt, ALU.add, (rtB,), (rtB,))
            P.memset("dve", tb[:, 0:1], 0.0, (rtB,))
            for e in range(1, NE):
                DV(tb[:, e:e + 1], tb[:, e - 1:e], ntl[:, e - 1:e], ALU.add)
            P.ts("dve", base, tb, 512.0, None, ALU.mult, None, (rtB,), (rtB,))
            DV(R(8), R(8), R(10), ALU.add)
            DV(R3(8), R3(8), base.unsqueeze(1).broadcast_to([128, NT, NE]), ALU.add)
            P.stt("dve", R(2), R(8), 1.0, R(3), ALU.add, ALU.mult, (rtB,), (rtB,))
            P.reduce(P1p, R3(2), ALU.max, (rtB,), (rtB,))
            P.reduce(sp, R3(2), ALU.add, (rtB,), (rtB,))
            DV(R3(1), R3(2), bc(P1p), ALU.is_equal)
            DV(R(1), R(1), R(5), ALU.mult)
            P.reduce(G1, R3(1), ALU.add, (rtB,), (rtB,))
            P.ts("dve", G2, G1, -1.0, 1.0, ALU.mult, ALU.add, (rtB,), (rtB,))
            P.ts("dve", P1, P1p, -1.0, None, ALU.add, None, (rtB,), (rtB,))
            P.stt("dve", P2, sp, -1.0, P1p, ALU.add, ALU.subtract, (rtB,), (rtB,))
            P.copy("dve", idx1, P1, (rtB,), (rtB,))
            P.copy("dve", idx2, P2, (rtB,), (rtB,))
            DV(R3(15), prm[:, PC_IOTA:PC_IOTA + 16].unsqueeze(2).broadcast_to([128, NT, NE]),
               tb.unsqueeze(1).broadcast_to([128, NT, NE]), ALU.is_ge)
            P.reduce(etf, R3(15), ALU.add, (rtB, cB), (rtB,))
            P.ts("dve", etf, etf, -1.0, None, ALU.add, None, (rtB,), (rtB,))
            P.copy("dve", eid, etf, (rtB,), (rtB,))
            g_ = nc.gpsimd
            for tt in range(NT):
                for ix in (idx1, idx2):
                    P.op("pool", (lambda ix=ix, tt=tt: g_.indirect_dma_start(
                        out=HG[:, :], out_offset=bass.IndirectOffsetOnAxis(ap=ix[:, tt:tt + 1], axis=0),
                        in_=htok[:, tt, :], in_offset=None)), list(htokB(tt)) + [rtB],
                        (hgB[(2 * tt + (0 if ix is idx1 else 1)) % 16],), dma=True)

            W.go()
            tcnt = [0]
            yacc = hT[:, 0:4, :].rearrange("p k t -> p (k t)").bitcast(F32).rearrange("p (s f) -> p s f", s=4)
            yaccB = lambda s_, fh: [hB[s_][2 * fh], hB[s_][2 * fh + 1]]
            hgs_l, hgsB_l, hgT_l, hgTB_l = [], [], [], []
            hgs_l.append(hT[:, 4:6, :].rearrange("p k t -> p (k t)").rearrange("p (s f) -> p s f", s=4))
            hgsB_l.append([hB[k][t] for k in (4, 5) for t in range(NTC)])
            hgT_l.append(hT[:, 6:8, :].rearrange("p k t -> p (k t)").rearrange("p (k s) -> p k s", k=KT))
            hgTB_l.append(lambda kt: [hB[6 + kt // 4][kt % 4]])
            a_, b_ = scr(8, 8, BF16)
            hgs_l.append(a_.rearrange("p (s f) -> p s f", s=4))
            hgsB_l.append(list(b_))
            a_, b2_ = scr(0, 8, BF16)
            hgT_l.append(a_.rearrange("p (k s) -> p k s", k=KT))
            hgTB_l.append(lambda kt, b2_=b2_: [b2_[kt]])
            actT = lambda a: ABf[:, a * 512:(a + 1) * 512]
            actB = lambda a: aB[a // 4][a % 4]
            rot_gu = Rot([0, 1, 2, 3])
            rot_dn = Rot([4, 5, 6, 7])

            def dyn_part(t, kind, gi):
                re_, rgu_, rdn_ = regs[t % 2]
                if kind == "d":
                    const = gi * 512 * D
                    pat = [[D, 128], [128 * D, 4], [1, D]]
                    th, rb, split = mdn_t, rdn_, 4
                else:
                    const = gi * 512 + (DFE if kind == "u" else 0)
                    pat = [[2 * DFE, 128], [128 * 2 * DFE, KT], [1, 512]]
                    th, rb, split = mgu_t, rgu_, KT
                rt_ = tmpr[tcnt[0] % 4]
                tcnt[0] += 1
                src = (lambda th=th, rt_=rt_, pat=pat: bass.AP(th, rt_, pat))
                return (0, SLOT, split, src, (lambda rt_=rt_, rb=rb, const=const: (g_.reg_add(rt_, rb, const), rt_)[1]))

            def tile_pre(t):
                re_, rgu_, rdn_ = regs[t % 2]

                def fn():
                    g_.reg_load(re_, eid[0:1, t:t + 1])
                    g_.reg_mul(rgu_, re_, D * 2 * DFE)
                    return g_.reg_mul(rdn_, re_, DFE * D)
                return (fn, (rtB,))

            def prep_tile(t):
                par = t % 2
                hgs, hgsB, hgT, hgTB = hgs_l[par], hgsB_l[par], hgT_l[par], hgTB_l[par]
                P.dma("sp", hgs, HG[t * 512:(t + 1) * 512, :].rearrange("(s p) d -> p s d", p=128), hgB, hgsB)
                for kp in range(4):
                    pt, ptB = rot_dn.next()
                    ptb = pt[:].bitcast(BF16)
                    for k2 in range(2):
                        kt = 2 * kp + k2
                        for s_ in range(4):
                            P.transpose(ptb[:, k2 * 512 + s_ * 128:k2 * 512 + (s_ + 1) * 128], hgs[:, s_, kt * 128:(kt + 1) * 128],
                                        ident_bf[:], list(hgsB) + [cB], (ptB,))
                    P.copy("act" if kp % 2 else "dve", hgT[:, 2 * kp:2 * kp + 2, :].rearrange("p k s -> p (k s)"), ptb,
                           (ptB,), hgTB(2 * kp) + hgTB(2 * kp + 1))

            prep_tile(0)
            for t in range(NTILE):
                hgT, hgTB = hgT_l[t % 2], hgTB_l[t % 2]
                for gi in range(7):
                    sg_, bg_ = W.get({"parts": [dyn_part(t, "g", gi)], "pre": tile_pre(t) if gi == 0 else None, "hold": True})
                    su_, bu_ = W.get({"parts": [dyn_part(t, "u", gi)], "pre": None, "hold": True})
                    sd_, bd_ = W.get({"parts": [dyn_part(t, "d", gi)], "pre": None, "hold": True})
                    abase = 4 * (gi % 2)
                    for jj in range(4):
                        a = abase + jj
                        pg, pgB = rot_gu.next()
                        pu, puB = rot_gu.next()
                        for (pt, ptB, slot, wb) in ((pg, pgB, sg_, bg_), (pu, puB, su_, bu_)):
                            for kt in range(KT):
                                cc = kt * 512 + jj * 128
                                P.matmul(pt[:], RW(slot, cc, cc + 128), hgT[:, kt, :], kt == 0, kt == KT - 1,
                                         [wb] + hgTB(kt), (ptB,))
                        sl, slB = scr(32 + 2 * (jj % 2), 2)
                        P.act(sl, pg[:], AF.Silu, (pgB,), slB)
                        P.tt("dve", actT(a), sl, pu[:], ALU.mult, list(slB) + [puB], (actB(a),))
                    W.release(); W.release()
                    if gi == 3 and t + 1 < NTILE:
                        prep_tile(t + 1)
                    for s_ in range(4):
                        for fh in range(2):
                            pt, ptB = rot_dn.next()
                            for jj in range(4):
                                cc = jj * D + fh * 512
                                P.matmul(pt[:], actT(abase + jj)[:, s_ * 128:(s_ + 1) * 128], RW(sd_, cc, cc + 512),
                                         jj == 0, jj == 3, (bd_, actB(abase + jj)), (ptB,))
                            ya = yacc[:, s_, fh * 512:(fh + 1) * 512]
                            if gi == 0:
                                P.copy("act", ya, pt[:], (ptB,), yaccB(s_, fh))
                            else:
                                P.tt("dve", ya, ya, pt[:], ALU.add, [ptB] + yaccB(s_, fh), yaccB(s_, fh))
                    W.release()
                P.dma("sp", YD[t * 512:(t + 1) * 512, :].rearrange("(s p) f -> p s f", p=128), yacc,
                      [b for s_ in range(4) for fh in range(2) for b in yaccB(s_, fh)], (ydB,))

            rot = Rot([0, 1, 2, 3])
            for t in range(NT):
                o3 = (0, 8, 16, 32)[t % 4]
                b1, b1B = scr(o3, 4)
                b2, b2B = scr(o3 + 4, 4)
                for (bb_, bbB_, ix) in ((b1, b1B, idx1), (b2, b2B, idx2)):
                    P.op("pool", (lambda bb_=bb_, ix=ix, t=t: g_.indirect_dma_start(
                        out=bb_, out_offset=None, in_=YD[:, :],
                        in_offset=bass.IndirectOffsetOnAxis(ap=ix[:, t:t + 1], axis=0))), (ydB, rtB), bbB_, dma=True)
                for hf in range(2):
                    pt, ptB = rot.next()
                    for k4 in range(4):
                        kt = 4 * hf + k4
                        P.transpose(pt[:, k4 * 128:(k4 + 1) * 128], xT[:, kt, t * 128:(t + 1) * 128], ident[:],
                                    (xB[kt][t // 4], cB), (ptB,))
                    hs = slice(hf * 512, (hf + 1) * 512)
                    P.stt("dve", b1[:, hs], b1[:, hs], G1[:, t:t + 1], pt[:], ALU.mult, ALU.add,
                          list(b1B) + [ptB, rtB], b1B)
                    P.stt("dve", b1[:, hs], b2[:, hs], G2[:, t:t + 1], b1[:, hs], ALU.mult, ALU.add,
                          list(b2B) + list(b1B) + [rtB], b1B)
                P.dma("sp", out_d[t * 128:(t + 1) * 128, :], b1, b1B, ())
        else:
            rot = Rot([0, 1, 2, 3])
            for t in range(NT):
                ob, obB = scr(4 * (t % 4), 4)
                for hf in range(2):
                    pt, ptB = rot.next()
                    for k4 in range(4):
                        kt = 4 * hf + k4
                        P.transpose(pt[:, k4 * 128:(k4 + 1) * 128], xT[:, kt, t * 128:(t + 1) * 128], ident[:],
                                    (xB[kt][t // 4], cB), (ptB,))
                    P.copy("act" if hf else "dve", ob[:, hf * 512:(hf + 1) * 512], pt[:], (ptB,), obB)
                P.dma("sp", out_d[t * 128:(t + 1) * 128, :], ob, obB, ())

    P.plan = True
    Wp = WStream(P, RW, wB, None)
    body(P, Wp)
    P.plan = False
    first_extra = {NS + j: [aB[2 + 2 * j + k][t] for k in range(2) for t in range(NTC)] for j in range(3)}
    W = WStream(P, RW, wB, Wp.blocks, first_extra)
    body(P, W)
    sems = []

    def sem_alloc(name):
        s = nc.alloc_semaphore(name)
        sems.append(s)
        return s
    n = P.finalize(sem_alloc)
    return nc, n


def host_inputs(inp):
    f = lambda a: np.ascontiguousarray(np.asarray(a, dtype=np.float32))
    prm = np.zeros((128, NPC), np.float32)

    def cols(v):
        return np.asarray(v, np.float32).reshape(KT, 128).T
    prm[:, PC_LNMIX0:PC_LNMIX0 + 8] = cols(inp["ln_mix"][0])
    prm[:, PC_LNFFN0:PC_LNFFN0 + 8] = cols(inp["ln_ffn"][0])
    prm[:, PC_LNKV:PC_LNKV + 8] = cols(inp["ln_kv"])
    prm[:, PC_LNMIX1:PC_LNMIX1 + 8] = cols(inp["ln_mix"][1])
    prm[:, PC_LNFFN1:PC_LNFFN1 + 8] = cols(inp["ln_ffn"][1])
    for j in range(3):
        prm[:, PC_CW + 8 * j:PC_CW + 8 * j + 8] = cols(inp["conv_w"][0][j])
    prm[:, PC_GK] = np.tile(np.asarray(inp["k_norm"], np.float32), 2)
    prm[:, PC_GQ] = np.tile(np.asarray(inp["q_norm"][0], np.float32), 2)
    prm[:, PC_GS] = np.asarray(inp["sub_norm"][0], np.float32)
    prm[:, PC_IOTA:PC_IOTA + 16] = np.arange(16, dtype=np.float32)[None, :]
    ident = np.eye(128, dtype=np.float32)
    k = np.arange(128)[:, None]
    q = np.arange(512)[None, :]
    masks = np.concatenate([(q >= d * 128 + k).astype(np.float32) for d in range(4)], axis=1)
    shared = {
        "params": prm,
        "lam": f(inp["lam_params"]).reshape(1, 256),
        "conv_w_in": f(inp["conv_w_in"][0]),
        "conv_w_out": f(inp["conv_w_out"][0]),
        "w_kv": f(inp["w_kv"]),
        "attn_w_q": f(inp["attn_w_q"][0]),
        "attn_w_o": f(inp["attn_w_o"][0]),
        "ffn_w_gu": f(inp["ffn_w_gu"][0]),
        "ffn_w_down": f(inp["ffn_w_down"][0]),
        "router_w": f(inp["router_w"][0]),
        "moe_w_gu": f(inp["moe_w_gu"][0]),
        "moe_w_down": f(inp["moe_w_down"][0]),
        "ident": ident,
        "tri": np.triu(np.ones((128, 128), np.float32), 1),
        "masks": np.ascontiguousarray(masks),
    }
    x = f(inp["x"])
    return [dict(shared, x=np.ascontiguousarray(x[b])) for b in range(8)]


_CACHE = {}


def kernel(**inputs):
    if "nc" not in _CACHE:
        _CACHE["nc"] = build()[0]
    nc = _CACHE["nc"]
    in_maps = host_inputs(inputs)
    res = run_bass_kernel_spmd(nc, in_maps, core_ids=list(range(8)))
    return np.stack([np.asarray(r["out"], dtype=np.float32) for r in res.results], axis=0)
```

```python
import math
import re
import numpy as np
import concourse.bass as bass
import concourse.mybir as mybir
from concourse.bass_utils import run_bass_kernel_spmd

F32 = mybir.dt.float32
BF16 = mybir.dt.bfloat16
AF = mybir.ActivationFunctionType
ALU = mybir.AluOpType
AX = mybir.AxisListType

D = 1024
S = 2048
KT = 8
NTC = 4
NT = 16
DFF = 2816
NFF = 22
NE = 8
DFE = 3584
NFE = 28
EPS = 1e-6
LAM_INIT = 0.8 - 0.6 * math.exp(-0.3 * 1.0)
NS = 4
SLOT = 4096

PC_LNMIX0, PC_LNFFN0, PC_LNKV, PC_LNMIX1, PC_LNFFN1 = 0, 8, 16, 24, 32
PC_CW = 40
PC_GK, PC_GQ, PC_GS = 64, 65, 66
PC_IOTA = 67
NPC = 83
NTILE = 15
NSLOT = NTILE * 512


class Buf:
    __slots__ = ("name", "lw", "rd")

    def __init__(self, name):
        self.name = name
        self.lw = None
        self.rd = {}


class Prog:
    def __init__(self, nc):
        self.nc = nc
        self.ops = []
        self.plan = False
        self.E = {"pe": nc.tensor, "act": nc.scalar, "dve": nc.vector,
                  "pool": nc.gpsimd, "sp": nc.sync}

    def op(self, eng, fn, r=(), w=(), dma=False):
        if self.plan:
            return
        self.ops.append([eng, fn, tuple(r), tuple(w), dma, None, False, 0])

    def matmul(self, out, lhsT, rhs, start, stop, r, w):
        nc = self.nc
        self.op("pe", lambda: nc.tensor.matmul(out, lhsT, rhs, start=start, stop=stop), r, w)

    def transpose(self, out, in_, ident, r, w):
        nc = self.nc
        self.op("pe", lambda: nc.tensor.transpose(out, in_, ident), r, w)

    def act(self, out, in_, func, r, w, bias=None, scale=None):
        nc = self.nc
        kw = {}
        if bias is not None:
            kw["bias"] = bias
        if scale is not None:
            kw["scale"] = scale
        self.op("act", lambda: nc.scalar.activation(out=out, in_=in_, func=func, **kw), r, w)

    def copy(self, eng, out, in_, r, w):
        nc = self.nc
        if eng == "act":
            self.op("act", lambda: nc.scalar.copy(out=out, in_=in_), r, w)
        else:
            e = self.E[eng]
            self.op(eng, lambda: e.tensor_copy(out=out, in_=in_), r, w)

    def tt(self, eng, out, in0, in1, op, r, w):
        e = self.E[eng]
        self.op(eng, lambda: e.tensor_tensor(out=out, in0=in0, in1=in1, op=op), r, w)

    def ts(self, eng, out, in0, s1, s2, op0, op1, r, w):
        e = self.E[eng]
        if s2 is None:
            self.op(eng, lambda: e.tensor_scalar(out=out, in0=in0, scalar1=s1, scalar2=None, op0=op0), r, w)
        else:
            self.op(eng, lambda: e.tensor_scalar(out=out, in0=in0, scalar1=s1, scalar2=s2, op0=op0, op1=op1), r, w)

    def stt(self, eng, out, in0, scalar, in1, op0, op1, r, w):
        e = self.E[eng]
        self.op(eng, lambda: e.scalar_tensor_tensor(out=out, in0=in0, scalar=scalar, in1=in1, op0=op0, op1=op1), r, w)

    def recip(self, out, in_, r, w):
        nc = self.nc
        self.op("dve", lambda: nc.vector.reciprocal(out=out, in_=in_), r, w)

    def reduce(self, out, in_, op, r, w):
        nc = self.nc
        self.op("dve", lambda: nc.vector.tensor_reduce(out=out, in_=in_, axis=AX.X, op=op), r, w)

    def memset(self, eng, ap, val, w):
        e = self.E[eng]
        self.op(eng, lambda: e.memset(ap, val), (), w)

    def dma(self, q, out, in_, r, w):
        e = self.E[q]
        self.op(q, lambda: e.dma_start(out=out, in_=in_), r, w, dma=True)

    @staticmethod
    def _ckey(o):
        if o[4]:
            b = o[3][0] if o[3] else o[2][0]
            return ("dma", o[0], b.name)
        return o[0]

    def finalize(self, sem_alloc):
        ops = self.ops
        ck = self._ckey
        for i, o in enumerate(ops):
            deps = {}

            def add(j):
                k = ck(ops[j])
                if deps.get(k, -1) < j:
                    deps[k] = j
            for b in o[2]:
                if b.lw is not None:
                    add(b.lw)
            for b in o[3]:
                if b.lw is not None:
                    add(b.lw)
                for j in b.rd.values():
                    add(j)
            if o[0] == "pe" and not o[4]:
                deps.pop("pe", None)
            o[5] = list(deps.values())
            for j in o[5]:
                ops[j][6] = True
            k = ck(o)
            for b in o[2]:
                b.rd[k] = i
            for b in o[3]:
                b.lw = i
                b.rd = {}
        sems = {}
        cnt = {}
        waited = {}
        for o in ops:
            eng = o[0]
            e = self.E[eng]
            need = {}
            for j in o[5]:
                d = ops[j]
                k = ck(d)
                if need.get(k, 0) < d[7]:
                    need[k] = d[7]
            wd = waited.setdefault(eng, {})
            for k, v in need.items():
                if wd.get(k, 0) >= v:
                    continue
                e.wait_ge(sems[k], v)
                wd[k] = v
            ins = o[1]()
            k = ck(o)
            if o[4]:
                if k not in sems:
                    sems[k] = sem_alloc("s_" + "_".join(k))
                    cnt[k] = 0
                cnt[k] += 16
                ins.then_inc(sems[k], 16)
                o[7] = cnt[k]
            elif o[6]:
                if k not in sems:
                    sems[k] = sem_alloc("s_" + k)
                    cnt[k] = 0
                cnt[k] += 1
                ins.then_inc(sems[k], 1)
                o[7] = cnt[k]
        for k, s in sems.items():
            if isinstance(k, tuple):
                self.nc.sync.wait_ge(s, cnt[k])
        return len(ops)


class WStream:
    def __init__(self, P, view, ring_bufs, blocks=None, first_extra=None):
        self.P = P
        self.view = view
        self.bufs = ring_bufs
        self.first_extra = dict(first_extra or {})
        self.ns = NS
        self.base = 0
        self.plan = blocks is None
        self.blocks = [] if blocks is None else blocks
        self.next_get = 0
        self.next_issue = 0
        self.n_released = 0
        self.unheld = False

    def _issue(self):
        i = self.next_issue
        s = (i - self.base) % self.ns
        blk = self.blocks[i]
        if isinstance(blk, dict):
            if blk.get("pre") is not None:
                fn, reads = blk["pre"]
                self.P.op("pool", fn, reads, ())
            parts = blk["parts"]
        else:
            parts = blk
        for part in parts:
            off, n, split, src = part[:4]
            prefn = part[4] if len(part) > 4 else None
            dst = self.view(s, off, off + n).rearrange("p (a b) -> p a b", a=split)
            wbufs = (self.bufs[s],) + tuple(self.first_extra.pop(s, ()))
            if prefn is None:
                self.P.dma("pool", dst, src, (), wbufs)
            else:
                self.P.op("pool", (lambda prefn=prefn, dst=dst, src=src: self._dyn_dma(prefn, dst, src)),
                          (), wbufs, dma=True)
        self.next_issue += 1

    def _dyn_dma(self, prefn, dst, srcfn):
        g = self.P.nc.gpsimd
        rt = prefn()
        ins = g.dma_start(out=dst, in_=srcfn())
        m = re.search(r"R\[(Pool_tmp_(\d+))\]", str(ins.ins))
        if m:
            RH = type(rt)
            n = int(m.group(2))
            names = [m.group(1)] + [f"Pool_{rt.name}_snap_{n - k}" for k in range(1, 5)]
            for nm in names:
                try:
                    g.free_register(RH(nm, rt.engine))
                except ValueError:
                    pass
        return ins

    def _fill(self):
        while self.next_issue < len(self.blocks) and self.next_issue - self.n_released < self.ns:
            blk = self.blocks[self.next_issue]
            if isinstance(blk, dict) and blk.get("hold") and not self.unheld:
                return
            self._issue()

    def start(self):
        if self.plan:
            return
        self._fill()

    def go(self):
        if self.plan:
            self.go_at = self.next_get
            return
        assert self.next_issue == self.n_released == self.next_get, "ring must be drained when go() is called"
        self.unheld = True
        self.base = self.next_issue
        self.ns = len(self.bufs)
        self._fill()

    def get(self, parts):
        i = self.next_get
        self.next_get += 1
        if self.plan:
            self.blocks.append(parts)
            return 0, self.bufs[0]
        sl = (i - self.base) % self.ns
        return sl, self.bufs[sl]

    def release(self):
        if self.plan:
            return
        self.n_released += 1
        self._fill()


def build(stage=99):
    nc = bass.Bass("TRN2", target_bir_lowering=False)
    P = Prog(nc)

    def din(name, shape):
        return nc.dram_tensor(name, shape, F32, kind="ExternalInput").ap()

    x_d = din("x", [S, D])
    params_d = din("params", [128, NPC])
    lam_d = din("lam", [1, 256])
    win_d = din("conv_w_in", [D, 3 * D])
    wout_d = din("conv_w_out", [D, D])
    wkv_d = din("w_kv", [D, 2 * D])
    wq_d = din("attn_w_q", [D, D])
    wo_d = din("attn_w_o", [D, D])
    wgu_d = din("ffn_w_gu", [D, 2 * DFF])
    wdn_d = din("ffn_w_down", [DFF, D])
    rw_d = din("router_w", [D, NE])
    if stage >= 5:
        mgu_t = nc.dram_tensor("moe_w_gu", [NE, D, 2 * DFE], F32, kind="ExternalInput")
        mdn_t = nc.dram_tensor("moe_w_down", [NE, DFE, D], F32, kind="ExternalInput")
    else:
        mgu_t = nc.dram_tensor("moe_w_gu", [NE, 1, 1], F32, kind="ExternalInput")
        mdn_t = nc.dram_tensor("moe_w_down", [NE, 1, 1], F32, kind="ExternalInput")
    mgu_d = mgu_t.ap()
    mdn_d = mdn_t.ap()
    tri_d = din("tri", [128, 128])
    HG = nc.dram_tensor("hg_scratch", [NSLOT, D], BF16, kind="Internal").ap()
    YD = nc.dram_tensor("y_scratch", [NSLOT, D], F32, kind="Internal").ap()
    ident_d = din("ident", [128, 128])
    masks_d = din("masks", [128, 4 * 512])
    out_d = nc.dram_tensor("out", [S, D], F32, kind="ExternalOutput").ap()

    xT = nc.alloc_sbuf_tensor("xT", [128, KT, S], F32)
    hT = nc.alloc_sbuf_tensor("hT", [128, KT, S], BF16)
    AB = nc.alloc_sbuf_tensor("actbuf", [128, KT, S], BF16)
    ring = nc.alloc_sbuf_tensor("wring", [128, NS, SLOT], BF16)
    SCR = nc.alloc_sbuf_tensor("scr", [128, 10240], F32)
    prm = nc.alloc_sbuf_tensor("prm", [128, NPC], F32)
    ident = nc.alloc_sbuf_tensor("ident_sb", [128, 128], F32)
    masks = nc.alloc_sbuf_tensor("masks_sb", [128, 4, 512], BF16)
    ones_f = nc.alloc_sbuf_tensor("ones_f", [8, 128], F32)
    tri_bf = nc.alloc_sbuf_tensor("tri_bf", [128, 128], BF16)
    ident_bf = nc.alloc_sbuf_tensor("ident_bf", [128, 128], BF16)
    rw = nc.alloc_sbuf_tensor("rw_sb", [128, KT, NE], F32)
    ones_bf = nc.alloc_sbuf_tensor("ones_bf", [128, 128], BF16)
    blk_bf = nc.alloc_sbuf_tensor("blk_bf", [128, 128], BF16)
    small = nc.alloc_sbuf_tensor("small", [128, 64], F32)

    xB = [[Buf(f"x{k}_{t}") for t in range(NTC)] for k in range(KT)]
    hB = [[Buf(f"h{k}_{t}") for t in range(NTC)] for k in range(KT)]
    aB = [[Buf(f"a{k}_{t}") for t in range(NTC)] for k in range(KT)]
    wB = [Buf(f"w{s}") for s in range(NS + 3)]
    ABf = AB[:].rearrange("p k t -> p (k t)")

    def RW(sid, lo, hi):
        if sid < NS:
            return ring[:, sid, lo:hi]
        o = SLOT * (sid - NS + 1)
        return ABf[:, o + lo:o + hi]
    sB = [Buf(f"scr{i}") for i in range(40)]
    cB = Buf("consts")
    hgB = [Buf(f"hg_dram{i}") for i in range(16)]
    ydB = Buf("y_dram")
    smB = Buf("small")
    psB = [Buf(f"ps{i}") for i in range(8)]
    ps = [nc.alloc_psum_tensor(f"ps{i}", [128, 512], F32) for i in range(8)]

    def scr(kb_off, kb, dtype=F32):
        lo = int(kb_off * 256)
        hi = int((kb_off + kb) * 256)
        ap = SCR[:, lo:hi]
        if dtype == BF16:
            ap = ap.bitcast(BF16)
        b0 = int(math.floor(kb_off))
        b1 = int(math.ceil(kb_off + kb))
        return ap, sB[b0:b1]

    def tcs(t):
        return slice(t * 512, (t + 1) * 512)

    def pcol(c, n=1):
        return prm[:, c:c + n]

    class Rot:
        def __init__(self, idx):
            self.idx = idx
            self.i = 0

        def next(self):
            b = self.idx[self.i % len(self.idx)]
            self.i += 1
            return ps[b], psB[b]

    regs = {}
    for p_ in range(2):
        regs[p_] = (nc.gpsimd.alloc_register(f"re{p_}"), nc.gpsimd.alloc_register(f"rgu{p_}"),
                    nc.gpsimd.alloc_register(f"rdn{p_}"))
    tmpr = [nc.gpsimd.alloc_register(f"rtmp{i}") for i in range(4)]

    def body(P, W):
        P.dma("pool", prm[:], params_d[:], (), (cB,))
        P.dma("pool", ident[:], ident_d[:], (), (cB,))
        P.dma("pool", masks[:].rearrange("p a b -> p (a b)"), masks_d[:], (), (cB,))
        P.dma("pool", tri_bf[:], tri_d[:], (), (cB,))
        P.dma("pool", ident_bf[:], ident_d[:], (), (cB,))
        P.dma("pool", rw[:], rw_d.rearrange("(kt p) e -> p kt e", p=128), (), (cB,))
        W.start()
        P.memset("dve", ones_bf[:], 1.0, (cB,))
        P.memset("dve", blk_bf[:], 0.0, (cB,))
        P.memset("dve", blk_bf[0:64, 0:64], 1.0, (cB,))
        P.memset("dve", blk_bf[64:128, 64:128], 1.0, (cB,))
        P.memset("dve", ones_f[:], 1.0, (cB,))

        rot = Rot([0, 1, 2, 3])
        for g in range(NTC):
            stg, stgB = scr(16 * (g % 2), 16)
            stg3 = stg.rearrange("p (t d) -> p t d", t=4)
            P.dma("sp", stg3, x_d[g * 512:(g + 1) * 512, :].rearrange("(t p) d -> p t d", p=128), (), stgB)
            for kt in range(KT):
                pt, ptB = rot.next()
                for t in range(4):
                    P.transpose(pt[:, t * 128:(t + 1) * 128], stg3[:, t, kt * 128:(kt + 1) * 128], ident[:],
                                list(stgB) + [cB], (ptB,))
                P.copy("act" if kt % 2 else "dve", xT[:, kt, tcs(g)], pt[:], (ptB,), (xB[kt][g],))

        if stage >= 5:
            zsrc = ABf[:, 12288:16384].rearrange("p (s f) -> p s f", s=4)
            zB = [aB[k][t] for k in (6, 7) for t in range(NTC)]
            P.memset("dve", ABf[:, 12288:16384], 0.0, zB)
            for t in range(NTILE):
                P.dma("sp", HG[t * 512:(t + 1) * 512, :].rearrange("(s p) d -> p s d", p=128), zsrc, zB, (hgB[t % 16],))

        def rmsnorm(gcol, post_rs=None, dst=None, post_tc=None):
            banks = [4, 5, 6, 7]
            for tc in range(NTC):
                sq, sqB = scr(8 * (tc % 2), 8, BF16)
                sq3 = sq.rearrange("p (k t) -> p k t", k=KT)
                P.act(sq3, xT[:, :, tcs(tc)], AF.Square, [xB[k][tc] for k in range(KT)], sqB)
                for kt in range(KT):
                    P.matmul(ps[banks[tc]][:], ones_bf[:], sq3[:, kt, :], kt == 0, kt == KT - 1,
                             list(sqB) + [cB], (psB[banks[tc]],))
            for tc in range(NTC):
                sd, sdB = scr(16 + 4 * (tc % 2), 2)
                rs, rsB = scr(18 + 4 * (tc % 2), 2)
                P.act(sd, ps[banks[tc]][:], AF.Ln, (psB[banks[tc]], smB), sdB, bias=small[:, 0:1], scale=1.0 / D)
                P.act(rs, sd, AF.Exp, sdB, rsB, scale=-0.5)
                if post_rs is not None:
                    post_rs(tc, rs, rsB)
                for kt in range(KT):
                    if dst is None:
                        o_ap, o_b = hT[:, kt, tcs(tc)], (hB[kt][tc],)
                    else:
                        o_ap, o_b = dst(kt, tc)
                    if gcol is None:
                        P.tt("dve", o_ap, xT[:, kt, tcs(tc)], rs, ALU.mult,
                             [xB[kt][tc]] + list(rsB), o_b)
                    else:
                        P.stt("dve", o_ap, xT[:, kt, tcs(tc)], pcol(gcol + kt), rs,
                              ALU.mult, ALU.mult, [xB[kt][tc], cB] + list(rsB), o_b)
                if post_tc is not None:
                    post_tc(tc)

        P.memset("dve", small[:, 0:1], EPS, (smB,))

        def add_residual(m, tc, pt, ptB):
            P.tt("dve", xT[:, m, tcs(tc)], xT[:, m, tcs(tc)], pt[:], ALU.add, (xB[m][tc], ptB), (xB[m][tc],))

        def linear_fm(inp, inB, nk, wslot, woff, wcols, wbuf, m_list, rot, epilogue):
            for m in m_list:
                for tc in range(NTC):
                    pt, ptB = rot.next()
                    for k in range(nk):
                        c0 = woff + k * wcols + m * 128
                        P.matmul(pt[:], RW(wslot, c0, c0 + 128), inp[:, k, tcs(tc)], k == 0, k == nk - 1,
                                 (wbuf, inB[k][tc]), (ptB,))
                    epilogue(m, tc, pt, ptB)

        def wcolblock(src2d, c0, ncols):
            return [(0, KT * ncols, KT, src2d[:, c0:c0 + ncols].rearrange("(kt p) f -> p kt f", p=128))]

        def wrowblock(src2d, r0, nch):
            return [(0, nch * D, nch, src2d[r0:r0 + nch * 128, :].rearrange("(c p) f -> p c f", p=128))]

        if stage >= 2:
            rmsnorm(PC_LNMIX0)
            rot = Rot([0, 1, 2, 3, 4, 5])
            for G in range(2):
                sb_, bb_ = W.get(wcolblock(win_d, G * 512, 512))
                sc_, bc_ = W.get(wcolblock(win_d, D + G * 512, 512))
                sv_, bv_ = W.get(wcolblock(win_d, 2 * D + G * 512, 512))
                for jj in range(4):
                    j = 4 * G + jj
                    ub, ubB = scr(8 * (j % 2), 8)
                    bbuf, bbB = scr(16 + 8 * (j % 2), 8)
                    for tc in range(NTC):
                        pc, pcB = rot.next()
                        pv, pvB = rot.next()
                        pb, pbB = rot.next()
                        for (pt, ptB, slot, wb) in ((pc, pcB, sc_, bc_), (pv, pvB, sv_, bv_), (pb, pbB, sb_, bb_)):
                            for kt in range(KT):
                                c0 = kt * 512 + jj * 128
                                P.matmul(pt[:], RW(slot, c0, c0 + 128), hT[:, kt, tcs(tc)], kt == 0, kt == KT - 1,
                                         (wb, hB[kt][tc]), (ptB,))
                        csb, csbB = scr(32 + 2 * (tc % 2), 2)
                        z, zB = scr(36 + 2 * (tc % 2), 2)
                        P.copy("act", csb, pc[:], (pcB,), csbB)
                        P.tt("dve", ub[:, tcs(tc)], csb, pv[:], ALU.mult, list(csbB) + [pvB], ubB)
                        P.copy("act", bbuf[:, tcs(tc)], pb[:], (pbB,), bbB)
                        lo = tc * 512
                        P.ts("dve", z, ub[:, lo:lo + 512], pcol(PC_CW + 16 + j), None, ALU.mult, None,
                             list(ubB) + [cB], zB)
                        for sh, tap in ((1, 1), (2, 0)):
                            a = sh if tc == 0 else 0
                            P.stt("dve", z[:, a:512], ub[:, lo + a - sh:lo + 512 - sh], pcol(PC_CW + 8 * tap + j),
                                  z[:, a:512], ALU.mult, ALU.add, list(ubB) + list(zB) + [cB], zB)
                        P.tt("dve", AB[:, j, tcs(tc)], bbuf[:, tcs(tc)], z, ALU.mult, list(bbB) + list(zB), (aB[j][tc],))
                W.release(); W.release(); W.release()
            rot = Rot([0, 1, 2, 3, 4, 5, 6, 7])
            for half in range(2):
                so_, bo_ = W.get(wcolblock(wout_d, half * 512, 512))
                linear_fm(AB, aB, KT, so_, 0, 512, bo_, range(4), rot,
                          lambda m, tc, pt, ptB, half=half: add_residual(4 * half + m, tc, pt, ptB))
                W.release()

        def ffn(gu_d, dn_d, nchunks, dff, cT=None):
            rot_gu = Rot([0, 1, 2, 3])
            rot_dn = Rot([4, 5, 6, 7])
            ngrp = (nchunks + 3) // 4
            for gi in range(ngrp):
                c0 = gi * 4
                nch = min(4, nchunks - c0)
                sg_, bg_ = W.get(wcolblock(gu_d, c0 * 128, nch * 128))
                su_, bu_ = W.get(wcolblock(gu_d, dff + c0 * 128, nch * 128))
                sd_, bd_ = W.get(wrowblock(dn_d, c0 * 128, nch))
                base = 4 * (gi % 2)
                for jj in range(nch):
                    a = base + jj
                    for tc in range(NTC):
                        pg, pgB = rot_gu.next()
                        pu, puB = rot_gu.next()
                        for (pt, ptB, slot, wb) in ((pg, pgB, sg_, bg_), (pu, puB, su_, bu_)):
                            for kt in range(KT):
                                cc = kt * nch * 128 + jj * 128
                                P.matmul(pt[:], RW(slot, cc, cc + 128), hT[:, kt, tcs(tc)], kt == 0, kt == KT - 1,
                                         (wb, hB[kt][tc]), (ptB,))
                        sl, slB = scr(32 + 2 * (tc % 2), 2)
                        P.act(sl, pg[:], AF.Silu, (pgB,), slB)
                        if cT is not None:
                            P.tt("dve", sl, sl, cT[0][:, tcs(tc)], ALU.mult, list(slB) + list(cT[1]), slB)
                        P.tt("dve", AB[:, a, tcs(tc)], sl, pu[:], ALU.mult, list(slB) + [puB], (aB[a][tc],))
                W.release(); W.release()
                for m in range(KT):
                    for tc in range(NTC):
                        pt, ptB = rot_dn.next()
                        for jj in range(nch):
                            cc = jj * D + m * 128
                            P.matmul(pt[:], RW(sd_, cc, cc + 128), AB[:, base + jj, tcs(tc)], jj == 0, jj == nch - 1,
                                     (bd_, aB[base + jj][tc]), (ptB,))
                        add_residual(m, tc, pt, ptB)
                W.release()

        if stage >= 3:
            rmsnorm(PC_LNFFN0)
            ffn(wgu_d, wdn_d, NFF, DFF)

        if stage >= 4:
            rmsnorm(None)
            tmp, tmpB = scr(0, 1)
            lamp, lampB = scr(1, 1)
            P.dma("sp", lamp, lam_d.broadcast_to([128, 256]), (), lampB)
            P.tt("dve", tmp[:, 0:64], lamp[:, 0:64], lamp[:, 64:128], ALU.mult, lampB, tmpB)
            P.reduce(small[:, 8:9], tmp[:, 0:64], ALU.add, tmpB, (smB,))
            P.tt("dve", tmp[:, 64:128], lamp[:, 128:192], lamp[:, 192:256], ALU.mult, lampB, tmpB)
            P.reduce(small[:, 9:10], tmp[:, 64:128], ALU.add, tmpB, (smB,))
            P.act(small[:, 10:12], small[:, 8:10], AF.Exp, (smB,), (smB,))
            P.stt("dve", small[:, 1:2], small[:, 11:12], -LAM_INIT, small[:, 10:11], ALU.add, ALU.subtract, (smB,), (smB,))
            P.ts("dve", small[:, 2:3], pcol(PC_GQ), 0.125, None, ALU.mult, None, (cB,), (smB,))
            P.ts("dve", small[:, 3:4], pcol(PC_GS), 1.0 - LAM_INIT, None, ALU.mult, None, (cB,), (smB,))

            kT0, kT0B = scr(0, 4, BF16)
            kT1, kT1B = scr(4, 4, BF16)
            qTh, qThB = scr(8, 4, BF16)
            Vh, VhB = scr(12, 4, BF16)
            Vh3 = Vh.rearrange("p (t e) -> p t e", t=NT)
            P.ts("dve", masks[:].rearrange("p a b -> p (a b)"), masks[:].rearrange("p a b -> p (a b)"), 30000.0, -30000.0,
                 ALU.mult, ALU.add, (cB,), (cB,))
            P.memset("dve", kT0[64:128, :], 0.0, kT0B)
            P.memset("dve", kT1[0:64, :], 0.0, kT1B)
            rot_s = Rot([0, 1, 2, 3])
            rot_p = rot_s
            gkv3 = prm[:, PC_LNKV:PC_LNKV + KT].unsqueeze(2)
            gm13 = prm[:, PC_LNMIX1:PC_LNMIX1 + KT].unsqueeze(2)
            estep = [0]
            for h in range(8):
                parts = [
                    (0, KT * 128, KT, wkv_d[:, h * 128:(h + 1) * 128].rearrange("(kt p) f -> p kt f", p=128)),
                    (KT * 128, KT * 128, KT, wkv_d[:, D + h * 128:D + (h + 1) * 128].rearrange("(kt p) f -> p kt f", p=128)),
                    (2 * KT * 128, KT * 128, KT, wq_d[:, h * 128:(h + 1) * 128].rearrange("(kt p) f -> p kt f", p=128)),
                ]
                sw_, bw_ = W.get(parts)
                for part, g3 in ((0, gkv3), (1, gkv3), (2, gm13)):
                    wv = RW(sw_, part * 1024, (part + 1) * 1024).rearrange("p (k f) -> p k f", k=KT)
                    P.tt("dve", wv, wv, g3.broadcast_to([128, KT, 128]), ALU.mult, (bw_, cB), (bw_,))
                fin_prev = [None]
                for which in range(2):
                    woff = 0 if which == 0 else 2048
                    for tc in range(NTC):
                        pt, ptB = rot_p.next()
                        for kt in range(KT):
                            c0 = woff + kt * 128
                            P.matmul(pt[:], RW(sw_, c0, c0 + 128), hT[:, kt, tcs(tc)], kt == 0, kt == KT - 1,
                                     (bw_, hB[kt][tc]), (ptB,))
                        slot2 = (4 * which + tc) % 2
                        kqc, kqcB = scr(30 + 2 * slot2, 2)
                        sqh, sqhB = scr(24 + slot2, 1, BF16)
                        P.copy("dve", kqc, pt[:], (ptB,), kqcB)
                        P.tt("dve", sqh, kqc, kqc, ALU.mult, kqcB, sqhB)

                        def fin(which=which, tc=tc, kqc=kqc, kqcB=kqcB, sqh=sqh, sqhB=sqhB):
                            pst, pstB = rot_p.next()
                            P.matmul(pst[:], blk_bf[:], sqh, True, True, list(sqhB) + [cB], (pstB,))
                            lnt, lntB = scr(26, 2)
                            rsh, rshB = scr(28, 2)
                            P.act(lnt, pst[:], AF.Ln, (pstB, smB), lntB, bias=small[:, 0:1], scale=1.0 / 64)
                            P.act(rsh, lnt, AF.Exp, lntB, rshB, scale=-0.5)
                            if which == 0:
                                P.stt("dve", kT0[0:64, tcs(tc)], kqc[0:64, :], prm[0:64, PC_GK:PC_GK + 1], rsh[0:64, :],
                                      ALU.mult, ALU.mult, list(kqcB) + list(rshB) + [cB], kT0B)
                                P.stt("dve", kT1[64:128, tcs(tc)], kqc[64:128, :], prm[64:128, PC_GK:PC_GK + 1], rsh[64:128, :],
                                      ALU.mult, ALU.mult, list(kqcB) + list(rshB) + [cB], kT1B)
                            else:
                                P.stt("dve", qTh[:, tcs(tc)], kqc, small[:, 2:3], rsh,
                                      ALU.mult, ALU.mult, list(kqcB) + list(rshB) + [smB], qThB)
                        if fin_prev[0] is not None:
                            fin_prev[0]()
                        fin_prev[0] = fin
                for tg in range(4):
                    pt, ptB = rot_p.next()
                    for t in range(4):
                        tok0 = (4 * tg + t) * 128
                        for kt in range(KT):
                            c0 = 1024 + kt * 128
                            P.matmul(pt[:, t * 128:(t + 1) * 128], hT[:, kt, tok0:tok0 + 128], RW(sw_, c0, c0 + 128),
                                     kt == 0, kt == KT - 1, (bw_, hB[kt][tg]), (ptB,))
                    P.copy("dve" if tg % 2 else "act", Vh[:, tg * 512:(tg + 1) * 512], pt[:], (ptB,), VhB)
                    if tg == 0:
                        fin_prev[0]()
                W.release()
                LA = 3
                pending = []
                for qc in range(NTC):
                    nk = 4 * qc + 4
                    seq = [(c, ki) for ki in range(nk) for c in (0, 1)]
                    ets = {}
                    acc = [(ps[4], psB[4], ps[6], psB[6]), (ps[5], psB[5], ps[7], psB[7])]
                    for step in range(len(seq) + LA):
                        if step < len(seq):
                            c, ki = seq[step]
                            kTc, kTcB = (kT0, kT0B) if c == 0 else (kT1, kT1B)
                            d = ki - 4 * qc
                            q0 = max(d, 0) * 128
                            pst, pstB = rot_s.next()
                            P.matmul(pst[:, q0:512], kTc[:, ki * 128:(ki + 1) * 128], qTh[:, qc * 512 + q0:(qc + 1) * 512],
                                     True, d < 0, list(kTcB) + list(qThB), (pstB,))
                            if d >= 0:
                                P.matmul(pst[:, q0:512], ident_bf[:], masks[:, d, q0:512], False, True, (cB,), (pstB,))
                            et, etB = scr(16 + (estep[0] % 8), 1, BF16)
                            estep[0] += 1
                            P.act(et[:, q0:512], pst[:, q0:512], AF.Exp, (pstB,), etB)
                            ets[step] = (et, etB, c, ki, q0)
                            if pending and step >= 1:
                                pending.pop(0)()
                        if step >= LA:
                            et, etB, c, ki, q0 = ets[step - LA]
                            po, poB, pz, pzB = acc[c]
                            P.matmul(po[:, q0:512], Vh3[:, ki, :], et[:, q0:512], ki == 0, ki == nk - 1,
                                     list(VhB) + list(etB), (poB,))
                            P.matmul(pz[:, q0:512], ones_bf[:], et[:, q0:512], ki == 0, ki == nk - 1,
                                     list(etB) + [cB], (pzB,))
                    o_aps = []
                    rzs = []
                    for c in range(2):
                        po, poB, pz, pzB = acc[c]
                        rz, rzB = scr(30 + 2 * c, 2)
                        oc, ocB = scr(36 + 2 * c, 2)
                        P.act(rz, pz[:], AF.Ln, (pzB,), rzB)
                        P.copy("dve", oc, po[:], (poB,), ocB)
                        o_aps.append((oc, ocB))
                        rzs.append((rz, rzB))
                    def tail(h=h, qc=qc, rzs=rzs, o_aps=o_aps):
                        (o0, o0B), (o1, o1B) = o_aps
                        osq, osqB = scr(24, 1, BF16)
                        oln, olnB = scr(26, 2)
                        ors, orsB = scr(28, 2)
                        st = {}

                        def stat_mm():
                            st["p"] = rot_s.next()
                            P.matmul(st["p"][0][:], ones_bf[:], osq, True, True, list(osqB) + [cB], (st["p"][1],))
                        ops_ = []
                        for c in range(2):
                            rz, rzB = rzs[c]
                            oc, ocB = o_aps[c]
                            ops_.append(lambda rz=rz, rzB=rzB: P.act(rz, rz, AF.Exp, rzB, rzB, scale=-1.0))
                            ops_.append(lambda oc=oc, ocB=ocB, rz=rz, rzB=rzB: P.tt("dve", oc, oc, rz, ALU.mult, list(ocB) + list(rzB), ocB))
                        ops_.append(lambda: P.stt("dve", o0, o1, small[:, 1:2], o0, ALU.mult, ALU.add,
                                                  list(o0B) + list(o1B) + [smB], o0B))
                        ops_.append(lambda: P.tt("dve", osq, o0, o0, ALU.mult, o0B, osqB))
                        ops_.append(stat_mm)
                        ops_.append(lambda: P.act(oln, st["p"][0][:], AF.Ln, (st["p"][1], smB), olnB, bias=small[:, 0:1], scale=1.0 / 128))
                        ops_.append(lambda: P.act(ors, oln, AF.Exp, olnB, orsB, scale=-0.5))
                        ops_.append(lambda: P.stt("dve", AB[:, h, tcs(qc)], o0, small[:, 3:4], ors, ALU.mult, ALU.mult,
                                                  list(o0B) + list(orsB) + [smB], (aB[h][qc],)))
                        return ops_
                    pending.extend(tail())
                while pending:
                    pending.pop(0)()
            rot = Rot([0, 1, 2, 3, 4, 5, 6, 7])
            for half in range(2):
                so_, bo_ = W.get(wcolblock(wo_d, half * 512, 512))
                linear_fm(AB, aB, KT, so_, 0, 512, bo_, range(4), rot,
                          lambda m, tc, pt, ptB, half=half: add_residual(4 * half + m, tc, pt, ptB))
                W.release()

        if stage >= 5:
            rtr_ap, rtr_bufs = scr(24, 8)
            rtr = rtr_ap.rearrange("p (a b) -> p a b", a=16)
            rtB = rtr_bufs[0]

            def R(i):
                return rtr[:, i, :]

            def R3(i):
                return rtr[:, i, :].rearrange("p (t e) -> p t e", t=NT)
            rstd_tok = rtr[:, 6, 0:16]
            m1 = rtr[:, 6, 16:32]
            m2 = rtr[:, 6, 32:48]
            den = rtr[:, 6, 48:64]
            rden = rtr[:, 6, 64:80]
            P1p = rtr[:, 6, 80:96]
            P2 = rtr[:, 6, 96:112]
            sp = rtr[:, 6, 112:128]
            gw = rtr[:, 7, 0:KT * NE].rearrange("p (k e) -> p k e", k=KT)
            ne_ = rtr[:, 7, 64:72]
            ntl = rtr[:, 7, 72:80]
            tb = rtr[:, 7, 80:88]
            base = rtr[:, 7, 88:96]
            P1 = rtr[:, 7, 96:112]
            G1 = rtr[:, 11, 0:16]
            G2 = rtr[:, 11, 16:32]
            etf = rtr[:, 11, 32:48]
            idx1 = rtr[:, 12, 0:16].bitcast(mybir.dt.int32)
            idx2 = rtr[:, 12, 16:32].bitcast(mybir.dt.int32)
            eid = rtr[:, 12, 32:48].bitcast(mybir.dt.int32)
            selb = rtr[:, 13, 0:64].bitcast(BF16)
            P.memset("dve", rtr_ap, 0.0, rtr_bufs)

            htok = hT[:].rearrange("p k t -> p (k t)").rearrange("p (a b) -> p a b", a=NT)

            def htokB(tt):
                return [hB[tt // 2][2 * (tt % 2)], hB[tt // 2][2 * (tt % 2) + 1]]
            hnc, hncB = scr(32, 8, BF16)
            hnc3 = hnc.rearrange("p (k t) -> p k t", k=KT)
            rot_t = Rot([0, 1, 2])

            def post_rs(tc, rs, rsB):
                pt, ptB = ps[3], psB[3]
                for t in range(4):
                    P.transpose(pt[:, t * 128:(t + 1) * 128], rs[:, t * 128:(t + 1) * 128], ident[:], list(rsB) + [cB], (ptB,))
                P.copy("dve", rstd_tok[:, 4 * tc:4 * tc + 4], pt[:].rearrange("p (t c) -> p t c", c=128)[:, :, 0], (ptB,), (rtB,))

            def post_tc(tc):
                for t in range(4):
                    tt = 4 * tc + t
                    pt, ptB = rot_t.next()
                    ptb = pt[:].bitcast(BF16)
                    for kt in range(KT):
                        P.transpose(ptb[:, kt * 128:(kt + 1) * 128], hnc3[:, kt, t * 128:(t + 1) * 128], ident_bf[:],
                                    list(hncB) + [cB], (ptB,))
                    P.copy("act" if t % 2 else "dve", htok[:, tt, :], ptb, (ptB,), htokB(tt))
            rmsnorm(PC_LNFFN1, post_rs, dst=lambda kt, tc: (hnc3[:, kt, :], hncB), post_tc=post_tc)

            P.tt("dve", gw, rw[:], prm[:, PC_LNFFN1:PC_LNFFN1 + KT].unsqueeze(2).broadcast_to([128, KT, NE]), ALU.mult,
                 (cB, rtB), (rtB,))
            pl, plB = ps[0], psB[0]
            for t in range(NT):
                for kt in range(KT):
                    P.matmul(pl[:, t * NE:(t + 1) * NE], xT[:, kt, t * 128:(t + 1) * 128], gw[:, kt, :], kt == 0, kt == KT - 1,
                             (xB[kt][t // 4], rtB), (plB,))
            bc = lambda v: v.unsqueeze(2).broadcast_to([128, NT, NE])
            DV = lambda *a: P.tt("dve", *a, (rtB,), (rtB,))
            P.tt("dve", R3(0), pl[:, 0:NT * NE].rearrange("p (t e) -> p t e", t=NT), bc(rstd_tok), ALU.mult, (plB, rtB), (rtB,))
            def first_one(src, ta, tb_):
                cur = src
                for sh, dst in ((1, ta), (2, tb_), (4, ta)):
                    P.copy("dve", R3(dst)[:, :, 0:sh], R3(cur)[:, :, 0:sh], (rtB,), (rtB,))
                    DV(R3(dst)[:, :, sh:NE], R3(cur)[:, :, sh:NE], R3(cur)[:, :, 0:NE - sh], ALU.add)
                    cur = dst
                P.ts("dve", R(tb_), R(cur), 1.0, None, ALU.is_equal, None, (rtB,), (rtB,))
                DV(R(src), R(src), R(tb_), ALU.mult)
            P.reduce(m1, R3(0), ALU.max, (rtB,), (rtB,))
            DV(R3(1), R3(0), bc(m1), ALU.is_equal)
            first_one(1, 8, 9)
            P.stt("dve", R(2), R(1), -1e30, R(0), ALU.mult, ALU.add, (rtB,), (rtB,))
            P.reduce(m2, R3(2), ALU.max, (rtB,), (rtB,))
            DV(R3(3), R3(2), bc(m2), ALU.is_equal)
            first_one(3, 8, 9)
            DV(R(3), R(3), R(1), ALU.add)
            DV(R3(4), R3(0), bc(m1), ALU.subtract)
            P.act(R(4), R(4), AF.Exp, (rtB,), (rtB,))
            DV(R(4), R(4), R(3), ALU.mult)
            P.reduce(den, R3(4), ALU.add, (rtB,), (rtB,))
            P.recip(rden, den, (rtB,), (rtB,))
            DV(R3(5), R3(4), bc(rden), ALU.mult)
            P.copy("dve", selb, R(3), (rtB,), (rtB,))
            pa, paB = ps[1], psB[1]
            pb, pbB = ps[2], psB[2]
            P.matmul(pa[:, 0:128], tri_bf[:], selb, True, True, (rtB, cB), (paB,))
            P.matmul(pb[:, 0:128], ones_bf[:], selb, True, True, (rtB, cB), (pbB,))
            P.copy("dve", R(8), pa[:, 0:128], (paB,), (rtB,))
            P.copy("dve", R(9), pb[:, 0:128], (pbB,), (rtB,))
            src_, dst_ = 9, 10
            for sh in (1, 2, 4, 8):
                P.copy("dve", R3(dst_)[:, 0:sh, :], R3(src_)[:, 0:sh, :], (rtB,), (rtB,))
                DV(R3(dst_)[:, sh:NT, :], R3(src_)[:, sh:NT, :], R3(src_)[:, 0:NT - sh, :], ALU.add)
                src_, dst_ = dst_, (14 if dst_ == 10 else 10)
            P.copy("dve", ne_, R3(src_)[:, NT - 1, :], (rtB,), (rtB,))
            if src_ != 10:
                DV(R(10), R(src_), R(9), ALU.subtract)
            else:
                DV(R(14), R(10), R(9), ALU.subtract)
                P.copy("dve", R(10), R(14), (rtB,), (rtB,))
            P.ts("dve", ntl, ne_, 0.0, None, ALU.is_gt, None, (rtB,), (rtB,))
            for thr in (512.0, 1024.0, 1536.0):
                P.stt("dve", ntl, ne_, thr, ntl, ALU.is_gt, ALU.add, (rtB,), (rtB,))
            P.memset("dve", tb[:, 0:1], 0.0, (rtB,))
            for e in range(1, NE):
                DV(tb[:, e:e + 1], tb[:, e - 1:e], ntl[:, e - 1:e], ALU.add)
            P.ts("dve", base, tb, 512.0, None, ALU.mult, None, (rtB,), (rtB,))
            DV(R(8), R(8), R(10), ALU.add)
            DV(R3(8), R3(8), base.unsqueeze(1).broadcast_to([128, NT, NE]), ALU.add)
            P.stt("dve", R(2), R(8), 1.0, R(3), ALU.add, ALU.mult, (rtB,), (rtB,))
            P.reduce(P1p, R3(2), ALU.max, (rtB,), (rtB,))
            P.reduce(sp, R3(2), ALU.add, (rtB,), (rtB,))
            DV(R3(1), R3(2), bc(P1p), ALU.is_equal)
            DV(R(1), R(1), R(5), ALU.mult)
            P.reduce(G1, R3(1), ALU.add, (rtB,), (rtB,))
            P.ts("dve", G2, G1, -1.0, 1.0, ALU.mult, ALU.add, (rtB,), (rtB,))
            P.ts("dve", P1, P1p, -1.0, None, ALU.add, None, (rtB,), (rtB,))
            P.stt("dve", P2, sp, -1.0, P1p, ALU.add, ALU.subtract, (rtB,), (rtB,))
            P.copy("dve", idx1, P1, (rtB,), (rtB,))
            P.copy("dve", idx2, P2, (rtB,), (rtB,))
            DV(R3(15), prm[:, PC_IOTA:PC_IOTA + 16].unsqueeze(2).broadcast_to([128, NT, NE]),
               tb.unsqueeze(1).broadcast_to([128, NT, NE]), ALU.is_ge)
            P.reduce(etf, R3(15), ALU.add, (rtB, cB), (rtB,))
            P.ts("dve", etf, etf, -1.0, None, ALU.add, None, (rtB,), (rtB,))
            P.copy("dve", eid, etf, (rtB,), (rtB,))
            g_ = nc.gpsimd
            for tt in range(NT):
                for ix in (idx1, idx2):
                    P.op("pool", (lambda ix=ix, tt=tt: g_.indirect_dma_start(
                        out=HG[:, :], out_offset=bass.IndirectOffsetOnAxis(ap=ix[:, tt:tt + 1], axis=0),
                        in_=htok[:, tt, :], in_offset=None)), list(htokB(tt)) + [rtB],
                        (hgB[(2 * tt + (0 if ix is idx1 else 1)) % 16],), dma=True)

            W.go()
            tcnt = [0]
            yacc = hT[:, 0:4, :].rearrange("p k t -> p (k t)").bitcast(F32).rearrange("p (s f) -> p s f", s=4)
            yaccB = lambda s_, fh: [hB[s_][2 * fh], hB[s_][2 * fh + 1]]
            hgs_l, hgsB_l, hgT_l, hgTB_l = [], [], [], []
            hgs_l.append(hT[:, 4:6, :].rearrange("p k t -> p (k t)").rearrange("p (s f) -> p s f", s=4))
            hgsB_l.append([hB[k][t] for k in (4, 5) for t in range(NTC)])
            hgT_l.append(hT[:, 6:8, :].rearrange("p k t -> p (k t)").rearrange("p (k s) -> p k s", k=KT))
            hgTB_l.append(lambda kt: [hB[6 + kt // 4][kt % 4]])
            a_, b_ = scr(8, 8, BF16)
            hgs_l.append(a_.rearrange("p (s f) -> p s f", s=4))
            hgsB_l.append(list(b_))
            a_, b2_ = scr(0, 8, BF16)
            hgT_l.append(a_.rearrange("p (k s) -> p k s", k=KT))
            hgTB_l.append(lambda kt, b2_=b2_: [b2_[kt]])
            actT = lambda a: ABf[:, a * 512:(a + 1) * 512]
            actB = lambda a: aB[a // 4][a % 4]
            rot_gu = Rot([0, 1, 2, 3])
            rot_dn = Rot([4, 5, 6, 7])

            def dyn_part(t, kind, gi):
                re_, rgu_, rdn_ = regs[t % 2]
                if kind == "d":
                    const = gi * 512 * D
                    pat = [[D, 128], [128 * D, 4], [1, D]]
                    th, rb, split = mdn_t, rdn_, 4
                else:
                    const = gi * 512 + (DFE if kind == "u" else 0)
                    pat = [[2 * DFE, 128], [128 * 2 * DFE, KT], [1, 512]]
                    th, rb, split = mgu_t, rgu_, KT
                rt_ = tmpr[tcnt[0] % 4]
                tcnt[0] += 1
                src = (lambda th=th, rt_=rt_, pat=pat: bass.AP(th, rt_, pat))
                return (0, SLOT, split, src, (lambda rt_=rt_, rb=rb, const=const: (g_.reg_add(rt_, rb, const), rt_)[1]))

            def tile_pre(t):
                re_, rgu_, rdn_ = regs[t % 2]

                def fn():
                    g_.reg_load(re_, eid[0:1, t:t + 1])
                    g_.reg_mul(rgu_, re_, D * 2 * DFE)
                    return g_.reg_mul(rdn_, re_, DFE * D)
                return (fn, (rtB,))

            def prep_tile(t):
                par = t % 2
                hgs, hgsB, hgT, hgTB = hgs_l[par], hgsB_l[par], hgT_l[par], hgTB_l[par]
                P.dma("sp", hgs, HG[t * 512:(t + 1) * 512, :].rearrange("(s p) d -> p s d", p=128), hgB, hgsB)
                for kp in range(4):
                    pt, ptB = rot_dn.next()
                    ptb = pt[:].bitcast(BF16)
                    for k2 in range(2):
                        kt = 2 * kp + k2
                        for s_ in range(4):
                            P.transpose(ptb[:, k2 * 512 + s_ * 128:k2 * 512 + (s_ + 1) * 128], hgs[:, s_, kt * 128:(kt + 1) * 128],
                                        ident_bf[:], list(hgsB) + [cB], (ptB,))
                    P.copy("act" if kp % 2 else "dve", hgT[:, 2 * kp:2 * kp + 2, :].rearrange("p k s -> p (k s)"), ptb,
                           (ptB,), hgTB(2 * kp) + hgTB(2 * kp + 1))

            prep_tile(0)
            for t in range(NTILE):
                hgT, hgTB = hgT_l[t % 2], hgTB_l[t % 2]
                for gi in range(7):
                    sg_, bg_ = W.get({"parts": [dyn_part(t, "g", gi)], "pre": tile_pre(t) if gi == 0 else None, "hold": True})
                    su_, bu_ = W.get({"parts": [dyn_part(t, "u", gi)], "pre": None, "hold": True})
                    sd_, bd_ = W.get({"parts": [dyn_part(t, "d", gi)], "pre": None, "hold": True})
                    abase = 4 * (gi % 2)
                    for jj in range(4):
                        a = abase + jj
                        pg, pgB = rot_gu.next()
                        pu, puB = rot_gu.next()
                        for (pt, ptB, slot, wb) in ((pg, pgB, sg_, bg_), (pu, puB, su_, bu_)):
                            for kt in range(KT):
                                cc = kt * 512 + jj * 128
                                P.matmul(pt[:], RW(slot, cc, cc + 128), hgT[:, kt, :], kt == 0, kt == KT - 1,
                                         [wb] + hgTB(kt), (ptB,))
                        sl, slB = scr(32 + 2 * (jj % 2), 2)
                        P.act(sl, pg[:], AF.Silu, (pgB,), slB)
                        P.tt("dve", actT(a), sl, pu[:], ALU.mult, list(slB) + [puB], (actB(a),))
                    W.release(); W.release()
                    if gi == 3 and t + 1 < NTILE:
                        prep_tile(t + 1)
                    for s_ in range(4):
                        for fh in range(2):
                            pt, ptB = rot_dn.next()
                            for jj in range(4):
                                cc = jj * D + fh * 512
                                P.matmul(pt[:], actT(abase + jj)[:, s_ * 128:(s_ + 1) * 128], RW(sd_, cc, cc + 512),
                                         jj == 0, jj == 3, (bd_, actB(abase + jj)), (ptB,))
                            ya = yacc[:, s_, fh * 512:(fh + 1) * 512]
                            if gi == 0:
                                P.copy("act", ya, pt[:], (ptB,), yaccB(s_, fh))
                            else:
                                P.tt("dve", ya, ya, pt[:], ALU.add, [ptB] + yaccB(s_, fh), yaccB(s_, fh))
                    W.release()
                P.dma("sp", YD[t * 512:(t + 1) * 512, :].rearrange("(s p) f -> p s f", p=128), yacc,
                      [b for s_ in range(4) for fh in range(2) for b in yaccB(s_, fh)], (ydB,))

            rot = Rot([0, 1, 2, 3])
            for t in range(NT):
                o3 = (0, 8, 16, 32)[t % 4]
                b1, b1B = scr(o3, 4)
                b2, b2B = scr(o3 + 4, 4)
                for (bb_, bbB_, ix) in ((b1, b1B, idx1), (b2, b2B, idx2)):
                    P.op("pool", (lambda bb_=bb_, ix=ix, t=t: g_.indirect_dma_start(
                        out=bb_, out_offset=None, in_=YD[:, :],
                        in_offset=bass.IndirectOffsetOnAxis(ap=ix[:, t:t + 1], axis=0))), (ydB, rtB), bbB_, dma=True)
                for hf in range(2):
                    pt, ptB = rot.next()
                    for k4 in range(4):
                        kt = 4 * hf + k4
                        P.transpose(pt[:, k4 * 128:(k4 + 1) * 128], xT[:, kt, t * 128:(t + 1) * 128], ident[:],
                                    (xB[kt][t // 4], cB), (ptB,))
                    hs = slice(hf * 512, (hf + 1) * 512)
                    P.stt("dve", b1[:, hs], b1[:, hs], G1[:, t:t + 1], pt[:], ALU.mult, ALU.add,
                          list(b1B) + [ptB, rtB], b1B)
                    P.stt("dve", b1[:, hs], b2[:, hs], G2[:, t:t + 1], b1[:, hs], ALU.mult, ALU.add,
                          list(b2B) + list(b1B) + [rtB], b1B)
                P.dma("sp", out_d[t * 128:(t + 1) * 128, :], b1, b1B, ())
        else:
            rot = Rot([0, 1, 2, 3])
            for t in range(NT):
                ob, obB = scr(4 * (t % 4), 4)
                for hf in range(2):
                    pt, ptB = rot.next()
                    for k4 in range(4):
                        kt = 4 * hf + k4
                        P.transpose(pt[:, k4 * 128:(k4 + 1) * 128], xT[:, kt, t * 128:(t + 1) * 128], ident[:],
                                    (xB[kt][t // 4], cB), (ptB,))
                    P.copy("act" if hf else "dve", ob[:, hf * 512:(hf + 1) * 512], pt[:], (ptB,), obB)
                P.dma("sp", out_d[t * 128:(t + 1) * 128, :], ob, obB, ())

    P.plan = True
    Wp = WStream(P, RW, wB, None)
    body(P, Wp)
    P.plan = False
    first_extra = {NS + j: [aB[2 + 2 * j + k][t] for k in range(2) for t in range(NTC)] for j in range(3)}
    W = WStream(P, RW, wB, Wp.blocks, first_extra)
    body(P, W)
    sems = []

    def sem_alloc(name):
        s = nc.alloc_semaphore(name)
        sems.append(s)
        return s
    n = P.finalize(sem_alloc)
    return nc, n


def host_inputs(inp):
    f = lambda a: np.ascontiguousarray(np.asarray(a, dtype=np.float32))
    prm = np.zeros((128, NPC), np.float32)

    def cols(v):
        return np.asarray(v, np.float32).reshape(KT, 128).T
    prm[:, PC_LNMIX0:PC_LNMIX0 + 8] = cols(inp["ln_mix"][0])
    prm[:, PC_LNFFN0:PC_LNFFN0 + 8] = cols(inp["ln_ffn"][0])
    prm[:, PC_LNKV:PC_LNKV + 8] = cols(inp["ln_kv"])
    prm[:, PC_LNMIX1:PC_LNMIX1 + 8] = cols(inp["ln_mix"][1])
    prm[:, PC_LNFFN1:PC_LNFFN1 + 8] = cols(inp["ln_ffn"][1])
    for j in range(3):
        prm[:, PC_CW + 8 * j:PC_CW + 8 * j + 8] = cols(inp["conv_w"][0][j])
    prm[:, PC_GK] = np.tile(np.asarray(inp["k_norm"], np.float32), 2)
    prm[:, PC_GQ] = np.tile(np.asarray(inp["q_norm"][0], np.float32), 2)
    prm[:, PC_GS] = np.asarray(inp["sub_norm"][0], np.float32)
    prm[:, PC_IOTA:PC_IOTA + 16] = np.arange(16, dtype=np.float32)[None, :]
    ident = np.eye(128, dtype=np.float32)
    k = np.arange(128)[:, None]
    q = np.arange(512)[None, :]
    masks = np.concatenate([(q >= d * 128 + k).astype(np.float32) for d in range(4)], axis=1)
    shared = {
        "params": prm,
        "lam": f(inp["lam_params"]).reshape(1, 256),
        "conv_w_in": f(inp["conv_w_in"][0]),
        "conv_w_out": f(inp["conv_w_out"][0]),
        "w_kv": f(inp["w_kv"]),
        "attn_w_q": f(inp["attn_w_q"][0]),
        "attn_w_o": f(inp["attn_w_o"][0]),
        "ffn_w_gu": f(inp["ffn_w_gu"][0]),
        "ffn_w_down": f(inp["ffn_w_down"][0]),
        "router_w": f(inp["router_w"][0]),
        "moe_w_gu": f(inp["moe_w_gu"][0]),
        "moe_w_down": f(inp["moe_w_down"][0]),
        "ident": ident,
        "tri": np.triu(np.ones((128, 128), np.float32), 1),
        "masks": np.ascontiguousarray(masks),
    }
    x = f(inp["x"])
    return [dict(shared, x=np.ascontiguousarray(x[b])) for b in range(8)]


_CACHE = {}


def kernel(**inputs):
    if "nc" not in _CACHE:
        _CACHE["nc"] = build()[0]
    nc = _CACHE["nc"]
    in_maps = host_inputs(inputs)
    res = run_bass_kernel_spmd(nc, in_maps, core_ids=list(range(8)))
    return np.stack([np.asarray(r["out"], dtype=np.float32) for r in res.results], axis=0)
```

```python
import math
import re
import numpy as np
import concourse.bass as bass
import concourse.mybir as mybir
from concourse.bass_utils import run_bass_kernel_spmd

F32 = mybir.dt.float32
BF16 = mybir.dt.bfloat16
AF = mybir.ActivationFunctionType
ALU = mybir.AluOpType
AX = mybir.AxisListType

D = 1024
S = 2048
KT = 8
NTC = 4
NT = 16
DFF = 2816
NFF = 22
NE = 8
DFE = 3584
NFE = 28
EPS = 1e-6
LAM_INIT = 0.8 - 0.6 * math.exp(-0.3 * 1.0)
NS = 4
SLOT = 4096

PC_LNMIX0, PC_LNFFN0, PC_LNKV, PC_LNMIX1, PC_LNFFN1 = 0, 8, 16, 24, 32
PC_CW = 40
PC_GK, PC_GQ, PC_GS = 64, 65, 66
PC_IOTA = 67
NPC = 83
NTILE = 15
NSLOT = NTILE * 512


class Buf:
    __slots__ = ("name", "lw", "rd")

    def __init__(self, name):
        self.name = name
        self.lw = None
        self.rd = {}


class Prog:
    def __init__(self, nc):
        self.nc = nc
        self.ops = []
        self.plan = False
        self.cond = None
        self.flag_op = None
        self.flag_ap = None
        self.E = {"pe": nc.tensor, "act": nc.scalar, "dve": nc.vector,
                  "pool": nc.gpsimd, "sp": nc.sync}

    def op(self, eng, fn, r=(), w=(), dma=False):
        if self.plan:
            return
        self.ops.append([eng, fn, tuple(r), tuple(w), dma, None, False, 0, self.cond])

    def matmul(self, out, lhsT, rhs, start, stop, r, w):
        nc = self.nc
        self.op("pe", lambda: nc.tensor.matmul(out, lhsT, rhs, start=start, stop=stop), r, w)

    def transpose(self, out, in_, ident, r, w):
        nc = self.nc
        self.op("pe", lambda: nc.tensor.transpose(out, in_, ident), r, w)

    def act(self, out, in_, func, r, w, bias=None, scale=None):
        nc = self.nc
        kw = {}
        if bias is not None:
            kw["bias"] = bias
        if scale is not None:
            kw["scale"] = scale
        self.op("act", lambda: nc.scalar.activation(out=out, in_=in_, func=func, **kw), r, w)

    def copy(self, eng, out, in_, r, w):
        nc = self.nc
        if eng == "act":
            self.op("act", lambda: nc.scalar.copy(out=out, in_=in_), r, w)
        else:
            e = self.E[eng]
            self.op(eng, lambda: e.tensor_copy(out=out, in_=in_), r, w)

    def tt(self, eng, out, in0, in1, op, r, w):
        e = self.E[eng]
        self.op(eng, lambda: e.tensor_tensor(out=out, in0=in0, in1=in1, op=op), r, w)

    def ts(self, eng, out, in0, s1, s2, op0, op1, r, w):
        e = self.E[eng]
        if s2 is None:
            self.op(eng, lambda: e.tensor_scalar(out=out, in0=in0, scalar1=s1, scalar2=None, op0=op0), r, w)
        else:
            self.op(eng, lambda: e.tensor_scalar(out=out, in0=in0, scalar1=s1, scalar2=s2, op0=op0, op1=op1), r, w)

    def stt(self, eng, out, in0, scalar, in1, op0, op1, r, w):
        e = self.E[eng]
        self.op(eng, lambda: e.scalar_tensor_tensor(out=out, in0=in0, scalar=scalar, in1=in1, op0=op0, op1=op1), r, w)

    def recip(self, out, in_, r, w):
        nc = self.nc
        self.op("dve", lambda: nc.vector.reciprocal(out=out, in_=in_), r, w)

    def reduce(self, out, in_, op, r, w):
        nc = self.nc
        self.op("dve", lambda: nc.vector.tensor_reduce(out=out, in_=in_, axis=AX.X, op=op), r, w)

    def memset(self, eng, ap, val, w):
        e = self.E[eng]
        self.op(eng, lambda: e.memset(ap, val), (), w)

    def dma(self, q, out, in_, r, w):
        e = self.E[q]
        self.op(q, lambda: e.dma_start(out=out, in_=in_), r, w, dma=True)

    @staticmethod
    def _ckey(o):
        if o[4]:
            b = o[3][0] if o[3] else o[2][0]
            return ("dma", o[0], b.name)
        return o[0]

    def finalize(self, sem_alloc):
        ops = self.ops
        ck = self._ckey
        nc = self.nc
        for i, o in enumerate(ops):
            deps = {}

            def add(j):
                k = ck(ops[j])
                if deps.get(k, -1) < j:
                    deps[k] = j
            for b in o[2]:
                if b.lw is not None:
                    add(b.lw)
            for b in o[3]:
                if b.lw is not None:
                    add(b.lw)
                for j in b.rd.values():
                    add(j)
            if o[0] == "pe" and not o[4]:
                deps.pop("pe", None)
            o[5] = list(deps.values())
            for j in o[5]:
                ops[j][6] = True
            k = ck(o)
            for b in o[2]:
                b.rd[k] = i
            for b in o[3]:
                b.lw = i
                b.rd = {}
        if self.flag_op is not None:
            ops[self.flag_op][6] = True
        sems = {}
        cnt = {}
        per_eng = {}
        runs = {}
        for i, o in enumerate(ops):
            k = ck(o)
            eng = o[0]
            per_eng.setdefault(eng, []).append(i)
            rl = runs.setdefault(eng, [])
            if not rl or rl[-1]["tag"] != o[8]:
                rl.append({"tag": o[8], "incs": {}, "pre": {}, "first": i})
            run = rl[-1]
            o.append(len(rl) - 1)
            inc = 16 if o[4] else (1 if o[6] else 0)
            if inc:
                if k not in sems:
                    sems[k] = sem_alloc("s_" + "_".join(k) if isinstance(k, tuple) else "s_" + k)
                    cnt[k] = 0
                if k not in run["pre"]:
                    run["pre"][k] = cnt[k]
                run["incs"][k] = run["incs"].get(k, 0) + inc
                cnt[k] += inc
                o[7] = cnt[k]
        for eng, idxs in per_eng.items():
            e = self.E[eng]
            wd = {}
            cur_run = -1
            guard = None
            snap = None
            freg = None
            flag_waited = False

            def close_run(run):
                guard.__exit__(None, None, None)
                with e.Else():
                    for k2, amt in run["incs"].items():
                        if run["pre"][k2] > 0:
                            e.wait_ge(sems[k2], run["pre"][k2])
                        e.sem_inc(sems[k2], amt)
            for i in idxs:
                o = ops[i]
                if o[9] != cur_run:
                    if guard is not None:
                        close_run(runs[eng][cur_run])
                        guard = None
                        wd = snap
                    cur_run = o[9]
                    run = runs[eng][cur_run]
                    if run["tag"] is not None:
                        if freg is None:
                            freg = e.alloc_register("flag_" + eng)
                        if not flag_waited:
                            fo = ops[self.flag_op]
                            if eng != "dve":
                                e.wait_ge(sems[ck(fo)], fo[7])
                            else:
                                e.wait_ge(sems["dve"], fo[7])
                            flag_waited = True
                        e.reg_load(freg, self.flag_ap[0:1, run["tag"]:run["tag"] + 1])
                        snap = dict(wd)
                        guard = e.If(freg)
                        guard.__enter__()
                need = {}
                for j in o[5]:
                    d = ops[j]
                    k = ck(d)
                    if need.get(k, 0) < d[7]:
                        need[k] = d[7]
                for k, v in need.items():
                    if wd.get(k, 0) >= v:
                        continue
                    e.wait_ge(sems[k], v)
                    wd[k] = v
                ins = o[1]()
                k = ck(o)
                if o[4]:
                    ins.then_inc(sems[k], 16)
                elif o[6]:
                    ins.then_inc(sems[k], 1)
            if guard is not None:
                close_run(runs[eng][cur_run])
        for k, s in sems.items():
            if isinstance(k, tuple):
                nc.sync.wait_ge(s, cnt[k])
        return len(ops)


class WStream:
    def __init__(self, P, view, ring_bufs, blocks=None, first_extra=None):
        self.P = P
        self.view = view
        self.bufs = ring_bufs
        self.first_extra = dict(first_extra or {})
        self.ns = NS
        self.base = 0
        self.plan = blocks is None
        self.blocks = [] if blocks is None else blocks
        self.next_get = 0
        self.next_issue = 0
        self.n_released = 0
        self.unheld = False

    def _issue(self):
        i = self.next_issue
        s = (i - self.base) % self.ns
        blk = self.blocks[i]
        saved_cond = self.P.cond
        self.P.cond = blk.get("cond") if isinstance(blk, dict) else None
        if isinstance(blk, dict):
            if blk.get("pre") is not None:
                fn, reads = blk["pre"]
                self.P.op("pool", fn, reads, ())
            parts = blk["parts"]
        else:
            parts = blk
        for part in parts:
            off, n, split, src = part[:4]
            prefn = part[4] if len(part) > 4 else None
            dst = self.view(s, off, off + n).rearrange("p (a b) -> p a b", a=split)
            wbufs = (self.bufs[s],) + tuple(self.first_extra.pop(s, ()))
            if prefn is None:
                self.P.dma("pool", dst, src, (), wbufs)
            else:
                self.P.op("pool", (lambda prefn=prefn, dst=dst, src=src: self._dyn_dma(prefn, dst, src)),
                          (), wbufs, dma=True)
        self.P.cond = saved_cond
        self.next_issue += 1

    def _dyn_dma(self, prefn, dst, srcfn):
        g = self.P.nc.gpsimd
        rt = prefn()
        ins = g.dma_start(out=dst, in_=srcfn())
        m = re.search(r"R\[(Pool_tmp_(\d+))\]", str(ins.ins))
        if m:
            RH = type(rt)
            n = int(m.group(2))
            names = [m.group(1)] + [f"Pool_{rt.name}_snap_{n - k}" for k in range(1, 5)]
            for nm in names:
                try:
                    g.free_register(RH(nm, rt.engine))
                except ValueError:
                    pass
        return ins

    def _fill(self):
        while self.next_issue < len(self.blocks) and self.next_issue - self.n_released < self.ns:
            blk = self.blocks[self.next_issue]
            if isinstance(blk, dict) and blk.get("hold") and not self.unheld:
                return
            self._issue()

    def start(self):
        if self.plan:
            return
        self._fill()

    def go(self):
        if self.plan:
            self.go_at = self.next_get
            return
        assert self.next_issue == self.n_released == self.next_get, "ring must be drained when go() is called"
        self.unheld = True
        self.base = self.next_issue
        self.ns = len(self.bufs)
        self._fill()

    def get(self, parts):
        i = self.next_get
        self.next_get += 1
        if self.plan:
            self.blocks.append(parts)
            return 0, self.bufs[0]
        sl = (i - self.base) % self.ns
        return sl, self.bufs[sl]

    def release(self):
        if self.plan:
            return
        self.n_released += 1
        self._fill()


def build(stage=99):
    nc = bass.Bass("TRN2", target_bir_lowering=False)
    P = Prog(nc)

    def din(name, shape):
        return nc.dram_tensor(name, shape, F32, kind="ExternalInput").ap()

    x_d = din("x", [S, D])
    params_d = din("params", [128, NPC])
    lam_d = din("lam", [1, 256])
    win_d = din("conv_w_in", [D, 3 * D])
    wout_d = din("conv_w_out", [D, D])
    wkv_d = din("w_kv", [D, 2 * D])
    wq_d = din("attn_w_q", [D, D])
    wo_d = din("attn_w_o", [D, D])
    wgu_d = din("ffn_w_gu", [D, 2 * DFF])
    wdn_d = din("ffn_w_down", [DFF, D])
    rw_d = din("router_w", [D, NE])
    if stage >= 5:
        mgu_t = nc.dram_tensor("moe_w_gu", [NE, D, 2 * DFE], F32, kind="ExternalInput")
        mdn_t = nc.dram_tensor("moe_w_down", [NE, DFE, D], F32, kind="ExternalInput")
    else:
        mgu_t = nc.dram_tensor("moe_w_gu", [NE, 1, 1], F32, kind="ExternalInput")
        mdn_t = nc.dram_tensor("moe_w_down", [NE, 1, 1], F32, kind="ExternalInput")
    mgu_d = mgu_t.ap()
    mdn_d = mdn_t.ap()
    tri_d = din("tri", [128, 128])
    HG = nc.dram_tensor("hg_scratch", [NSLOT, D], BF16, kind="Internal").ap()
    YD = nc.dram_tensor("y_scratch", [NSLOT, D], F32, kind="Internal").ap()
    ident_d = din("ident", [128, 128])
    masks_d = din("masks", [128, 4 * 512])
    out_d = nc.dram_tensor("out", [S, D], F32, kind="ExternalOutput").ap()

    xT = nc.alloc_sbuf_tensor("xT", [128, KT, S], F32)
    hT = nc.alloc_sbuf_tensor("hT", [128, KT, S], BF16)
    AB = nc.alloc_sbuf_tensor("actbuf", [128, KT, S], BF16)
    ring = nc.alloc_sbuf_tensor("wring", [128, NS, SLOT], BF16)
    SCR = nc.alloc_sbuf_tensor("scr", [128, 10240], F32)
    prm = nc.alloc_sbuf_tensor("prm", [128, NPC], F32)
    ident = nc.alloc_sbuf_tensor("ident_sb", [128, 128], F32)
    masks = nc.alloc_sbuf_tensor("masks_sb", [128, 4, 512], BF16)
    ones_f = nc.alloc_sbuf_tensor("ones_f", [8, 128], F32)
    tri_bf = nc.alloc_sbuf_tensor("tri_bf", [128, 128], BF16)
    ident_bf = nc.alloc_sbuf_tensor("ident_bf", [128, 128], BF16)
    rw = nc.alloc_sbuf_tensor("rw_sb", [128, KT, NE], F32)
    ones_bf = nc.alloc_sbuf_tensor("ones_bf", [128, 128], BF16)
    blk_bf = nc.alloc_sbuf_tensor("blk_bf", [128, 128], BF16)
    small = nc.alloc_sbuf_tensor("small", [128, 64], F32)

    xB = [[Buf(f"x{k}_{t}") for t in range(NTC)] for k in range(KT)]
    hB = [[Buf(f"h{k}_{t}") for t in range(NTC)] for k in range(KT)]
    aB = [[Buf(f"a{k}_{t}") for t in range(NTC)] for k in range(KT)]
    wB = [Buf(f"w{s}") for s in range(NS + 3)]
    ABf = AB[:].rearrange("p k t -> p (k t)")

    def RW(sid, lo, hi):
        if sid < NS:
            return ring[:, sid, lo:hi]
        o = SLOT * (sid - NS + 1)
        return ABf[:, o + lo:o + hi]
    sB = [Buf(f"scr{i}") for i in range(40)]
    cB = Buf("consts")
    hgB = [Buf(f"hg_dram{i}") for i in range(16)]
    ydB = Buf("y_dram")
    smB = Buf("small")
    psB = [Buf(f"ps{i}") for i in range(8)]
    ps = [nc.alloc_psum_tensor(f"ps{i}", [128, 512], F32) for i in range(8)]

    def scr(kb_off, kb, dtype=F32):
        lo = int(kb_off * 256)
        hi = int((kb_off + kb) * 256)
        ap = SCR[:, lo:hi]
        if dtype == BF16:
            ap = ap.bitcast(BF16)
        b0 = int(math.floor(kb_off))
        b1 = int(math.ceil(kb_off + kb))
        return ap, sB[b0:b1]

    def tcs(t):
        return slice(t * 512, (t + 1) * 512)

    def pcol(c, n=1):
        return prm[:, c:c + n]

    class Rot:
        def __init__(self, idx):
            self.idx = idx
            self.i = 0

        def next(self):
            b = self.idx[self.i % len(self.idx)]
            self.i += 1
            return ps[b], psB[b]

    regs = {}
    for p_ in range(2):
        regs[p_] = (nc.gpsimd.alloc_register(f"re{p_}"), nc.gpsimd.alloc_register(f"rgu{p_}"),
                    nc.gpsimd.alloc_register(f"rdn{p_}"))
    tmpr = [nc.gpsimd.alloc_register(f"rtmp{i}") for i in range(4)]

    def body(P, W):
        P.dma("pool", prm[:], params_d[:], (), (cB,))
        P.dma("pool", ident[:], ident_d[:], (), (cB,))
        P.dma("pool", masks[:].rearrange("p a b -> p (a b)"), masks_d[:], (), (cB,))
        P.dma("pool", tri_bf[:], tri_d[:], (), (cB,))
        P.dma("pool", ident_bf[:], ident_d[:], (), (cB,))
        P.dma("pool", rw[:], rw_d.rearrange("(kt p) e -> p kt e", p=128), (), (cB,))
        W.start()
        P.memset("dve", ones_bf[:], 1.0, (cB,))
        P.memset("dve", blk_bf[:], 0.0, (cB,))
        P.memset("dve", blk_bf[0:64, 0:64], 1.0, (cB,))
        P.memset("dve", blk_bf[64:128, 64:128], 1.0, (cB,))
        P.memset("dve", ones_f[:], 1.0, (cB,))

        rot = Rot([0, 1, 2, 3])
        for g in range(NTC):
            stg, stgB = scr(16 * (g % 2), 16)
            stg3 = stg.rearrange("p (t d) -> p t d", t=4)
            P.dma("sp", stg3, x_d[g * 512:(g + 1) * 512, :].rearrange("(t p) d -> p t d", p=128), (), stgB)
            for kt in range(KT):
                pt, ptB = rot.next()
                for t in range(4):
                    P.transpose(pt[:, t * 128:(t + 1) * 128], stg3[:, t, kt * 128:(kt + 1) * 128], ident[:],
                                list(stgB) + [cB], (ptB,))
                P.copy("act" if kt % 2 else "dve", xT[:, kt, tcs(g)], pt[:], (ptB,), (xB[kt][g],))

        if stage >= 5:
            zsrc = ABf[:, 12288:16384].rearrange("p (s f) -> p s f", s=4)
            zB = [aB[k][t] for k in (6, 7) for t in range(NTC)]
            P.memset("dve", ABf[:, 12288:16384], 0.0, zB)
            for t in range(NTILE):
                P.dma("sp", HG[t * 512:(t + 1) * 512, :].rearrange("(s p) d -> p s d", p=128), zsrc, zB, (hgB[t % 16],))

        def rmsnorm(gcol, post_rs=None, dst=None, post_tc=None):
            banks = [4, 5, 6, 7]
            for tc in range(NTC):
                sq, sqB = scr(8 * (tc % 2), 8, BF16)
                sq3 = sq.rearrange("p (k t) -> p k t", k=KT)
                P.act(sq3, xT[:, :, tcs(tc)], AF.Square, [xB[k][tc] for k in range(KT)], sqB)
                for kt in range(KT):
                    P.matmul(ps[banks[tc]][:], ones_bf[:], sq3[:, kt, :], kt == 0, kt == KT - 1,
                             list(sqB) + [cB], (psB[banks[tc]],))
            for tc in range(NTC):
                sd, sdB = scr(16 + 4 * (tc % 2), 2)
                rs, rsB = scr(18 + 4 * (tc % 2), 2)
                P.act(sd, ps[banks[tc]][:], AF.Ln, (psB[banks[tc]], smB), sdB, bias=small[:, 0:1], scale=1.0 / D)
                P.act(rs, sd, AF.Exp, sdB, rsB, scale=-0.5)
                if post_rs is not None:
                    post_rs(tc, rs, rsB)
                for kt in range(KT):
                    if dst is None:
                        o_ap, o_b = hT[:, kt, tcs(tc)], (hB[kt][tc],)
                    else:
                        o_ap, o_b = dst(kt, tc)
                    if gcol is None:
                        P.tt("dve", o_ap, xT[:, kt, tcs(tc)], rs, ALU.mult,
                             [xB[kt][tc]] + list(rsB), o_b)
                    else:
                        P.stt("dve", o_ap, xT[:, kt, tcs(tc)], pcol(gcol + kt), rs,
                              ALU.mult, ALU.mult, [xB[kt][tc], cB] + list(rsB), o_b)
                if post_tc is not None:
                    post_tc(tc)

        P.memset("dve", small[:, 0:1], EPS, (smB,))

        def add_residual(m, tc, pt, ptB):
            P.tt("dve", xT[:, m, tcs(tc)], xT[:, m, tcs(tc)], pt[:], ALU.add, (xB[m][tc], ptB), (xB[m][tc],))

        def linear_fm(inp, inB, nk, wslot, woff, wcols, wbuf, m_list, rot, epilogue):
            for m in m_list:
                for tc in range(NTC):
                    pt, ptB = rot.next()
                    for k in range(nk):
                        c0 = woff + k * wcols + m * 128
                        P.matmul(pt[:], RW(wslot, c0, c0 + 128), inp[:, k, tcs(tc)], k == 0, k == nk - 1,
                                 (wbuf, inB[k][tc]), (ptB,))
                    epilogue(m, tc, pt, ptB)

        def wcolblock(src2d, c0, ncols):
            return [(0, KT * ncols, KT, src2d[:, c0:c0 + ncols].rearrange("(kt p) f -> p kt f", p=128))]

        def wrowblock(src2d, r0, nch):
            return [(0, nch * D, nch, src2d[r0:r0 + nch * 128, :].rearrange("(c p) f -> p c f", p=128))]

        if stage >= 2:
            rmsnorm(PC_LNMIX0)
            rot = Rot([0, 1, 2, 3, 4, 5])
            for G in range(2):
                sb_, bb_ = W.get(wcolblock(win_d, G * 512, 512))
                sc_, bc_ = W.get(wcolblock(win_d, D + G * 512, 512))
                sv_, bv_ = W.get(wcolblock(win_d, 2 * D + G * 512, 512))
                for jj in range(4):
                    j = 4 * G + jj
                    ub, ubB = scr(8 * (j % 2), 8)
                    bbuf, bbB = scr(16 + 8 * (j % 2), 8)
                    for tc in range(NTC):
                        pc, pcB = rot.next()
                        pv, pvB = rot.next()
                        pb, pbB = rot.next()
                        for (pt, ptB, slot, wb) in ((pc, pcB, sc_, bc_), (pv, pvB, sv_, bv_), (pb, pbB, sb_, bb_)):
                            for kt in range(KT):
                                c0 = kt * 512 + jj * 128
                                P.matmul(pt[:], RW(slot, c0, c0 + 128), hT[:, kt, tcs(tc)], kt == 0, kt == KT - 1,
                                         (wb, hB[kt][tc]), (ptB,))
                        csb, csbB = scr(32 + 2 * (tc % 2), 2)
                        z, zB = scr(36 + 2 * (tc % 2), 2)
                        P.copy("act", csb, pc[:], (pcB,), csbB)
                        P.tt("dve", ub[:, tcs(tc)], csb, pv[:], ALU.mult, list(csbB) + [pvB], ubB)
                        P.copy("act", bbuf[:, tcs(tc)], pb[:], (pbB,), bbB)
                        lo = tc * 512
                        P.ts("dve", z, ub[:, lo:lo + 512], pcol(PC_CW + 16 + j), None, ALU.mult, None,
                             list(ubB) + [cB], zB)
                        for sh, tap in ((1, 1), (2, 0)):
                            a = sh if tc == 0 else 0
                            P.stt("dve", z[:, a:512], ub[:, lo + a - sh:lo + 512 - sh], pcol(PC_CW + 8 * tap + j),
                                  z[:, a:512], ALU.mult, ALU.add, list(ubB) + list(zB) + [cB], zB)
                        P.tt("dve", AB[:, j, tcs(tc)], bbuf[:, tcs(tc)], z, ALU.mult, list(bbB) + list(zB), (aB[j][tc],))
                W.release(); W.release(); W.release()
            rot = Rot([0, 1, 2, 3, 4, 5, 6, 7])
            for half in range(2):
                so_, bo_ = W.get(wcolblock(wout_d, half * 512, 512))
                linear_fm(AB, aB, KT, so_, 0, 512, bo_, range(4), rot,
                          lambda m, tc, pt, ptB, half=half: add_residual(4 * half + m, tc, pt, ptB))
                W.release()

        def ffn(gu_d, dn_d, nchunks, dff, cT=None):
            rot_gu = Rot([0, 1, 2, 3])
            rot_dn = Rot([4, 5, 6, 7])
            ngrp = (nchunks + 3) // 4
            for gi in range(ngrp):
                c0 = gi * 4
                nch = min(4, nchunks - c0)
                sg_, bg_ = W.get(wcolblock(gu_d, c0 * 128, nch * 128))
                su_, bu_ = W.get(wcolblock(gu_d, dff + c0 * 128, nch * 128))
                sd_, bd_ = W.get(wrowblock(dn_d, c0 * 128, nch))
                base = 4 * (gi % 2)
                for jj in range(nch):
                    a = base + jj
                    for tc in range(NTC):
                        pg, pgB = rot_gu.next()
                        pu, puB = rot_gu.next()
                        for (pt, ptB, slot, wb) in ((pg, pgB, sg_, bg_), (pu, puB, su_, bu_)):
                            for kt in range(KT):
                                cc = kt * nch * 128 + jj * 128
                                P.matmul(pt[:], RW(slot, cc, cc + 128), hT[:, kt, tcs(tc)], kt == 0, kt == KT - 1,
                                         (wb, hB[kt][tc]), (ptB,))
                        sl, slB = scr(32 + 2 * (tc % 2), 2)
                        P.act(sl, pg[:], AF.Silu, (pgB,), slB)
                        if cT is not None:
                            P.tt("dve", sl, sl, cT[0][:, tcs(tc)], ALU.mult, list(slB) + list(cT[1]), slB)
                        P.tt("dve", AB[:, a, tcs(tc)], sl, pu[:], ALU.mult, list(slB) + [puB], (aB[a][tc],))
                W.release(); W.release()
                for m in range(KT):
                    for tc in range(NTC):
                        pt, ptB = rot_dn.next()
                        for jj in range(nch):
                            cc = jj * D + m * 128
                            P.matmul(pt[:], RW(sd_, cc, cc + 128), AB[:, base + jj, tcs(tc)], jj == 0, jj == nch - 1,
                                     (bd_, aB[base + jj][tc]), (ptB,))
                        add_residual(m, tc, pt, ptB)
                W.release()

        if stage >= 3:
            rmsnorm(PC_LNFFN0)
            if stage >= 5:
                zy, zyB = scr(36, 4)
                P.memset("dve", zy, 0.0, zyB)
                for t in range(8, NTILE):
                    for q4 in range(4):
                        P.dma("sp", YD[t * 512 + q4 * 128:t * 512 + (q4 + 1) * 128, :], zy, zyB, (ydB,))
            ffn(wgu_d, wdn_d, NFF, DFF)

        if stage >= 4:
            rmsnorm(None)
            tmp, tmpB = scr(0, 1)
            lamp, lampB = scr(1, 1)
            P.dma("sp", lamp, lam_d.broadcast_to([128, 256]), (), lampB)
            P.tt("dve", tmp[:, 0:64], lamp[:, 0:64], lamp[:, 64:128], ALU.mult, lampB, tmpB)
            P.reduce(small[:, 8:9], tmp[:, 0:64], ALU.add, tmpB, (smB,))
            P.tt("dve", tmp[:, 64:128], lamp[:, 128:192], lamp[:, 192:256], ALU.mult, lampB, tmpB)
            P.reduce(small[:, 9:10], tmp[:, 64:128], ALU.add, tmpB, (smB,))
            P.act(small[:, 10:12], small[:, 8:10], AF.Exp, (smB,), (smB,))
            P.stt("dve", small[:, 1:2], small[:, 11:12], -LAM_INIT, small[:, 10:11], ALU.add, ALU.subtract, (smB,), (smB,))
            P.ts("dve", small[:, 2:3], pcol(PC_GQ), 0.125, None, ALU.mult, None, (cB,), (smB,))
            P.ts("dve", small[:, 3:4], pcol(PC_GS), 1.0 - LAM_INIT, None, ALU.mult, None, (cB,), (smB,))

            kT0, kT0B = scr(0, 4, BF16)
            kT1, kT1B = scr(4, 4, BF16)
            qTh, qThB = scr(8, 4, BF16)
            Vh, VhB = scr(12, 4, BF16)
            Vh3 = Vh.rearrange("p (t e) -> p t e", t=NT)
            P.ts("dve", masks[:].rearrange("p a b -> p (a b)"), masks[:].rearrange("p a b -> p (a b)"), 30000.0, -30000.0,
                 ALU.mult, ALU.add, (cB,), (cB,))
            P.memset("dve", kT0[64:128, :], 0.0, kT0B)
            P.memset("dve", kT1[0:64, :], 0.0, kT1B)
            rot_s = Rot([0, 1, 2, 3])
            rot_p = rot_s
            gkv3 = prm[:, PC_LNKV:PC_LNKV + KT].unsqueeze(2)
            gm13 = prm[:, PC_LNMIX1:PC_LNMIX1 + KT].unsqueeze(2)
            estep = [0]
            for h in range(8):
                parts = [
                    (0, KT * 128, KT, wkv_d[:, h * 128:(h + 1) * 128].rearrange("(kt p) f -> p kt f", p=128)),
                    (KT * 128, KT * 128, KT, wkv_d[:, D + h * 128:D + (h + 1) * 128].rearrange("(kt p) f -> p kt f", p=128)),
                    (2 * KT * 128, KT * 128, KT, wq_d[:, h * 128:(h + 1) * 128].rearrange("(kt p) f -> p kt f", p=128)),
                ]
                sw_, bw_ = W.get(parts)
                for part, g3 in ((0, gkv3), (1, gkv3), (2, gm13)):
                    wv = RW(sw_, part * 1024, (part + 1) * 1024).rearrange("p (k f) -> p k f", k=KT)
                    P.tt("dve", wv, wv, g3.broadcast_to([128, KT, 128]), ALU.mult, (bw_, cB), (bw_,))
                fin_prev = [None]
                for which in range(2):
                    woff = 0 if which == 0 else 2048
                    for tc in range(NTC):
                        pt, ptB = rot_p.next()
                        for kt in range(KT):
                            c0 = woff + kt * 128
                            P.matmul(pt[:], RW(sw_, c0, c0 + 128), hT[:, kt, tcs(tc)], kt == 0, kt == KT - 1,
                                     (bw_, hB[kt][tc]), (ptB,))
                        slot2 = (4 * which + tc) % 2
                        kqc, kqcB = scr(30 + 2 * slot2, 2)
                        sqh, sqhB = scr(24 + slot2, 1, BF16)
                        P.copy("dve", kqc, pt[:], (ptB,), kqcB)
                        P.tt("dve", sqh, kqc, kqc, ALU.mult, kqcB, sqhB)

                        def fin(which=which, tc=tc, kqc=kqc, kqcB=kqcB, sqh=sqh, sqhB=sqhB):
                            pst, pstB = rot_p.next()
                            P.matmul(pst[:], blk_bf[:], sqh, True, True, list(sqhB) + [cB], (pstB,))
                            lnt, lntB = scr(26, 2)
                            rsh, rshB = scr(28, 2)
                            P.act(lnt, pst[:], AF.Ln, (pstB, smB), lntB, bias=small[:, 0:1], scale=1.0 / 64)
                            P.act(rsh, lnt, AF.Exp, lntB, rshB, scale=-0.5)
                            if which == 0:
                                P.stt("dve", kT0[0:64, tcs(tc)], kqc[0:64, :], prm[0:64, PC_GK:PC_GK + 1], rsh[0:64, :],
                                      ALU.mult, ALU.mult, list(kqcB) + list(rshB) + [cB], kT0B)
                                P.stt("dve", kT1[64:128, tcs(tc)], kqc[64:128, :], prm[64:128, PC_GK:PC_GK + 1], rsh[64:128, :],
                                      ALU.mult, ALU.mult, list(kqcB) + list(rshB) + [cB], kT1B)
                            else:
                                P.stt("dve", qTh[:, tcs(tc)], kqc, small[:, 2:3], rsh,
                                      ALU.mult, ALU.mult, list(kqcB) + list(rshB) + [smB], qThB)
                        if fin_prev[0] is not None:
                            fin_prev[0]()
                        fin_prev[0] = fin
                for tg in range(4):
                    pt, ptB = rot_p.next()
                    for t in range(4):
                        tok0 = (4 * tg + t) * 128
                        for kt in range(KT):
                            c0 = 1024 + kt * 128
                            P.matmul(pt[:, t * 128:(t + 1) * 128], hT[:, kt, tok0:tok0 + 128], RW(sw_, c0, c0 + 128),
                                     kt == 0, kt == KT - 1, (bw_, hB[kt][tg]), (ptB,))
                    P.copy("dve" if tg % 2 else "act", Vh[:, tg * 512:(tg + 1) * 512], pt[:], (ptB,), VhB)
                    if tg == 0:
                        fin_prev[0]()
                W.release()
                LA = 3
                pending = []
                for qc in range(NTC):
                    nk = 4 * qc + 4
                    seq = [(c, ki) for ki in range(nk) for c in (0, 1)]
                    ets = {}
                    acc = [(ps[4], psB[4], ps[6], psB[6]), (ps[5], psB[5], ps[7], psB[7])]
                    for step in range(len(seq) + LA):
                        if step < len(seq):
                            c, ki = seq[step]
                            kTc, kTcB = (kT0, kT0B) if c == 0 else (kT1, kT1B)
                            d = ki - 4 * qc
                            q0 = max(d, 0) * 128
                            pst, pstB = rot_s.next()
                            P.matmul(pst[:, q0:512], kTc[:, ki * 128:(ki + 1) * 128], qTh[:, qc * 512 + q0:(qc + 1) * 512],
                                     True, d < 0, list(kTcB) + list(qThB), (pstB,))
                            if d >= 0:
                                P.matmul(pst[:, q0:512], ident_bf[:], masks[:, d, q0:512], False, True, (cB,), (pstB,))
                            et, etB = scr(16 + (estep[0] % 8), 1, BF16)
                            estep[0] += 1
                            P.act(et[:, q0:512], pst[:, q0:512], AF.Exp, (pstB,), etB)
                            ets[step] = (et, etB, c, ki, q0)
                            if pending and step >= 1:
                                pending.pop(0)()
                        if step >= LA:
                            et, etB, c, ki, q0 = ets[step - LA]
                            po, poB, pz, pzB = acc[c]
                            P.matmul(po[:, q0:512], Vh3[:, ki, :], et[:, q0:512], ki == 0, ki == nk - 1,
                                     list(VhB) + list(etB), (poB,))
                            P.matmul(pz[:, q0:512], ones_bf[:], et[:, q0:512], ki == 0, ki == nk - 1,
                                     list(etB) + [cB], (pzB,))
                    o_aps = []
                    rzs = []
                    for c in range(2):
                        po, poB, pz, pzB = acc[c]
                        rz, rzB = scr(30 + 2 * c, 2)
                        oc, ocB = scr(36 + 2 * c, 2)
                        P.act(rz, pz[:], AF.Ln, (pzB,), rzB)
                        P.copy("dve", oc, po[:], (poB,), ocB)
                        o_aps.append((oc, ocB))
                        rzs.append((rz, rzB))
                    def tail(h=h, qc=qc, rzs=rzs, o_aps=o_aps):
                        (o0, o0B), (o1, o1B) = o_aps
                        osq, osqB = scr(24, 1, BF16)
                        oln, olnB = scr(26, 2)
                        ors, orsB = scr(28, 2)
                        st = {}

                        def stat_mm():
                            st["p"] = rot_s.next()
                            P.matmul(st["p"][0][:], ones_bf[:], osq, True, True, list(osqB) + [cB], (st["p"][1],))
                        ops_ = []
                        for c in range(2):
                            rz, rzB = rzs[c]
                            oc, ocB = o_aps[c]
                            ops_.append(lambda rz=rz, rzB=rzB: P.act(rz, rz, AF.Exp, rzB, rzB, scale=-1.0))
                            ops_.append(lambda oc=oc, ocB=ocB, rz=rz, rzB=rzB: P.tt("dve", oc, oc, rz, ALU.mult, list(ocB) + list(rzB), ocB))
                        ops_.append(lambda: P.stt("dve", o0, o1, small[:, 1:2], o0, ALU.mult, ALU.add,
                                                  list(o0B) + list(o1B) + [smB], o0B))
                        ops_.append(lambda: P.tt("dve", osq, o0, o0, ALU.mult, o0B, osqB))
                        ops_.append(stat_mm)
                        ops_.append(lambda: P.act(oln, st["p"][0][:], AF.Ln, (st["p"][1], smB), olnB, bias=small[:, 0:1], scale=1.0 / 128))
                        ops_.append(lambda: P.act(ors, oln, AF.Exp, olnB, orsB, scale=-0.5))
                        ops_.append(lambda: P.stt("dve", AB[:, h, tcs(qc)], o0, small[:, 3:4], ors, ALU.mult, ALU.mult,
                                                  list(o0B) + list(orsB) + [smB], (aB[h][qc],)))
                        return ops_
                    pending.extend(tail())
                while pending:
                    pending.pop(0)()
            rot = Rot([0, 1, 2, 3, 4, 5, 6, 7])
            for half in range(2):
                so_, bo_ = W.get(wcolblock(wo_d, half * 512, 512))
                linear_fm(AB, aB, KT, so_, 0, 512, bo_, range(4), rot,
                          lambda m, tc, pt, ptB, half=half: add_residual(4 * half + m, tc, pt, ptB))
                W.release()

        if stage >= 5:
            rtr_ap, rtr_bufs = scr(24, 8)
            rtr = rtr_ap.rearrange("p (a b) -> p a b", a=16)
            rtB = rtr_bufs[0]

            def R(i):
                return rtr[:, i, :]

            def R3(i):
                return rtr[:, i, :].rearrange("p (t e) -> p t e", t=NT)
            rstd_tok = rtr[:, 6, 0:16]
            m1 = rtr[:, 6, 16:32]
            m2 = rtr[:, 6, 32:48]
            den = rtr[:, 6, 48:64]
            rden = rtr[:, 6, 64:80]
            P1p = rtr[:, 6, 80:96]
            P2 = rtr[:, 6, 96:112]
            sp = rtr[:, 6, 112:128]
            gw = rtr[:, 7, 0:KT * NE].rearrange("p (k e) -> p k e", k=KT)
            ne_ = rtr[:, 7, 64:72]
            ntl = rtr[:, 7, 72:80]
            tb = rtr[:, 7, 80:88]
            base = rtr[:, 7, 88:96]
            P1 = rtr[:, 7, 96:112]
            G1 = rtr[:, 11, 0:16]
            G2 = rtr[:, 11, 16:32]
            etf = rtr[:, 11, 32:48]
            idx1 = rtr[:, 12, 0:16].bitcast(mybir.dt.int32)
            idx2 = rtr[:, 12, 16:32].bitcast(mybir.dt.int32)
            eid = rtr[:, 12, 32:48].bitcast(mybir.dt.int32)
            selb = rtr[:, 13, 0:64].bitcast(BF16)
            P.memset("dve", rtr_ap, 0.0, rtr_bufs)

            htok = hT[:].rearrange("p k t -> p (k t)").rearrange("p (a b) -> p a b", a=NT)

            def htokB(tt):
                return [hB[tt // 2][2 * (tt % 2)], hB[tt // 2][2 * (tt % 2) + 1]]
            hnc, hncB = scr(32, 8, BF16)
            hnc3 = hnc.rearrange("p (k t) -> p k t", k=KT)
            rot_t = Rot([0, 1, 2])

            def post_rs(tc, rs, rsB):
                pt, ptB = ps[3], psB[3]
                for t in range(4):
                    P.transpose(pt[:, t * 128:(t + 1) * 128], rs[:, t * 128:(t + 1) * 128], ident[:], list(rsB) + [cB], (ptB,))
                P.copy("dve", rstd_tok[:, 4 * tc:4 * tc + 4], pt[:].rearrange("p (t c) -> p t c", c=128)[:, :, 0], (ptB,), (rtB,))

            def post_tc(tc):
                for t in range(4):
                    tt = 4 * tc + t
                    pt, ptB = rot_t.next()
                    ptb = pt[:].bitcast(BF16)
                    for kt in range(KT):
                        P.transpose(ptb[:, kt * 128:(kt + 1) * 128], hnc3[:, kt, t * 128:(t + 1) * 128], ident_bf[:],
                                    list(hncB) + [cB], (ptB,))
                    P.copy("act" if t % 2 else "dve", htok[:, tt, :], ptb, (ptB,), htokB(tt))
            rmsnorm(PC_LNFFN1, post_rs, dst=lambda kt, tc: (hnc3[:, kt, :], hncB), post_tc=post_tc)

            P.tt("dve", gw, rw[:], prm[:, PC_LNFFN1:PC_LNFFN1 + KT].unsqueeze(2).broadcast_to([128, KT, NE]), ALU.mult,
                 (cB, rtB), (rtB,))
            pl, plB = ps[0], psB[0]
            for t in range(NT):
                for kt in range(KT):
                    P.matmul(pl[:, t * NE:(t + 1) * NE], xT[:, kt, t * 128:(t + 1) * 128], gw[:, kt, :], kt == 0, kt == KT - 1,
                             (xB[kt][t // 4], rtB), (plB,))
            bc = lambda v: v.unsqueeze(2).broadcast_to([128, NT, NE])
            DV = lambda *a: P.tt("dve", *a, (rtB,), (rtB,))
            P.tt("dve", R3(0), pl[:, 0:NT * NE].rearrange("p (t e) -> p t e", t=NT), bc(rstd_tok), ALU.mult, (plB, rtB), (rtB,))
            def first_one(src, ta, tb_):
                cur = src
                for sh, dst in ((1, ta), (2, tb_), (4, ta)):
                    P.copy("dve", R3(dst)[:, :, 0:sh], R3(cur)[:, :, 0:sh], (rtB,), (rtB,))
                    DV(R3(dst)[:, :, sh:NE], R3(cur)[:, :, sh:NE], R3(cur)[:, :, 0:NE - sh], ALU.add)
                    cur = dst
                P.ts("dve", R(tb_), R(cur), 1.0, None, ALU.is_equal, None, (rtB,), (rtB,))
                DV(R(src), R(src), R(tb_), ALU.mult)
            P.reduce(m1, R3(0), ALU.max, (rtB,), (rtB,))
            DV(R3(1), R3(0), bc(m1), ALU.is_equal)
            first_one(1, 8, 9)
            P.stt("dve", R(2), R(1), -1e30, R(0), ALU.mult, ALU.add, (rtB,), (rtB,))
            P.reduce(m2, R3(2), ALU.max, (rtB,), (rtB,))
            DV(R3(3), R3(2), bc(m2), ALU.is_equal)
            first_one(3, 8, 9)
            DV(R(3), R(3), R(1), ALU.add)
            DV(R3(4), R3(0), bc(m1), ALU.subtract)
            P.act(R(4), R(4), AF.Exp, (rtB,), (rtB,))
            DV(R(4), R(4), R(3), ALU.mult)
            P.reduce(den, R3(4), ALU.add, (rtB,), (rtB,))
            P.recip(rden, den, (rtB,), (rtB,))
            DV(R3(5), R3(4), bc(rden), ALU.mult)
            P.copy("dve", selb, R(3), (rtB,), (rtB,))
            pa, paB = ps[1], psB[1]
            pb, pbB = ps[2], psB[2]
            P.matmul(pa[:, 0:128], tri_bf[:], selb, True, True, (rtB, cB), (paB,))
            P.matmul(pb[:, 0:128], ones_bf[:], selb, True, True, (rtB, cB), (pbB,))
            P.copy("dve", R(8), pa[:, 0:128], (paB,), (rtB,))
            P.copy("dve", R(9), pb[:, 0:128], (pbB,), (rtB,))
            src_, dst_ = 9, 10
            for sh in (1, 2, 4, 8):
                P.copy("dve", R3(dst_)[:, 0:sh, :], R3(src_)[:, 0:sh, :], (rtB,), (rtB,))
                DV(R3(dst_)[:, sh:NT, :], R3(src_)[:, sh:NT, :], R3(src_)[:, 0:NT - sh, :], ALU.add)
                src_, dst_ = dst_, (14 if dst_ == 10 else 10)
            P.copy("dve", ne_, R3(src_)[:, NT - 1, :], (rtB,), (rtB,))
            if src_ != 10:
                DV(R(10), R(src_), R(9), ALU.subtract)
            else:
                DV(R(14), R(10), R(9), ALU.subtract)
                P.copy("dve", R(10), R(14), (rtB,), (rtB,))
            P.ts("dve", ntl, ne_, 0.0, None, ALU.is_gt, None, (rtB,), (rtB,))
            for thr in (512.0, 1024.0, 1536.0):
                P.stt("dve", ntl, ne_, thr, ntl, ALU.is_gt, ALU.add, (rtB,), (rtB,))
            P.memset("dve", tb[:, 0:1], 0.0, (rtB,))
            for e in range(1, NE):
                DV(tb[:, e:e + 1], tb[:, e - 1:e], ntl[:, e - 1:e], ALU.add)
            P.ts("dve", base, tb, 512.0, None, ALU.mult, None, (rtB,), (rtB,))
            DV(R(8), R(8), R(10), ALU.add)
            DV(R3(8), R3(8), base.unsqueeze(1).broadcast_to([128, NT, NE]), ALU.add)
            P.stt("dve", R(2), R(8), 1.0, R(3), ALU.add, ALU.mult, (rtB,), (rtB,))
            P.reduce(P1p, R3(2), ALU.max, (rtB,), (rtB,))
            P.reduce(sp, R3(2), ALU.add, (rtB,), (rtB,))
            DV(R3(1), R3(2), bc(P1p), ALU.is_equal)
            DV(R(1), R(1), R(5), ALU.mult)
            P.reduce(G1, R3(1), ALU.add, (rtB,), (rtB,))
            P.ts("dve", G2, G1, -1.0, 1.0, ALU.mult, ALU.add, (rtB,), (rtB,))
            P.ts("dve", P1, P1p, -1.0, None, ALU.add, None, (rtB,), (rtB,))
            P.stt("dve", P2, sp, -1.0, P1p, ALU.add, ALU.subtract, (rtB,), (rtB,))
            P.copy("dve", idx1, P1, (rtB,), (rtB,))
            P.copy("dve", idx2, P2, (rtB,), (rtB,))
            DV(R3(15), prm[:, PC_IOTA:PC_IOTA + 16].unsqueeze(2).broadcast_to([128, NT, NE]),
               tb.unsqueeze(1).broadcast_to([128, NT, NE]), ALU.is_ge)
            P.reduce(etf, R3(15), ALU.add, (rtB, cB), (rtB,))
            P.ts("dve", etf, etf, -1.0, None, ALU.add, None, (rtB,), (rtB,))
            P.copy("dve", eid, etf, (rtB,), (rtB,))
            nused = rtr[:, 11, 48:49]
            flagf = rtr[:, 11, 64:80]
            flags = rtr[:, 12, 48:64].bitcast(mybir.dt.int32)
            P.reduce(nused, ntl, ALU.add, (rtB,), (rtB,))
            P.ts("dve", flagf, prm[:, PC_IOTA:PC_IOTA + 16], nused, None, ALU.is_lt, None, (rtB, cB), (rtB,))
            if not P.plan:
                P.flag_op = len(P.ops)
                P.flag_ap = flags
            P.copy("dve", flags, flagf, (rtB,), (rtB,))
            g_ = nc.gpsimd
            for tt in range(NT):
                for ix in (idx1, idx2):
                    P.op("pool", (lambda ix=ix, tt=tt: g_.indirect_dma_start(
                        out=HG[:, :], out_offset=bass.IndirectOffsetOnAxis(ap=ix[:, tt:tt + 1], axis=0),
                        in_=htok[:, tt, :], in_offset=None)), list(htokB(tt)) + [rtB],
                        (hgB[(2 * tt + (0 if ix is idx1 else 1)) % 16],), dma=True)

            W.go()
            tcnt = [0]
            yacc = hT[:, 0:4, :].rearrange("p k t -> p (k t)").bitcast(F32).rearrange("p (s f) -> p s f", s=4)
            yaccB = lambda s_, fh: [hB[s_][2 * fh], hB[s_][2 * fh + 1]]
            hgs_l, hgsB_l, hgT_l, hgTB_l = [], [], [], []
            hgs_l.append(hT[:, 4:6, :].rearrange("p k t -> p (k t)").rearrange("p (s f) -> p s f", s=4))
            hgsB_l.append([hB[k][t] for k in (4, 5) for t in range(NTC)])
            hgT_l.append(hT[:, 6:8, :].rearrange("p k t -> p (k t)").rearrange("p (k s) -> p k s", k=KT))
            hgTB_l.append(lambda kt: [hB[6 + kt // 4][kt % 4]])
            a_, b_ = scr(8, 8, BF16)
            hgs_l.append(a_.rearrange("p (s f) -> p s f", s=4))
            hgsB_l.append(list(b_))
            a_, b2_ = scr(0, 8, BF16)
            hgT_l.append(a_.rearrange("p (k s) -> p k s", k=KT))
            hgTB_l.append(lambda kt, b2_=b2_: [b2_[kt]])
            actT = lambda a: ABf[:, a * 512:(a + 1) * 512]
            actB = lambda a: aB[a // 4][a % 4]
            rot_gu = Rot([0, 1, 2, 3])
            rot_dn = Rot([4, 5, 6, 7])

            def dyn_part(t, kind, gi):
                re_, rgu_, rdn_ = regs[t % 2]
                if kind == "d":
                    const = gi * 512 * D
                    pat = [[D, 128], [128 * D, 4], [1, D]]
                    th, rb, split = mdn_t, rdn_, 4
                else:
                    const = gi * 512 + (DFE if kind == "u" else 0)
                    pat = [[2 * DFE, 128], [128 * 2 * DFE, KT], [1, 512]]
                    th, rb, split = mgu_t, rgu_, KT
                rt_ = tmpr[tcnt[0] % 4]
                tcnt[0] += 1
                src = (lambda th=th, rt_=rt_, pat=pat: bass.AP(th, rt_, pat))
                return (0, SLOT, split, src, (lambda rt_=rt_, rb=rb, const=const: (g_.reg_add(rt_, rb, const), rt_)[1]))

            def tile_pre(t):
                re_, rgu_, rdn_ = regs[t % 2]

                def fn():
                    g_.reg_load(re_, eid[0:1, t:t + 1])
                    g_.reg_mul(rgu_, re_, D * 2 * DFE)
                    return g_.reg_mul(rdn_, re_, DFE * D)
                return (fn, (rtB,))

            def prep_tile(t):
                par = t % 2
                hgs, hgsB, hgT, hgTB = hgs_l[par], hgsB_l[par], hgT_l[par], hgTB_l[par]
                P.dma("sp", hgs, HG[t * 512:(t + 1) * 512, :].rearrange("(s p) d -> p s d", p=128), hgB, hgsB)
                for kp in range(4):
                    pt, ptB = rot_dn.next()
                    ptb = pt[:].bitcast(BF16)
                    for k2 in range(2):
                        kt = 2 * kp + k2
                        for s_ in range(4):
                            P.transpose(ptb[:, k2 * 512 + s_ * 128:k2 * 512 + (s_ + 1) * 128], hgs[:, s_, kt * 128:(kt + 1) * 128],
                                        ident_bf[:], list(hgsB) + [cB], (ptB,))
                    P.copy("act" if kp % 2 else "dve", hgT[:, 2 * kp:2 * kp + 2, :].rearrange("p k s -> p (k s)"), ptb,
                           (ptB,), hgTB(2 * kp) + hgTB(2 * kp + 1))

            ctag = lambda t: (t if t >= 8 else None)
            prep_tile(0)
            for t in range(NTILE):
                P.cond = ctag(t)
                hgT, hgTB = hgT_l[t % 2], hgTB_l[t % 2]
                for gi in range(7):
                    sg_, bg_ = W.get({"parts": [dyn_part(t, "g", gi)], "pre": tile_pre(t) if gi == 0 else None, "hold": True, "cond": ctag(t)})
                    su_, bu_ = W.get({"parts": [dyn_part(t, "u", gi)], "pre": None, "hold": True, "cond": ctag(t)})
                    sd_, bd_ = W.get({"parts": [dyn_part(t, "d", gi)], "pre": None, "hold": True, "cond": ctag(t)})
                    abase = 4 * (gi % 2)
                    for jj in range(4):
                        a = abase + jj
                        pg, pgB = rot_gu.next()
                        pu, puB = rot_gu.next()
                        for (pt, ptB, slot, wb) in ((pg, pgB, sg_, bg_), (pu, puB, su_, bu_)):
                            for kt in range(KT):
                                cc = kt * 512 + jj * 128
                                P.matmul(pt[:], RW(slot, cc, cc + 128), hgT[:, kt, :], kt == 0, kt == KT - 1,
                                         [wb] + hgTB(kt), (ptB,))
                        sl, slB = scr(32 + 2 * (jj % 2), 2)
                        P.act(sl, pg[:], AF.Silu, (pgB,), slB)
                        P.tt("dve", actT(a), sl, pu[:], ALU.mult, list(slB) + [puB], (actB(a),))
                    W.release(); W.release()
                    if gi == 3 and t + 1 < NTILE:
                        P.cond = ctag(t + 1)
                        prep_tile(t + 1)
                        P.cond = ctag(t)
                    for s_ in range(4):
                        for fh in range(2):
                            pt, ptB = rot_dn.next()
                            for jj in range(4):
                                cc = jj * D + fh * 512
                                P.matmul(pt[:], actT(abase + jj)[:, s_ * 128:(s_ + 1) * 128], RW(sd_, cc, cc + 512),
                                         jj == 0, jj == 3, (bd_, actB(abase + jj)), (ptB,))
                            ya = yacc[:, s_, fh * 512:(fh + 1) * 512]
                            if gi == 0:
                                P.copy("act", ya, pt[:], (ptB,), yaccB(s_, fh))
                            else:
                                P.tt("dve", ya, ya, pt[:], ALU.add, [ptB] + yaccB(s_, fh), yaccB(s_, fh))
                    W.release()
                P.dma("sp", YD[t * 512:(t + 1) * 512, :].rearrange("(s p) f -> p s f", p=128), yacc,
                      [b for s_ in range(4) for fh in range(2) for b in yaccB(s_, fh)], (ydB,))
            P.cond = None

            rot = Rot([0, 1, 2, 3])
            for t in range(NT):
                o3 = (0, 8, 16, 32)[t % 4]
                b1, b1B = scr(o3, 4)
                b2, b2B = scr(o3 + 4, 4)
                for (bb_, bbB_, ix) in ((b1, b1B, idx1), (b2, b2B, idx2)):
                    P.op("pool", (lambda bb_=bb_, ix=ix, t=t: g_.indirect_dma_start(
                        out=bb_, out_offset=None, in_=YD[:, :],
                        in_offset=bass.IndirectOffsetOnAxis(ap=ix[:, t:t + 1], axis=0))), (ydB, rtB), bbB_, dma=True)
                for hf in range(2):
                    pt, ptB = rot.next()
                    for k4 in range(4):
                        kt = 4 * hf + k4
                        P.transpose(pt[:, k4 * 128:(k4 + 1) * 128], xT[:, kt, t * 128:(t + 1) * 128], ident[:],
                                    (xB[kt][t // 4], cB), (ptB,))
                    hs = slice(hf * 512, (hf + 1) * 512)
                    P.stt("dve", b1[:, hs], b1[:, hs], G1[:, t:t + 1], pt[:], ALU.mult, ALU.add,
                          list(b1B) + [ptB, rtB], b1B)
                    P.stt("dve", b1[:, hs], b2[:, hs], G2[:, t:t + 1], b1[:, hs], ALU.mult, ALU.add,
                          list(b2B) + list(b1B) + [rtB], b1B)
                P.dma("sp", out_d[t * 128:(t + 1) * 128, :], b1, b1B, ())
        else:
            rot = Rot([0, 1, 2, 3])
            for t in range(NT):
                ob, obB = scr(4 * (t % 4), 4)
                for hf in range(2):
                    pt, ptB = rot.next()
                    for k4 in range(4):
                        kt = 4 * hf + k4
                        P.transpose(pt[:, k4 * 128:(k4 + 1) * 128], xT[:, kt, t * 128:(t + 1) * 128], ident[:],
                                    (xB[kt][t // 4], cB), (ptB,))
                    P.copy("act" if hf else "dve", ob[:, hf * 512:(hf + 1) * 512], pt[:], (ptB,), obB)
                P.dma("sp", out_d[t * 128:(t + 1) * 128, :], ob, obB, ())

    P.plan = True
    Wp = WStream(P, RW, wB, None)
    body(P, Wp)
    P.plan = False
    first_extra = {NS + j: [aB[2 + 2 * j + k][t] for k in range(2) for t in range(NTC)] for j in range(3)}
    W = WStream(P, RW, wB, Wp.blocks, first_extra)
    body(P, W)
    sems = []

    def sem_alloc(name):
        s = nc.alloc_semaphore(name)
        sems.append(s)
        return s
    n = P.finalize(sem_alloc)
    return nc, n


def host_inputs(inp):
    f = lambda a: np.ascontiguousarray(np.asarray(a, dtype=np.float32))
    prm = np.zeros((128, NPC), np.float32)

    def cols(v):
        return np.asarray(v, np.float32).reshape(KT, 128).T
    prm[:, PC_LNMIX0:PC_LNMIX0 + 8] = cols(inp["ln_mix"][0])
    prm[:, PC_LNFFN0:PC_LNFFN0 + 8] = cols(inp["ln_ffn"][0])
    prm[:, PC_LNKV:PC_LNKV + 8] = cols(inp["ln_kv"])
    prm[:, PC_LNMIX1:PC_LNMIX1 + 8] = cols(inp["ln_mix"][1])
    prm[:, PC_LNFFN1:PC_LNFFN1 + 8] = cols(inp["ln_ffn"][1])
    for j in range(3):
        prm[:, PC_CW + 8 * j:PC_CW + 8 * j + 8] = cols(inp["conv_w"][0][j])
    prm[:, PC_GK] = np.tile(np.asarray(inp["k_norm"], np.float32), 2)
    prm[:, PC_GQ] = np.tile(np.asarray(inp["q_norm"][0], np.float32), 2)
    prm[:, PC_GS] = np.asarray(inp["sub_norm"][0], np.float32)
    prm[:, PC_IOTA:PC_IOTA + 16] = np.arange(16, dtype=np.float32)[None, :]
    ident = np.eye(128, dtype=np.float32)
    k = np.arange(128)[:, None]
    q = np.arange(512)[None, :]
    masks = np.concatenate([(q >= d * 128 + k).astype(np.float32) for d in range(4)], axis=1)
    shared = {
        "params": prm,
        "lam": f(inp["lam_params"]).reshape(1, 256),
        "conv_w_in": f(inp["conv_w_in"][0]),
        "conv_w_out": f(inp["conv_w_out"][0]),
        "w_kv": f(inp["w_kv"]),
        "attn_w_q": f(inp["attn_w_q"][0]),
        "attn_w_o": f(inp["attn_w_o"][0]),
        "ffn_w_gu": f(inp["ffn_w_gu"][0]),
        "ffn_w_down": f(inp["ffn_w_down"][0]),
        "router_w": f(inp["router_w"][0]),
        "moe_w_gu": f(inp["moe_w_gu"][0]),
        "moe_w_down": f(inp["moe_w_down"][0]),
        "ident": ident,
        "tri": np.triu(np.ones((128, 128), np.float32), 1),
        "masks": np.ascontiguousarray(masks),
    }
    x = f(inp["x"])
    return [dict(shared, x=np.ascontiguousarray(x[b])) for b in range(8)]


_CACHE = {}


def kernel(**inputs):
    if "nc" not in _CACHE:
        _CACHE["nc"] = build()[0]
    nc = _CACHE["nc"]
    in_maps = host_inputs(inputs)
    res = run_bass_kernel_spmd(nc, in_maps, core_ids=list(range(8)))
    return np.stack([np.asarray(r["out"], dtype=np.float32) for r in res.results], axis=0)
```

```python
import math
import re
import numpy as np
import concourse.bass as bass
import concourse.mybir as mybir
from concourse.bass_utils import run_bass_kernel_spmd

F32 = mybir.dt.float32
BF16 = mybir.dt.bfloat16
AF = mybir.ActivationFunctionType
ALU = mybir.AluOpType
AX = mybir.AxisListType

D = 1024
S = 2048
KT = 8
NTC = 4
NT = 16
DFF = 2816
NFF = 22
NE = 8
DFE = 3584
NFE = 28
EPS = 1e-6
LAM_INIT = 0.8 - 0.6 * math.exp(-0.3 * 1.0)
NS = 4
SLOT = 4096

PC_LNMIX0, PC_LNFFN0, PC_LNKV, PC_LNMIX1, PC_LNFFN1 = 0, 8, 16, 24, 32
PC_CW = 40
PC_GK, PC_GQ, PC_GS = 64, 65, 66
PC_IOTA = 67
NPC = 83
NTILE = 15
NSLOT = NTILE * 512


class Buf:
    __slots__ = ("name", "lw", "rd")

    def __init__(self, name):
        self.name = name
        self.lw = None
        self.rd = {}


class Prog:
    def __init__(self, nc):
        self.nc = nc
        self.ops = []
        self.plan = False
        self.cond = None
        self.flag_op = None
        self.flag_ap = None
        self.E = {"pe": nc.tensor, "act": nc.scalar, "dve": nc.vector,
                  "pool": nc.gpsimd, "sp": nc.sync}

    def op(self, eng, fn, r=(), w=(), dma=False):
        if self.plan:
            return
        self.ops.append([eng, fn, tuple(r), tuple(w), dma, None, False, 0, self.cond])

    def matmul(self, out, lhsT, rhs, start, stop, r, w):
        nc = self.nc
        self.op("pe", lambda: nc.tensor.matmul(out, lhsT, rhs, start=start, stop=stop), r, w)

    def transpose(self, out, in_, ident, r, w):
        nc = self.nc
        self.op("pe", lambda: nc.tensor.transpose(out, in_, ident), r, w)

    def act(self, out, in_, func, r, w, bias=None, scale=None):
        nc = self.nc
        kw = {}
        if bias is not None:
            kw["bias"] = bias
        if scale is not None:
            kw["scale"] = scale
        self.op("act", lambda: nc.scalar.activation(out=out, in_=in_, func=func, **kw), r, w)

    def copy(self, eng, out, in_, r, w):
        nc = self.nc
        if eng == "act":
            self.op("act", lambda: nc.scalar.copy(out=out, in_=in_), r, w)
        else:
            e = self.E[eng]
            self.op(eng, lambda: e.tensor_copy(out=out, in_=in_), r, w)

    def tt(self, eng, out, in0, in1, op, r, w):
        e = self.E[eng]
        self.op(eng, lambda: e.tensor_tensor(out=out, in0=in0, in1=in1, op=op), r, w)

    def ts(self, eng, out, in0, s1, s2, op0, op1, r, w):
        e = self.E[eng]
        if s2 is None:
            self.op(eng, lambda: e.tensor_scalar(out=out, in0=in0, scalar1=s1, scalar2=None, op0=op0), r, w)
        else:
            self.op(eng, lambda: e.tensor_scalar(out=out, in0=in0, scalar1=s1, scalar2=s2, op0=op0, op1=op1), r, w)

    def stt(self, eng, out, in0, scalar, in1, op0, op1, r, w):
        e = self.E[eng]
        self.op(eng, lambda: e.scalar_tensor_tensor(out=out, in0=in0, scalar=scalar, in1=in1, op0=op0, op1=op1), r, w)

    def recip(self, out, in_, r, w):
        nc = self.nc
        self.op("dve", lambda: nc.vector.reciprocal(out=out, in_=in_), r, w)

    def reduce(self, out, in_, op, r, w):
        nc = self.nc
        self.op("dve", lambda: nc.vector.tensor_reduce(out=out, in_=in_, axis=AX.X, op=op), r, w)

    def memset(self, eng, ap, val, w):
        e = self.E[eng]
        self.op(eng, lambda: e.memset(ap, val), (), w)

    def dma(self, q, out, in_, r, w):
        e = self.E[q]
        self.op(q, lambda: e.dma_start(out=out, in_=in_), r, w, dma=True)

    @staticmethod
    def _ckey(o):
        if o[4]:
            b = o[3][0] if o[3] else o[2][0]
            return ("dma", o[0], b.name)
        return o[0]

    def finalize(self, sem_alloc):
        ops = self.ops
        ck = self._ckey
        nc = self.nc
        for i, o in enumerate(ops):
            deps = {}

            def add(j):
                k = ck(ops[j])
                if deps.get(k, -1) < j:
                    deps[k] = j
            for b in o[2]:
                if b.lw is not None:
                    add(b.lw)
            for b in o[3]:
                if b.lw is not None:
                    add(b.lw)
                for j in b.rd.values():
                    add(j)
            if o[0] == "pe" and not o[4]:
                deps.pop("pe", None)
            o[5] = list(deps.values())
            for j in o[5]:
                ops[j][6] = True
            k = ck(o)
            for b in o[2]:
                b.rd[k] = i
            for b in o[3]:
                b.lw = i
                b.rd = {}
        if self.flag_op is not None:
            ops[self.flag_op][6] = True
        sems = {}
        cnt = {}
        per_eng = {}
        runs = {}
        for i, o in enumerate(ops):
            k = ck(o)
            eng = o[0]
            per_eng.setdefault(eng, []).append(i)
            rl = runs.setdefault(eng, [])
            if not rl or rl[-1]["tag"] != o[8]:
                rl.append({"tag": o[8], "incs": {}, "pre": {}, "first": i})
            run = rl[-1]
            o.append(len(rl) - 1)
            inc = 16 if o[4] else (1 if o[6] else 0)
            if inc:
                if k not in sems:
                    sems[k] = sem_alloc("s_" + "_".join(k) if isinstance(k, tuple) else "s_" + k)
                    cnt[k] = 0
                if k not in run["pre"]:
                    run["pre"][k] = cnt[k]
                run["incs"][k] = run["incs"].get(k, 0) + inc
                cnt[k] += inc
                o[7] = cnt[k]
        for eng, idxs in per_eng.items():
            e = self.E[eng]
            wd = {}
            cur_run = -1
            guard = None
            snap = None
            freg = None
            flag_waited = False

            def close_run(run):
                guard.__exit__(None, None, None)
                with e.Else():
                    for k2, amt in run["incs"].items():
                        if run["pre"][k2] > 0:
                            e.wait_ge(sems[k2], run["pre"][k2])
                        e.sem_inc(sems[k2], amt)
            for i in idxs:
                o = ops[i]
                if o[9] != cur_run:
                    if guard is not None:
                        close_run(runs[eng][cur_run])
                        guard = None
                        wd = snap
                    cur_run = o[9]
                    run = runs[eng][cur_run]
                    if run["tag"] is not None:
                        if freg is None:
                            freg = e.alloc_register("flag_" + eng)
                        if not flag_waited:
                            fo = ops[self.flag_op]
                            if eng != "dve":
                                e.wait_ge(sems[ck(fo)], fo[7])
                            else:
                                e.wait_ge(sems["dve"], fo[7])
                            flag_waited = True
                        e.reg_load(freg, self.flag_ap[0:1, run["tag"]:run["tag"] + 1])
                        snap = dict(wd)
                        guard = e.If(freg)
                        guard.__enter__()
                need = {}
                for j in o[5]:
                    d = ops[j]
                    k = ck(d)
                    if need.get(k, 0) < d[7]:
                        need[k] = d[7]
                for k, v in need.items():
                    if wd.get(k, 0) >= v:
                        continue
                    e.wait_ge(sems[k], v)
                    wd[k] = v
                ins = o[1]()
                k = ck(o)
                if o[4]:
                    ins.then_inc(sems[k], 16)
                elif o[6]:
                    ins.then_inc(sems[k], 1)
            if guard is not None:
                close_run(runs[eng][cur_run])
        for k, s in sems.items():
            if isinstance(k, tuple):
                nc.sync.wait_ge(s, cnt[k])
        return len(ops)


class WStream:
    def __init__(self, P, view, ring_bufs, blocks=None, first_extra=None):
        self.P = P
        self.view = view
        self.bufs = ring_bufs
        self.first_extra = dict(first_extra or {})
        self.ns = NS
        self.base = 0
        self.plan = blocks is None
        self.blocks = [] if blocks is None else blocks
        self.next_get = 0
        self.next_issue = 0
        self.n_released = 0
        self.unheld = False

    def _issue(self):
        i = self.next_issue
        s = (i - self.base) % self.ns
        blk = self.blocks[i]
        saved_cond = self.P.cond
        self.P.cond = blk.get("cond") if isinstance(blk, dict) else None
        if isinstance(blk, dict):
            if blk.get("pre") is not None:
                fn, reads = blk["pre"]
                self.P.op("pool", fn, reads, ())
            parts = blk["parts"]
        else:
            parts = blk
        for part in parts:
            off, n, split, src = part[:4]
            prefn = part[4] if len(part) > 4 else None
            dst = self.view(s, off, off + n).rearrange("p (a b) -> p a b", a=split)
            wbufs = (self.bufs[s],) + tuple(self.first_extra.pop(s, ()))
            if prefn is None:
                self.P.dma("pool", dst, src, (), wbufs)
            else:
                self.P.op("pool", (lambda prefn=prefn, dst=dst, src=src: self._dyn_dma(prefn, dst, src)),
                          (), wbufs, dma=True)
        self.P.cond = saved_cond
        self.next_issue += 1

    def _dyn_dma(self, prefn, dst, srcfn):
        g = self.P.nc.gpsimd
        rt = prefn()
        ins = g.dma_start(out=dst, in_=srcfn())
        m = re.search(r"R\[(Pool_tmp_(\d+))\]", str(ins.ins))
        if m:
            RH = type(rt)
            n = int(m.group(2))
            names = [m.group(1)] + [f"Pool_{rt.name}_snap_{n - k}" for k in range(1, 5)]
            for nm in names:
                try:
                    g.free_register(RH(nm, rt.engine))
                except ValueError:
                    pass
        return ins

    def _fill(self):
        while self.next_issue < len(self.blocks) and self.next_issue - self.n_released < self.ns:
            blk = self.blocks[self.next_issue]
            if isinstance(blk, dict) and blk.get("hold") and not self.unheld:
                return
            self._issue()

    def start(self):
        if self.plan:
            return
        self._fill()

    def go(self):
        if self.plan:
            self.go_at = self.next_get
            return
        assert self.next_issue == self.n_released == self.next_get, "ring must be drained when go() is called"
        self.unheld = True
        self.base = self.next_issue
        self.ns = len(self.bufs)
        self._fill()

    def get(self, parts):
        i = self.next_get
        self.next_get += 1
        if self.plan:
            self.blocks.append(parts)
            return 0, self.bufs[0]
        sl = (i - self.base) % self.ns
        return sl, self.bufs[sl]

    def release(self):
        if self.plan:
            return
        self.n_released += 1
        self._fill()


def build(stage=99):
    nc = bass.Bass("TRN2", target_bir_lowering=False)
    P = Prog(nc)

    def din(name, shape):
        return nc.dram_tensor(name, shape, F32, kind="ExternalInput").ap()

    x_d = din("x", [S, D])
    params_d = din("params", [128, NPC])
    lam_d = din("lam", [1, 256])
    win_d = din("conv_w_in", [D, 3 * D])
    wout_d = din("conv_w_out", [D, D])
    wkv_d = din("w_kv", [D, 2 * D])
    wq_d = din("attn_w_q", [D, D])
    wo_d = din("attn_w_o", [D, D])
    wgu_d = din("ffn_w_gu", [D, 2 * DFF])
    wdn_d = din("ffn_w_down", [DFF, D])
    rw_d = din("router_w", [D, NE])
    if stage >= 5:
        mgu_t = nc.dram_tensor("moe_w_gu", [NE, D, 2 * DFE], F32, kind="ExternalInput")
        mdn_t = nc.dram_tensor("moe_w_down", [NE, DFE, D], F32, kind="ExternalInput")
    else:
        mgu_t = nc.dram_tensor("moe_w_gu", [NE, 1, 1], F32, kind="ExternalInput")
        mdn_t = nc.dram_tensor("moe_w_down", [NE, 1, 1], F32, kind="ExternalInput")
    mgu_d = mgu_t.ap()
    mdn_d = mdn_t.ap()
    tri_d = din("tri", [128, 128])
    HG = nc.dram_tensor("hg_scratch", [NSLOT, D], BF16, kind="Internal").ap()
    YD = nc.dram_tensor("y_scratch", [NSLOT, D], F32, kind="Internal").ap()
    ident_d = din("ident", [128, 128])
    masks_d = din("masks", [128, 4 * 512])
    out_d = nc.dram_tensor("out", [S, D], F32, kind="ExternalOutput").ap()

    xT = nc.alloc_sbuf_tensor("xT", [128, KT, S], F32)
    hT = nc.alloc_sbuf_tensor("hT", [128, KT, S], BF16)
    AB = nc.alloc_sbuf_tensor("actbuf", [128, KT, S], BF16)
    ring = nc.alloc_sbuf_tensor("wring", [128, NS, SLOT], BF16)
    SCR = nc.alloc_sbuf_tensor("scr", [128, 10240], F32)
    prm = nc.alloc_sbuf_tensor("prm", [128, NPC], F32)
    ident = nc.alloc_sbuf_tensor("ident_sb", [128, 128], F32)
    masks = nc.alloc_sbuf_tensor("masks_sb", [128, 4, 512], BF16)
    ones_f = nc.alloc_sbuf_tensor("ones_f", [8, 128], F32)
    tri_bf = nc.alloc_sbuf_tensor("tri_bf", [128, 128], BF16)
    ident_bf = nc.alloc_sbuf_tensor("ident_bf", [128, 128], BF16)
    rw = nc.alloc_sbuf_tensor("rw_sb", [128, KT, NE], F32)
    ones_bf = nc.alloc_sbuf_tensor("ones_bf", [128, 128], BF16)
    blk_bf = nc.alloc_sbuf_tensor("blk_bf", [128, 128], BF16)
    small = nc.alloc_sbuf_tensor("small", [128, 64], F32)

    xB = [[Buf(f"x{k}_{t}") for t in range(NTC)] for k in range(KT)]
    hB = [[Buf(f"h{k}_{t}") for t in range(NTC)] for k in range(KT)]
    aB = [[Buf(f"a{k}_{t}") for t in range(NTC)] for k in range(KT)]
    wB = [Buf(f"w{s}") for s in range(NS + 3)]
    ABf = AB[:].rearrange("p k t -> p (k t)")

    def RW(sid, lo, hi):
        if sid < NS:
            return ring[:, sid, lo:hi]
        o = SLOT * (sid - NS + 1)
        return ABf[:, o + lo:o + hi]
    sB = [Buf(f"scr{i}") for i in range(40)]
    cB = Buf("consts")
    hgB = [Buf(f"hg_dram{i}") for i in range(16)]
    ydB = Buf("y_dram")
    smB = Buf("small")
    psB = [Buf(f"ps{i}") for i in range(8)]
    ps = [nc.alloc_psum_tensor(f"ps{i}", [128, 512], F32) for i in range(8)]

    def scr(kb_off, kb, dtype=F32):
        lo = int(kb_off * 256)
        hi = int((kb_off + kb) * 256)
        ap = SCR[:, lo:hi]
        if dtype == BF16:
            ap = ap.bitcast(BF16)
        b0 = int(math.floor(kb_off))
        b1 = int(math.ceil(kb_off + kb))
        return ap, sB[b0:b1]

    def tcs(t):
        return slice(t * 512, (t + 1) * 512)

    def pcol(c, n=1):
        return prm[:, c:c + n]

    class Rot:
        def __init__(self, idx):
            self.idx = idx
            self.i = 0

        def next(self):
            b = self.idx[self.i % len(self.idx)]
            self.i += 1
            return ps[b], psB[b]

    regs = {}
    for p_ in range(2):
        regs[p_] = (nc.gpsimd.alloc_register(f"re{p_}"), nc.gpsimd.alloc_register(f"rgu{p_}"),
                    nc.gpsimd.alloc_register(f"rdn{p_}"))
    tmpr = [nc.gpsimd.alloc_register(f"rtmp{i}") for i in range(4)]

    def body(P, W):
        P.dma("pool", prm[:], params_d[:], (), (cB,))
        P.dma("pool", ident[:], ident_d[:], (), (cB,))
        P.dma("pool", masks[:].rearrange("p a b -> p (a b)"), masks_d[:], (), (cB,))
        P.dma("pool", tri_bf[:], tri_d[:], (), (cB,))
        P.dma("pool", ident_bf[:], ident_d[:], (), (cB,))
        P.dma("pool", rw[:], rw_d.rearrange("(kt p) e -> p kt e", p=128), (), (cB,))
        W.start()
        P.memset("dve", ones_bf[:], 1.0, (cB,))
        P.memset("dve", blk_bf[:], 0.0, (cB,))
        P.memset("dve", blk_bf[0:64, 0:64], 1.0, (cB,))
        P.memset("dve", blk_bf[64:128, 64:128], 1.0, (cB,))
        P.memset("dve", ones_f[:], 1.0, (cB,))

        rot = Rot([0, 1, 2, 3])
        for g in range(NTC):
            stg, stgB = scr(16 * (g % 2), 16)
            stg3 = stg.rearrange("p (t d) -> p t d", t=4)
            P.dma("sp", stg3, x_d[g * 512:(g + 1) * 512, :].rearrange("(t p) d -> p t d", p=128), (), stgB)
            for kt in range(KT):
                pt, ptB = rot.next()
                for t in range(4):
                    P.transpose(pt[:, t * 128:(t + 1) * 128], stg3[:, t, kt * 128:(kt + 1) * 128], ident[:],
                                list(stgB) + [cB], (ptB,))
                P.copy("act" if kt % 2 else "dve", xT[:, kt, tcs(g)], pt[:], (ptB,), (xB[kt][g],))

        if stage >= 5:
            zsrc = ABf[:, 12288:16384].rearrange("p (s f) -> p s f", s=4)
            zB = [aB[k][t] for k in (6, 7) for t in range(NTC)]
            P.memset("dve", ABf[:, 12288:16384], 0.0, zB)
            for t in range(NTILE):
                P.dma("sp", HG[t * 512:(t + 1) * 512, :].rearrange("(s p) d -> p s d", p=128), zsrc, zB, (hgB[t % 16],))

        def rmsnorm(gcol, post_rs=None, dst=None, post_tc=None):
            banks = [4, 5, 6, 7]
            for tc in range(NTC):
                sq, sqB = scr(8 * (tc % 2), 8, BF16)
                sq3 = sq.rearrange("p (k t) -> p k t", k=KT)
                P.act(sq3, xT[:, :, tcs(tc)], AF.Square, [xB[k][tc] for k in range(KT)], sqB)
                for kt in range(KT):
                    P.matmul(ps[banks[tc]][:], ones_bf[:], sq3[:, kt, :], kt == 0, kt == KT - 1,
                             list(sqB) + [cB], (psB[banks[tc]],))
            for tc in range(NTC):
                sd, sdB = scr(16 + 4 * (tc % 2), 2)
                rs, rsB = scr(18 + 4 * (tc % 2), 2)
                P.act(sd, ps[banks[tc]][:], AF.Ln, (psB[banks[tc]], smB), sdB, bias=small[:, 0:1], scale=1.0 / D)
                P.act(rs, sd, AF.Exp, sdB, rsB, scale=-0.5)
                if post_rs is not None:
                    post_rs(tc, rs, rsB)
                for kt in range(KT):
                    if dst is None:
                        o_ap, o_b = hT[:, kt, tcs(tc)], (hB[kt][tc],)
                    else:
                        o_ap, o_b = dst(kt, tc)
                    if gcol is None:
                        P.tt("dve", o_ap, xT[:, kt, tcs(tc)], rs, ALU.mult,
                             [xB[kt][tc]] + list(rsB), o_b)
                    else:
                        P.stt("dve", o_ap, xT[:, kt, tcs(tc)], pcol(gcol + kt), rs,
                              ALU.mult, ALU.mult, [xB[kt][tc], cB] + list(rsB), o_b)
                if post_tc is not None:
                    post_tc(tc)

        P.memset("dve", small[:, 0:1], EPS, (smB,))

        def add_residual(m, tc, pt, ptB):
            P.tt("dve", xT[:, m, tcs(tc)], xT[:, m, tcs(tc)], pt[:], ALU.add, (xB[m][tc], ptB), (xB[m][tc],))

        def linear_fm(inp, inB, nk, wslot, woff, wcols, wbuf, m_list, rot, epilogue):
            for m in m_list:
                for tc in range(NTC):
                    pt, ptB = rot.next()
                    for k in range(nk):
                        c0 = woff + k * wcols + m * 128
                        P.matmul(pt[:], RW(wslot, c0, c0 + 128), inp[:, k, tcs(tc)], k == 0, k == nk - 1,
                                 (wbuf, inB[k][tc]), (ptB,))
                    epilogue(m, tc, pt, ptB)

        def wcolblock(src2d, c0, ncols):
            return [(0, KT * ncols, KT, src2d[:, c0:c0 + ncols].rearrange("(kt p) f -> p kt f", p=128))]

        def wrowblock(src2d, r0, nch):
            return [(0, nch * D, nch, src2d[r0:r0 + nch * 128, :].rearrange("(c p) f -> p c f", p=128))]

        if stage >= 2:
            rmsnorm(PC_LNMIX0)
            rot = Rot([0, 1, 2, 3, 4, 5])
            for G in range(2):
                sb_, bb_ = W.get(wcolblock(win_d, G * 512, 512))
                sc_, bc_ = W.get(wcolblock(win_d, D + G * 512, 512))
                sv_, bv_ = W.get(wcolblock(win_d, 2 * D + G * 512, 512))
                for jj in range(4):
                    j = 4 * G + jj
                    ub, ubB = scr(8 * (j % 2), 8)
                    bbuf, bbB = scr(16 + 8 * (j % 2), 8)
                    for tc in range(NTC):
                        pc, pcB = rot.next()
                        pv, pvB = rot.next()
                        pb, pbB = rot.next()
                        for (pt, ptB, slot, wb) in ((pc, pcB, sc_, bc_), (pv, pvB, sv_, bv_), (pb, pbB, sb_, bb_)):
                            for kt in range(KT):
                                c0 = kt * 512 + jj * 128
                                P.matmul(pt[:], RW(slot, c0, c0 + 128), hT[:, kt, tcs(tc)], kt == 0, kt == KT - 1,
                                         (wb, hB[kt][tc]), (ptB,))
                        csb, csbB = scr(32 + 2 * (tc % 2), 2)
                        z, zB = scr(36 + 2 * (tc % 2), 2)
                        P.copy("act", csb, pc[:], (pcB,), csbB)
                        P.tt("dve", ub[:, tcs(tc)], csb, pv[:], ALU.mult, list(csbB) + [pvB], ubB)
                        P.copy("act", bbuf[:, tcs(tc)], pb[:], (pbB,), bbB)
                        lo = tc * 512
                        P.ts("dve", z, ub[:, lo:lo + 512], pcol(PC_CW + 16 + j), None, ALU.mult, None,
                             list(ubB) + [cB], zB)
                        for sh, tap in ((1, 1), (2, 0)):
                            a = sh if tc == 0 else 0
                            P.stt("dve", z[:, a:512], ub[:, lo + a - sh:lo + 512 - sh], pcol(PC_CW + 8 * tap + j),
                                  z[:, a:512], ALU.mult, ALU.add, list(ubB) + list(zB) + [cB], zB)
                        P.tt("dve", AB[:, j, tcs(tc)], bbuf[:, tcs(tc)], z, ALU.mult, list(bbB) + list(zB), (aB[j][tc],))
                W.release(); W.release(); W.release()
            rot = Rot([0, 1, 2, 3, 4, 5, 6, 7])
            for half in range(2):
                so_, bo_ = W.get(wcolblock(wout_d, half * 512, 512))
                linear_fm(AB, aB, KT, so_, 0, 512, bo_, range(4), rot,
                          lambda m, tc, pt, ptB, half=half: add_residual(4 * half + m, tc, pt, ptB))
                W.release()

        def ffn(gu_d, dn_d, nchunks, dff, cT=None):
            rot_gu = Rot([0, 1, 2, 3])
            rot_dn = Rot([4, 5, 6, 7])
            ngrp = (nchunks + 3) // 4
            for gi in range(ngrp):
                c0 = gi * 4
                nch = min(4, nchunks - c0)
                sg_, bg_ = W.get(wcolblock(gu_d, c0 * 128, nch * 128))
                su_, bu_ = W.get(wcolblock(gu_d, dff + c0 * 128, nch * 128))
                sd_, bd_ = W.get(wrowblock(dn_d, c0 * 128, nch))
                base = 4 * (gi % 2)
                for jj in range(nch):
                    a = base + jj
                    for tc in range(NTC):
                        pg, pgB = rot_gu.next()
                        pu, puB = rot_gu.next()
                        for (pt, ptB, slot, wb) in ((pg, pgB, sg_, bg_), (pu, puB, su_, bu_)):
                            for kt in range(KT):
                                cc = kt * nch * 128 + jj * 128
                                P.matmul(pt[:], RW(slot, cc, cc + 128), hT[:, kt, tcs(tc)], kt == 0, kt == KT - 1,
                                         (wb, hB[kt][tc]), (ptB,))
                        sl, slB = scr(32 + 2 * (tc % 2), 2)
                        P.act(sl, pg[:], AF.Silu, (pgB,), slB)
                        if cT is not None:
                            P.tt("dve", sl, sl, cT[0][:, tcs(tc)], ALU.mult, list(slB) + list(cT[1]), slB)
                        P.tt("dve", AB[:, a, tcs(tc)], sl, pu[:], ALU.mult, list(slB) + [puB], (aB[a][tc],))
                W.release(); W.release()
                for m in range(KT):
                    for tc in range(NTC):
                        pt, ptB = rot_dn.next()
                        for jj in range(nch):
                            cc = jj * D + m * 128
                            P.matmul(pt[:], RW(sd_, cc, cc + 128), AB[:, base + jj, tcs(tc)], jj == 0, jj == nch - 1,
                                     (bd_, aB[base + jj][tc]), (ptB,))
                        add_residual(m, tc, pt, ptB)
                W.release()

        if stage >= 3:
            rmsnorm(PC_LNFFN0)
            if stage >= 5:
                zy, zyB = scr(36, 4)
                P.memset("dve", zy, 0.0, zyB)
                for t in range(8, NTILE):
                    for q4 in range(4):
                        P.dma("sp", YD[t * 512 + q4 * 128:t * 512 + (q4 + 1) * 128, :], zy, zyB, (ydB,))
            ffn(wgu_d, wdn_d, NFF, DFF)

        if stage >= 4:
            rmsnorm(None)
            tmp, tmpB = scr(0, 1)
            lamp, lampB = scr(1, 1)
            P.dma("sp", lamp, lam_d.broadcast_to([128, 256]), (), lampB)
            P.tt("dve", tmp[:, 0:64], lamp[:, 0:64], lamp[:, 64:128], ALU.mult, lampB, tmpB)
            P.reduce(small[:, 8:9], tmp[:, 0:64], ALU.add, tmpB, (smB,))
            P.tt("dve", tmp[:, 64:128], lamp[:, 128:192], lamp[:, 192:256], ALU.mult, lampB, tmpB)
            P.reduce(small[:, 9:10], tmp[:, 64:128], ALU.add, tmpB, (smB,))
            P.act(small[:, 10:12], small[:, 8:10], AF.Exp, (smB,), (smB,))
            P.stt("dve", small[:, 1:2], small[:, 11:12], -LAM_INIT, small[:, 10:11], ALU.add, ALU.subtract, (smB,), (smB,))
            P.ts("dve", small[:, 2:3], pcol(PC_GQ), 0.125, None, ALU.mult, None, (cB,), (smB,))
            P.ts("dve", small[:, 3:4], pcol(PC_GS), 1.0 - LAM_INIT, None, ALU.mult, None, (cB,), (smB,))

            kT0, kT0B = scr(0, 4, BF16)
            kT1, kT1B = scr(4, 4, BF16)
            qTh, qThB = scr(8, 4, BF16)
            Vh, VhB = scr(12, 4, BF16)
            Vh3 = Vh.rearrange("p (t e) -> p t e", t=NT)
            P.ts("dve", masks[:].rearrange("p a b -> p (a b)"), masks[:].rearrange("p a b -> p (a b)"), 30000.0, -30000.0,
                 ALU.mult, ALU.add, (cB,), (cB,))
            P.memset("dve", kT0[64:128, :], 0.0, kT0B)
            P.memset("dve", kT1[0:64, :], 0.0, kT1B)
            rot_s = Rot([0, 1, 2, 3])
            rot_p = rot_s
            gkv3 = prm[:, PC_LNKV:PC_LNKV + KT].unsqueeze(2)
            gm13 = prm[:, PC_LNMIX1:PC_LNMIX1 + KT].unsqueeze(2)
            estep = [0]
            for h in range(8):
                parts = [
                    (0, KT * 128, KT, wkv_d[:, h * 128:(h + 1) * 128].rearrange("(kt p) f -> p kt f", p=128)),
                    (KT * 128, KT * 128, KT, wkv_d[:, D + h * 128:D + (h + 1) * 128].rearrange("(kt p) f -> p kt f", p=128)),
                    (2 * KT * 128, KT * 128, KT, wq_d[:, h * 128:(h + 1) * 128].rearrange("(kt p) f -> p kt f", p=128)),
                ]
                sw_, bw_ = W.get(parts)
                for part, g3 in ((0, gkv3), (1, gkv3), (2, gm13)):
                    wv = RW(sw_, part * 1024, (part + 1) * 1024).rearrange("p (k f) -> p k f", k=KT)
                    P.tt("dve", wv, wv, g3.broadcast_to([128, KT, 128]), ALU.mult, (bw_, cB), (bw_,))
                fin_prev = [None]
                for which in range(2):
                    woff = 0 if which == 0 else 2048
                    for tc in range(NTC):
                        pt, ptB = rot_p.next()
                        for kt in range(KT):
                            c0 = woff + kt * 128
                            P.matmul(pt[:], RW(sw_, c0, c0 + 128), hT[:, kt, tcs(tc)], kt == 0, kt == KT - 1,
                                     (bw_, hB[kt][tc]), (ptB,))
                        slot2 = (4 * which + tc) % 2
                        kqc, kqcB = scr(30 + 2 * slot2, 2)
                        sqh, sqhB = scr(24 + slot2, 1, BF16)
                        P.copy("dve", kqc, pt[:], (ptB,), kqcB)
                        P.tt("dve", sqh, kqc, kqc, ALU.mult, kqcB, sqhB)

                        def fin(which=which, tc=tc, kqc=kqc, kqcB=kqcB, sqh=sqh, sqhB=sqhB):
                            pst, pstB = rot_p.next()
                            P.matmul(pst[:], blk_bf[:], sqh, True, True, list(sqhB) + [cB], (pstB,))
                            lnt, lntB = scr(26, 2)
                            rsh, rshB = scr(28, 2)
                            P.act(lnt, pst[:], AF.Ln, (pstB, smB), lntB, bias=small[:, 0:1], scale=1.0 / 64)
                            P.act(rsh, lnt, AF.Exp, lntB, rshB, scale=-0.5)
                            if which == 0:
                                P.stt("dve", kT0[0:64, tcs(tc)], kqc[0:64, :], prm[0:64, PC_GK:PC_GK + 1], rsh[0:64, :],
                                      ALU.mult, ALU.mult, list(kqcB) + list(rshB) + [cB], kT0B)
                                P.stt("dve", kT1[64:128, tcs(tc)], kqc[64:128, :], prm[64:128, PC_GK:PC_GK + 1], rsh[64:128, :],
                                      ALU.mult, ALU.mult, list(kqcB) + list(rshB) + [cB], kT1B)
                            else:
                                P.stt("dve", qTh[:, tcs(tc)], kqc, small[:, 2:3], rsh,
                                      ALU.mult, ALU.mult, list(kqcB) + list(rshB) + [smB], qThB)
                        if fin_prev[0] is not None:
                            fin_prev[0]()
                        fin_prev[0] = fin
                for tg in range(4):
                    pt, ptB = rot_p.next()
                    for t in range(4):
                        tok0 = (4 * tg + t) * 128
                        for kt in range(KT):
                            c0 = 1024 + kt * 128
                            P.matmul(pt[:, t * 128:(t + 1) * 128], hT[:, kt, tok0:tok0 + 128], RW(sw_, c0, c0 + 128),
                                     kt == 0, kt == KT - 1, (bw_, hB[kt][tg]), (ptB,))
                    P.copy("dve" if tg % 2 else "act", Vh[:, tg * 512:(tg + 1) * 512], pt[:], (ptB,), VhB)
                    if tg == 0:
                        fin_prev[0]()
                W.release()
                LA = 6
                pending = []
                for qc in range(NTC):
                    nk = 4 * qc + 4
                    seq = [(c, ki) for ki in range(nk) for c in (0, 1)]
                    ets = {}
                    acc = [(ps[4], psB[4], ps[6], psB[6]), (ps[5], psB[5], ps[7], psB[7])]
                    for step in range(len(seq) + LA):
                        if step < len(seq):
                            c, ki = seq[step]
                            kTc, kTcB = (kT0, kT0B) if c == 0 else (kT1, kT1B)
                            d = ki - 4 * qc
                            q0 = max(d, 0) * 128
                            pst, pstB = rot_s.next()
                            P.matmul(pst[:, q0:512], kTc[:, ki * 128:(ki + 1) * 128], qTh[:, qc * 512 + q0:(qc + 1) * 512],
                                     True, d < 0, list(kTcB) + list(qThB), (pstB,))
                            if d >= 0:
                                P.matmul(pst[:, q0:512], ident_bf[:], masks[:, d, q0:512], False, True, (cB,), (pstB,))
                            et, etB = scr(16 + (estep[0] % 8), 1, BF16)
                            estep[0] += 1
                            P.act(et[:, q0:512], pst[:, q0:512], AF.Exp, (pstB,), etB)
                            ets[step] = (et, etB, c, ki, q0)
                            if pending and step >= 1:
                                pending.pop(0)()
                        if step >= LA:
                            et, etB, c, ki, q0 = ets[step - LA]
                            po, poB, pz, pzB = acc[c]
                            P.matmul(po[:, q0:512], Vh3[:, ki, :], et[:, q0:512], ki == 0, ki == nk - 1,
                                     list(VhB) + list(etB), (poB,))
                            P.matmul(pz[:, q0:512], ones_bf[:], et[:, q0:512], ki == 0, ki == nk - 1,
                                     list(etB) + [cB], (pzB,))
                    o_aps = []
                    rzs = []
                    for c in range(2):
                        po, poB, pz, pzB = acc[c]
                        rz, rzB = scr(30 + 2 * c, 2)
                        oc, ocB = scr(36 + 2 * c, 2)
                        P.act(rz, pz[:], AF.Ln, (pzB,), rzB)
                        P.copy("dve", oc, po[:], (poB,), ocB)
                        o_aps.append((oc, ocB))
                        rzs.append((rz, rzB))
                    def tail(h=h, qc=qc, rzs=rzs, o_aps=o_aps):
                        (o0, o0B), (o1, o1B) = o_aps
                        osq, osqB = scr(24, 1, BF16)
                        oln, olnB = scr(26, 2)
                        ors, orsB = scr(28, 2)
                        st = {}

                        def stat_mm():
                            st["p"] = rot_s.next()
                            P.matmul(st["p"][0][:], ones_bf[:], osq, True, True, list(osqB) + [cB], (st["p"][1],))
                        ops_ = []
                        for c in range(2):
                            rz, rzB = rzs[c]
                            oc, ocB = o_aps[c]
                            ops_.append(lambda rz=rz, rzB=rzB: P.act(rz, rz, AF.Exp, rzB, rzB, scale=-1.0))
                            ops_.append(lambda oc=oc, ocB=ocB, rz=rz, rzB=rzB: P.tt("dve", oc, oc, rz, ALU.mult, list(ocB) + list(rzB), ocB))
                        ops_.append(lambda: P.stt("dve", o0, o1, small[:, 1:2], o0, ALU.mult, ALU.add,
                                                  list(o0B) + list(o1B) + [smB], o0B))
                        ops_.append(lambda: P.tt("dve", osq, o0, o0, ALU.mult, o0B, osqB))
                        ops_.append(stat_mm)
                        ops_.append(lambda: P.act(oln, st["p"][0][:], AF.Ln, (st["p"][1], smB), olnB, bias=small[:, 0:1], scale=1.0 / 128))
                        ops_.append(lambda: P.act(ors, oln, AF.Exp, olnB, orsB, scale=-0.5))
                        ops_.append(lambda: P.stt("dve", AB[:, h, tcs(qc)], o0, small[:, 3:4], ors, ALU.mult, ALU.mult,
                                                  list(o0B) + list(orsB) + [smB], (aB[h][qc],)))
                        return ops_
                    pending.extend(tail())
                while pending:
                    pending.pop(0)()
            rot = Rot([0, 1, 2, 3, 4, 5, 6, 7])
            for half in range(2):
                so_, bo_ = W.get(wcolblock(wo_d, half * 512, 512))
                linear_fm(AB, aB, KT, so_, 0, 512, bo_, range(4), rot,
                          lambda m, tc, pt, ptB, half=half: add_residual(4 * half + m, tc, pt, ptB))
                W.release()

        if stage >= 5:
            rtr_ap, rtr_bufs = scr(24, 8)
            rtr = rtr_ap.rearrange("p (a b) -> p a b", a=16)
            rtB = rtr_bufs[0]

            def R(i):
                return rtr[:, i, :]

            def R3(i):
                return rtr[:, i, :].rearrange("p (t e) -> p t e", t=NT)
            rstd_tok = rtr[:, 6, 0:16]
            m1 = rtr[:, 6, 16:32]
            m2 = rtr[:, 6, 32:48]
            den = rtr[:, 6, 48:64]
            rden = rtr[:, 6, 64:80]
            P1p = rtr[:, 6, 80:96]
            P2 = rtr[:, 6, 96:112]
            sp = rtr[:, 6, 112:128]
            gw = rtr[:, 7, 0:KT * NE].rearrange("p (k e) -> p k e", k=KT)
            ne_ = rtr[:, 7, 64:72]
            ntl = rtr[:, 7, 72:80]
            tb = rtr[:, 7, 80:88]
            base = rtr[:, 7, 88:96]
            P1 = rtr[:, 7, 96:112]
            G1 = rtr[:, 11, 0:16]
            G2 = rtr[:, 11, 16:32]
            etf = rtr[:, 11, 32:48]
            idx1 = rtr[:, 12, 0:16].bitcast(mybir.dt.int32)
            idx2 = rtr[:, 12, 16:32].bitcast(mybir.dt.int32)
            eid = rtr[:, 12, 32:48].bitcast(mybir.dt.int32)
            selb = rtr[:, 13, 0:64].bitcast(BF16)
            P.memset("dve", rtr_ap, 0.0, rtr_bufs)

            htok = hT[:].rearrange("p k t -> p (k t)").rearrange("p (a b) -> p a b", a=NT)

            def htokB(tt):
                return [hB[tt // 2][2 * (tt % 2)], hB[tt // 2][2 * (tt % 2) + 1]]
            hnc, hncB = scr(32, 8, BF16)
            hnc3 = hnc.rearrange("p (k t) -> p k t", k=KT)
            rot_t = Rot([0, 1, 2])

            def post_rs(tc, rs, rsB):
                pt, ptB = ps[3], psB[3]
                for t in range(4):
                    P.transpose(pt[:, t * 128:(t + 1) * 128], rs[:, t * 128:(t + 1) * 128], ident[:], list(rsB) + [cB], (ptB,))
                P.copy("dve", rstd_tok[:, 4 * tc:4 * tc + 4], pt[:].rearrange("p (t c) -> p t c", c=128)[:, :, 0], (ptB,), (rtB,))

            def post_tc(tc):
                for t in range(4):
                    tt = 4 * tc + t
                    pt, ptB = rot_t.next()
                    ptb = pt[:].bitcast(BF16)
                    for kt in range(KT):
                        P.transpose(ptb[:, kt * 128:(kt + 1) * 128], hnc3[:, kt, t * 128:(t + 1) * 128], ident_bf[:],
                                    list(hncB) + [cB], (ptB,))
                    P.copy("act" if t % 2 else "dve", htok[:, tt, :], ptb, (ptB,), htokB(tt))
            rmsnorm(PC_LNFFN1, post_rs, dst=lambda kt, tc: (hnc3[:, kt, :], hncB), post_tc=post_tc)

            P.tt("dve", gw, rw[:], prm[:, PC_LNFFN1:PC_LNFFN1 + KT].unsqueeze(2).broadcast_to([128, KT, NE]), ALU.mult,
                 (cB, rtB), (rtB,))
            pl, plB = ps[0], psB[0]
            for t in range(NT):
                for kt in range(KT):
                    P.matmul(pl[:, t * NE:(t + 1) * NE], xT[:, kt, t * 128:(t + 1) * 128], gw[:, kt, :], kt == 0, kt == KT - 1,
                             (xB[kt][t // 4], rtB), (plB,))
            bc = lambda v: v.unsqueeze(2).broadcast_to([128, NT, NE])
            DV = lambda *a: P.tt("dve", *a, (rtB,), (rtB,))
            P.tt("dve", R3(0), pl[:, 0:NT * NE].rearrange("p (t e) -> p t e", t=NT), bc(rstd_tok), ALU.mult, (plB, rtB), (rtB,))
            def first_one(src, ta, tb_):
                cur = src
                for sh, dst in ((1, ta), (2, tb_), (4, ta)):
                    P.copy("dve", R3(dst)[:, :, 0:sh], R3(cur)[:, :, 0:sh], (rtB,), (rtB,))
                    DV(R3(dst)[:, :, sh:NE], R3(cur)[:, :, sh:NE], R3(cur)[:, :, 0:NE - sh], ALU.add)
                    cur = dst
                P.ts("dve", R(tb_), R(cur), 1.0, None, ALU.is_equal, None, (rtB,), (rtB,))
                DV(R(src), R(src), R(tb_), ALU.mult)
            P.reduce(m1, R3(0), ALU.max, (rtB,), (rtB,))
            DV(R3(1), R3(0), bc(m1), ALU.is_equal)
            first_one(1, 8, 9)
            P.stt("dve", R(2), R(1), -1e30, R(0), ALU.mult, ALU.add, (rtB,), (rtB,))
            P.reduce(m2, R3(2), ALU.max, (rtB,), (rtB,))
            DV(R3(3), R3(2), bc(m2), ALU.is_equal)
            first_one(3, 8, 9)
            DV(R(3), R(3), R(1), ALU.add)
            DV(R3(4), R3(0), bc(m1), ALU.subtract)
            P.act(R(4), R(4), AF.Exp, (rtB,), (rtB,))
            DV(R(4), R(4), R(3), ALU.mult)
            P.reduce(den, R3(4), ALU.add, (rtB,), (rtB,))
            P.recip(rden, den, (rtB,), (rtB,))
            DV(R3(5), R3(4), bc(rden), ALU.mult)
            P.copy("dve", selb, R(3), (rtB,), (rtB,))
            pa, paB = ps[1], psB[1]
            pb, pbB = ps[2], psB[2]
            P.matmul(pa[:, 0:128], tri_bf[:], selb, True, True, (rtB, cB), (paB,))
            P.matmul(pb[:, 0:128], ones_bf[:], selb, True, True, (rtB, cB), (pbB,))
            P.copy("dve", R(8), pa[:, 0:128], (paB,), (rtB,))
            P.copy("dve", R(9), pb[:, 0:128], (pbB,), (rtB,))
            src_, dst_ = 9, 10
            for sh in (1, 2, 4, 8):
                P.copy("dve", R3(dst_)[:, 0:sh, :], R3(src_)[:, 0:sh, :], (rtB,), (rtB,))
                DV(R3(dst_)[:, sh:NT, :], R3(src_)[:, sh:NT, :], R3(src_)[:, 0:NT - sh, :], ALU.add)
                src_, dst_ = dst_, (14 if dst_ == 10 else 10)
            P.copy("dve", ne_, R3(src_)[:, NT - 1, :], (rtB,), (rtB,))
            if src_ != 10:
                DV(R(10), R(src_), R(9), ALU.subtract)
            else:
                DV(R(14), R(10), R(9), ALU.subtract)
                P.copy("dve", R(10), R(14), (rtB,), (rtB,))
            P.ts("dve", ntl, ne_, 0.0, None, ALU.is_gt, None, (rtB,), (rtB,))
            for thr in (512.0, 1024.0, 1536.0):
                P.stt("dve", ntl, ne_, thr, ntl, ALU.is_gt, ALU.add, (rtB,), (rtB,))
            P.memset("dve", tb[:, 0:1], 0.0, (rtB,))
            for e in range(1, NE):
                DV(tb[:, e:e + 1], tb[:, e - 1:e], ntl[:, e - 1:e], ALU.add)
            P.ts("dve", base, tb, 512.0, None, ALU.mult, None, (rtB,), (rtB,))
            DV(R(8), R(8), R(10), ALU.add)
            DV(R3(8), R3(8), base.unsqueeze(1).broadcast_to([128, NT, NE]), ALU.add)
            P.stt("dve", R(2), R(8), 1.0, R(3), ALU.add, ALU.mult, (rtB,), (rtB,))
            P.reduce(P1p, R3(2), ALU.max, (rtB,), (rtB,))
            P.reduce(sp, R3(2), ALU.add, (rtB,), (rtB,))
            DV(R3(1), R3(2), bc(P1p), ALU.is_equal)
            DV(R(1), R(1), R(5), ALU.mult)
            P.reduce(G1, R3(1), ALU.add, (rtB,), (rtB,))
            P.ts("dve", G2, G1, -1.0, 1.0, ALU.mult, ALU.add, (rtB,), (rtB,))
            P.ts("dve", P1, P1p, -1.0, None, ALU.add, None, (rtB,), (rtB,))
            P.stt("dve", P2, sp, -1.0, P1p, ALU.add, ALU.subtract, (rtB,), (rtB,))
            P.copy("dve", idx1, P1, (rtB,), (rtB,))
            P.copy("dve", idx2, P2, (rtB,), (rtB,))
            DV(R3(15), prm[:, PC_IOTA:PC_IOTA + 16].unsqueeze(2).broadcast_to([128, NT, NE]),
               tb.unsqueeze(1).broadcast_to([128, NT, NE]), ALU.is_ge)
            P.reduce(etf, R3(15), ALU.add, (rtB, cB), (rtB,))
            P.ts("dve", etf, etf, -1.0, None, ALU.add, None, (rtB,), (rtB,))
            P.copy("dve", eid, etf, (rtB,), (rtB,))
            nused = rtr[:, 11, 48:49]
            flagf = rtr[:, 11, 64:80]
            flags = rtr[:, 12, 48:64].bitcast(mybir.dt.int32)
            P.reduce(nused, ntl, ALU.add, (rtB,), (rtB,))
            P.ts("dve", flagf, prm[:, PC_IOTA:PC_IOTA + 16], nused, None, ALU.is_lt, None, (rtB, cB), (rtB,))
            if not P.plan:
                P.flag_op = len(P.ops)
                P.flag_ap = flags
            P.copy("dve", flags, flagf, (rtB,), (rtB,))
            g_ = nc.gpsimd
            for tt in range(NT):
                for ix in (idx1, idx2):
                    P.op("pool", (lambda ix=ix, tt=tt: g_.indirect_dma_start(
                        out=HG[:, :], out_offset=bass.IndirectOffsetOnAxis(ap=ix[:, tt:tt + 1], axis=0),
                        in_=htok[:, tt, :], in_offset=None)), list(htokB(tt)) + [rtB],
                        (hgB[(2 * tt + (0 if ix is idx1 else 1)) % 16],), dma=True)

            W.go()
            tcnt = [0]
            yacc = hT[:, 0:4, :].rearrange("p k t -> p (k t)").bitcast(F32).rearrange("p (s f) -> p s f", s=4)
            yaccB = lambda s_, fh: [hB[s_][2 * fh], hB[s_][2 * fh + 1]]
            hgs_l, hgsB_l, hgT_l, hgTB_l = [], [], [], []
            hgs_l.append(hT[:, 4:6, :].rearrange("p k t -> p (k t)").rearrange("p (s f) -> p s f", s=4))
            hgsB_l.append([hB[k][t] for k in (4, 5) for t in range(NTC)])
            hgT_l.append(hT[:, 6:8, :].rearrange("p k t -> p (k t)").rearrange("p (k s) -> p k s", k=KT))
            hgTB_l.append(lambda kt: [hB[6 + kt // 4][kt % 4]])
            a_, b_ = scr(8, 8, BF16)
            hgs_l.append(a_.rearrange("p (s f) -> p s f", s=4))
            hgsB_l.append(list(b_))
            a_, b2_ = scr(0, 8, BF16)
            hgT_l.append(a_.rearrange("p (k s) -> p k s", k=KT))
            hgTB_l.append(lambda kt, b2_=b2_: [b2_[kt]])
            actT = lambda a: ABf[:, a * 512:(a + 1) * 512]
            actB = lambda a: aB[a // 4][a % 4]
            rot_gu = Rot([0, 1, 2, 3])
            rot_dn = Rot([4, 5, 6, 7])

            def dyn_part(t, kind, gi):
                re_, rgu_, rdn_ = regs[t % 2]
                if kind == "d":
                    const = gi * 512 * D
                    pat = [[D, 128], [128 * D, 4], [1, D]]
                    th, rb, split = mdn_t, rdn_, 4
                else:
                    const = gi * 512 + (DFE if kind == "u" else 0)
                    pat = [[2 * DFE, 128], [128 * 2 * DFE, KT], [1, 512]]
                    th, rb, split = mgu_t, rgu_, KT
                rt_ = tmpr[tcnt[0] % 4]
                tcnt[0] += 1
                src = (lambda th=th, rt_=rt_, pat=pat: bass.AP(th, rt_, pat))
                return (0, SLOT, split, src, (lambda rt_=rt_, rb=rb, const=const: (g_.reg_add(rt_, rb, const), rt_)[1]))

            def tile_pre(t):
                re_, rgu_, rdn_ = regs[t % 2]

                def fn():
                    g_.reg_load(re_, eid[0:1, t:t + 1])
                    g_.reg_mul(rgu_, re_, D * 2 * DFE)
                    return g_.reg_mul(rdn_, re_, DFE * D)
                return (fn, (rtB,))

            def prep_tile(t):
                par = t % 2
                hgs, hgsB, hgT, hgTB = hgs_l[par], hgsB_l[par], hgT_l[par], hgTB_l[par]
                P.dma("sp", hgs, HG[t * 512:(t + 1) * 512, :].rearrange("(s p) d -> p s d", p=128), hgB, hgsB)
                for kp in range(4):
                    pt, ptB = rot_dn.next()
                    ptb = pt[:].bitcast(BF16)
                    for k2 in range(2):
                        kt = 2 * kp + k2
                        for s_ in range(4):
                            P.transpose(ptb[:, k2 * 512 + s_ * 128:k2 * 512 + (s_ + 1) * 128], hgs[:, s_, kt * 128:(kt + 1) * 128],
                                        ident_bf[:], list(hgsB) + [cB], (ptB,))
                    P.copy("act" if kp % 2 else "dve", hgT[:, 2 * kp:2 * kp + 2, :].rearrange("p k s -> p (k s)"), ptb,
                           (ptB,), hgTB(2 * kp) + hgTB(2 * kp + 1))

            ctag = lambda t: (t if t >= 8 else None)
            prep_tile(0)
            for t in range(NTILE):
                P.cond = ctag(t)
                hgT, hgTB = hgT_l[t % 2], hgTB_l[t % 2]
                for gi in range(7):
                    sg_, bg_ = W.get({"parts": [dyn_part(t, "g", gi)], "pre": tile_pre(t) if gi == 0 else None, "hold": True, "cond": ctag(t)})
                    su_, bu_ = W.get({"parts": [dyn_part(t, "u", gi)], "pre": None, "hold": True, "cond": ctag(t)})
                    sd_, bd_ = W.get({"parts": [dyn_part(t, "d", gi)], "pre": None, "hold": True, "cond": ctag(t)})
                    abase = 4 * (gi % 2)
                    for jj in range(4):
                        a = abase + jj
                        pg, pgB = rot_gu.next()
                        pu, puB = rot_gu.next()
                        for (pt, ptB, slot, wb) in ((pg, pgB, sg_, bg_), (pu, puB, su_, bu_)):
                            for kt in range(KT):
                                cc = kt * 512 + jj * 128
                                P.matmul(pt[:], RW(slot, cc, cc + 128), hgT[:, kt, :], kt == 0, kt == KT - 1,
                                         [wb] + hgTB(kt), (ptB,))
                        sl, slB = scr(32 + 2 * (jj % 2), 2)
                        P.act(sl, pg[:], AF.Silu, (pgB,), slB)
                        P.tt("dve", actT(a), sl, pu[:], ALU.mult, list(slB) + [puB], (actB(a),))
                    W.release(); W.release()
                    if gi == 3 and t + 1 < NTILE:
                        P.cond = ctag(t + 1)
                        prep_tile(t + 1)
                        P.cond = ctag(t)
                    for s_ in range(4):
                        for fh in range(2):
                            pt, ptB = rot_dn.next()
                            for jj in range(4):
                                cc = jj * D + fh * 512
                                P.matmul(pt[:], actT(abase + jj)[:, s_ * 128:(s_ + 1) * 128], RW(sd_, cc, cc + 512),
                                         jj == 0, jj == 3, (bd_, actB(abase + jj)), (ptB,))
                            ya = yacc[:, s_, fh * 512:(fh + 1) * 512]
                            if gi == 0:
                                P.copy("act", ya, pt[:], (ptB,), yaccB(s_, fh))
                            else:
                                P.tt("dve", ya, ya, pt[:], ALU.add, [ptB] + yaccB(s_, fh), yaccB(s_, fh))
                    W.release()
                P.dma("sp", YD[t * 512:(t + 1) * 512, :].rearrange("(s p) f -> p s f", p=128), yacc,
                      [b for s_ in range(4) for fh in range(2) for b in yaccB(s_, fh)], (ydB,))
            P.cond = None

            rot = Rot([0, 1, 2, 3])
            for t in range(NT):
                o3 = (0, 8, 16, 32)[t % 4]
                b1, b1B = scr(o3, 4)
                b2, b2B = scr(o3 + 4, 4)
                for (bb_, bbB_, ix) in ((b1, b1B, idx1), (b2, b2B, idx2)):
                    P.op("pool", (lambda bb_=bb_, ix=ix, t=t: g_.indirect_dma_start(
                        out=bb_, out_offset=None, in_=YD[:, :],
                        in_offset=bass.IndirectOffsetOnAxis(ap=ix[:, t:t + 1], axis=0))), (ydB, rtB), bbB_, dma=True)
                for hf in range(2):
                    pt, ptB = rot.next()
                    for k4 in range(4):
                        kt = 4 * hf + k4
                        P.transpose(pt[:, k4 * 128:(k4 + 1) * 128], xT[:, kt, t * 128:(t + 1) * 128], ident[:],
                                    (xB[kt][t // 4], cB), (ptB,))
                    hs = slice(hf * 512, (hf + 1) * 512)
                    P.stt("dve", b1[:, hs], b1[:, hs], G1[:, t:t + 1], pt[:], ALU.mult, ALU.add,
                          list(b1B) + [ptB, rtB], b1B)
                    P.stt("dve", b1[:, hs], b2[:, hs], G2[:, t:t + 1], b1[:, hs], ALU.mult, ALU.add,
                          list(b2B) + list(b1B) + [rtB], b1B)
                P.dma("sp", out_d[t * 128:(t + 1) * 128, :], b1, b1B, ())
        else:
            rot = Rot([0, 1, 2, 3])
            for t in range(NT):
                ob, obB = scr(4 * (t % 4), 4)
                for hf in range(2):
                    pt, ptB = rot.next()
                    for k4 in range(4):
                        kt = 4 * hf + k4
                        P.transpose(pt[:, k4 * 128:(k4 + 1) * 128], xT[:, kt, t * 128:(t + 1) * 128], ident[:],
                                    (xB[kt][t // 4], cB), (ptB,))
                    P.copy("act" if hf else "dve", ob[:, hf * 512:(hf + 1) * 512], pt[:], (ptB,), obB)
                P.dma("sp", out_d[t * 128:(t + 1) * 128, :], ob, obB, ())

    P.plan = True
    Wp = WStream(P, RW, wB, None)
    body(P, Wp)
    P.plan = False
    first_extra = {NS + j: [aB[2 + 2 * j + k][t] for k in range(2) for t in range(NTC)] for j in range(3)}
    W = WStream(P, RW, wB, Wp.blocks, first_extra)
    body(P, W)
    sems = []

    def sem_alloc(name):
        s = nc.alloc_semaphore(name)
        sems.append(s)
        return s
    n = P.finalize(sem_alloc)
    return nc, n


def host_inputs(inp):
    f = lambda a: np.ascontiguousarray(np.asarray(a, dtype=np.float32))
    prm = np.zeros((128, NPC), np.float32)

    def cols(v):
        return np.asarray(v, np.float32).reshape(KT, 128).T
    prm[:, PC_LNMIX0:PC_LNMIX0 + 8] = cols(inp["ln_mix"][0])
    prm[:, PC_LNFFN0:PC_LNFFN0 + 8] = cols(inp["ln_ffn"][0])
    prm[:, PC_LNKV:PC_LNKV + 8] = cols(inp["ln_kv"])
    prm[:, PC_LNMIX1:PC_LNMIX1 + 8] = cols(inp["ln_mix"][1])
    prm[:, PC_LNFFN1:PC_LNFFN1 + 8] = cols(inp["ln_ffn"][1])
    for j in range(3):
        prm[:, PC_CW + 8 * j:PC_CW + 8 * j + 8] = cols(inp["conv_w"][0][j])
    prm[:, PC_GK] = np.tile(np.asarray(inp["k_norm"], np.float32), 2)
    prm[:, PC_GQ] = np.tile(np.asarray(inp["q_norm"][0], np.float32), 2)
    prm[:, PC_GS] = np.asarray(inp["sub_norm"][0], np.float32)
    prm[:, PC_IOTA:PC_IOTA + 16] = np.arange(16, dtype=np.float32)[None, :]
    ident = np.eye(128, dtype=np.float32)
    k = np.arange(128)[:, None]
    q = np.arange(512)[None, :]
    masks = np.concatenate([(q >= d * 128 + k).astype(np.float32) for d in range(4)], axis=1)
    shared = {
        "params": prm,
        "lam": f(inp["lam_params"]).reshape(1, 256),
        "conv_w_in": f(inp["conv_w_in"][0]),
        "conv_w_out": f(inp["conv_w_out"][0]),
        "w_kv": f(inp["w_kv"]),
        "attn_w_q": f(inp["attn_w_q"][0]),
        "attn_w_o": f(inp["attn_w_o"][0]),
        "ffn_w_gu": f(inp["ffn_w_gu"][0]),
        "ffn_w_down": f(inp["ffn_w_down"][0]),
        "router_w": f(inp["router_w"][0]),
        "moe_w_gu": f(inp["moe_w_gu"][0]),
        "moe_w_down": f(inp["moe_w_down"][0]),
        "ident": ident,
        "tri": np.triu(np.ones((128, 128), np.float32), 1),
        "masks": np.ascontiguousarray(masks),
    }
    x = f(inp["x"])
    return [dict(shared, x=np.ascontiguousarray(x[b])) for b in range(8)]


_CACHE = {}


def kernel(**inputs):
    if "nc" not in _CACHE:
        _CACHE["nc"] = build()[0]
    nc = _CACHE["nc"]
    in_maps = host_inputs(inputs)
    res = run_bass_kernel_spmd(nc, in_maps, core_ids=list(range(8)))
    return np.stack([np.asarray(r["out"], dtype=np.float32) for r in res.results], axis=0)
```

```python
import math
import re
import numpy as np
import concourse.bass as bass
import concourse.mybir as mybir
from concourse.bass_utils import run_bass_kernel_spmd

F32 = mybir.dt.float32
BF16 = mybir.dt.bfloat16
AF = mybir.ActivationFunctionType
ALU = mybir.AluOpType
AX = mybir.AxisListType

D = 1024
S = 2048
KT = 8
NTC = 4
NT = 16
DFF = 2816
NFF = 22
NE = 8
DFE = 3584
NFE = 28
EPS = 1e-6
LAM_INIT = 0.8 - 0.6 * math.exp(-0.3 * 1.0)
NS = 4
SLOT = 4096

PC_LNMIX0, PC_LNFFN0, PC_LNKV, PC_LNMIX1, PC_LNFFN1 = 0, 8, 16, 24, 32
PC_CW = 40
PC_GK, PC_GQ, PC_GS = 64, 65, 66
PC_IOTA = 67
NPC = 83
NTILE = 15
NSLOT = NTILE * 512


class Buf:
    __slots__ = ("name", "lw", "rd")

    def __init__(self, name):
        self.name = name
        self.lw = None
        self.rd = {}


class Prog:
    def __init__(self, nc):
        self.nc = nc
        self.ops = []
        self.plan = False
        self.cond = None
        self.flag_op = None
        self.flag_ap = None
        self.E = {"pe": nc.tensor, "act": nc.scalar, "dve": nc.vector,
                  "pool": nc.gpsimd, "sp": nc.sync}

    def op(self, eng, fn, r=(), w=(), dma=False):
        if self.plan:
            return
        self.ops.append([eng, fn, tuple(r), tuple(w), dma, None, False, 0, self.cond])

    def matmul(self, out, lhsT, rhs, start, stop, r, w):
        nc = self.nc
        self.op("pe", lambda: nc.tensor.matmul(out, lhsT, rhs, start=start, stop=stop), r, w)

    def transpose(self, out, in_, ident, r, w):
        nc = self.nc
        self.op("pe", lambda: nc.tensor.transpose(out, in_, ident), r, w)

    def act(self, out, in_, func, r, w, bias=None, scale=None):
        nc = self.nc
        kw = {}
        if bias is not None:
            kw["bias"] = bias
        if scale is not None:
            kw["scale"] = scale
        self.op("act", lambda: nc.scalar.activation(out=out, in_=in_, func=func, **kw), r, w)

    def copy(self, eng, out, in_, r, w):
        nc = self.nc
        if eng == "act":
            self.op("act", lambda: nc.scalar.copy(out=out, in_=in_), r, w)
        else:
            e = self.E[eng]
            self.op(eng, lambda: e.tensor_copy(out=out, in_=in_), r, w)

    def tt(self, eng, out, in0, in1, op, r, w):
        e = self.E[eng]
        self.op(eng, lambda: e.tensor_tensor(out=out, in0=in0, in1=in1, op=op), r, w)

    def ts(self, eng, out, in0, s1, s2, op0, op1, r, w):
        e = self.E[eng]
        if s2 is None:
            self.op(eng, lambda: e.tensor_scalar(out=out, in0=in0, scalar1=s1, scalar2=None, op0=op0), r, w)
        else:
            self.op(eng, lambda: e.tensor_scalar(out=out, in0=in0, scalar1=s1, scalar2=s2, op0=op0, op1=op1), r, w)

    def stt(self, eng, out, in0, scalar, in1, op0, op1, r, w):
        e = self.E[eng]
        self.op(eng, lambda: e.scalar_tensor_tensor(out=out, in0=in0, scalar=scalar, in1=in1, op0=op0, op1=op1), r, w)

    def recip(self, out, in_, r, w):
        nc = self.nc
        self.op("dve", lambda: nc.vector.reciprocal(out=out, in_=in_), r, w)

    def reduce(self, out, in_, op, r, w):
        nc = self.nc
        self.op("dve", lambda: nc.vector.tensor_reduce(out=out, in_=in_, axis=AX.X, op=op), r, w)

    def memset(self, eng, ap, val, w):
        e = self.E[eng]
        self.op(eng, lambda: e.memset(ap, val), (), w)

    def dma(self, q, out, in_, r, w):
        e = self.E[q]
        self.op(q, lambda: e.dma_start(out=out, in_=in_), r, w, dma=True)

    @staticmethod
    def _ckey(o):
        if o[4]:
            b = o[3][0] if o[3] else o[2][0]
            return ("dma", o[0], b.name)
        return o[0]

    def finalize(self, sem_alloc):
        ops = self.ops
        ck = self._ckey
        nc = self.nc
        for i, o in enumerate(ops):
            deps = {}

            def add(j):
                k = ck(ops[j])
                if deps.get(k, -1) < j:
                    deps[k] = j
            for b in o[2]:
                if b.lw is not None:
                    add(b.lw)
            for b in o[3]:
                if b.lw is not None:
                    add(b.lw)
                for j in b.rd.values():
                    add(j)
            if o[0] == "pe" and not o[4]:
                deps.pop("pe", None)
            o[5] = list(deps.values())
            for j in o[5]:
                ops[j][6] = True
            k = ck(o)
            for b in o[2]:
                b.rd[k] = i
            for b in o[3]:
                b.lw = i
                b.rd = {}
        if self.flag_op is not None:
            ops[self.flag_op][6] = True
        sems = {}
        cnt = {}
        per_eng = {}
        runs = {}
        nodrain = set()
        for i, o in enumerate(ops):
            k = ck(o)
            eng = o[0]
            per_eng.setdefault(eng, []).append(i)
            rl = runs.setdefault(eng, [])
            if not rl or rl[-1]["tag"] != o[8]:
                rl.append({"tag": o[8], "incs": {}, "pre": {}, "first": i})
            run = rl[-1]
            o.append(len(rl) - 1)
            inc = 16 if o[4] else (1 if o[6] else 0)
            if inc:
                if k not in sems:
                    sems[k] = sem_alloc("s_" + "_".join(k) if isinstance(k, tuple) else "s_" + k)
                    cnt[k] = 0
                if o[4] and eng == "pool" and o[8] is not None:
                    nodrain.add(k)
                else:
                    if k not in run["pre"]:
                        run["pre"][k] = cnt[k]
                    run["incs"][k] = run["incs"].get(k, 0) + inc
                cnt[k] += inc
                o[7] = cnt[k]
        for eng, idxs in per_eng.items():
            e = self.E[eng]
            wd = {}
            cur_run = -1
            guard = None
            snap = None
            freg = None
            flag_waited = False

            def close_run(run):
                guard.__exit__(None, None, None)
                with e.Else():
                    for k2, amt in run["incs"].items():
                        if run["pre"][k2] > 0:
                            e.wait_ge(sems[k2], run["pre"][k2])
                        e.sem_inc(sems[k2], amt)
            for i in idxs:
                o = ops[i]
                if o[9] != cur_run:
                    if guard is not None:
                        close_run(runs[eng][cur_run])
                        guard = None
                        wd = snap
                    cur_run = o[9]
                    run = runs[eng][cur_run]
                    if run["tag"] is not None:
                        if freg is None:
                            freg = e.alloc_register("flag_" + eng)
                        if not flag_waited:
                            fo = ops[self.flag_op]
                            if eng != "dve":
                                e.wait_ge(sems[ck(fo)], fo[7])
                            else:
                                e.wait_ge(sems["dve"], fo[7])
                            flag_waited = True
                        e.reg_load(freg, self.flag_ap[0:1, run["tag"]:run["tag"] + 1])
                        snap = dict(wd)
                        guard = e.If(freg)
                        guard.__enter__()
                need = {}
                for j in o[5]:
                    d = ops[j]
                    k = ck(d)
                    if need.get(k, 0) < d[7]:
                        need[k] = d[7]
                for k, v in need.items():
                    if wd.get(k, 0) >= v:
                        continue
                    e.wait_ge(sems[k], v)
                    wd[k] = v
                ins = o[1]()
                k = ck(o)
                if o[4]:
                    ins.then_inc(sems[k], 16)
                elif o[6]:
                    ins.then_inc(sems[k], 1)
            if guard is not None:
                close_run(runs[eng][cur_run])
        for k, s in sems.items():
            if isinstance(k, tuple) and k not in nodrain:
                nc.sync.wait_ge(s, cnt[k])
        return len(ops)


class WStream:
    def __init__(self, P, view, ring_bufs, blocks=None, first_extra=None):
        self.P = P
        self.view = view
        self.bufs = ring_bufs
        self.first_extra = dict(first_extra or {})
        self.ns = NS
        self.base = 0
        self.plan = blocks is None
        self.blocks = [] if blocks is None else blocks
        self.next_get = 0
        self.next_issue = 0
        self.n_released = 0
        self.unheld = False

    def _issue(self):
        i = self.next_issue
        s = (i - self.base) % self.ns
        blk = self.blocks[i]
        saved_cond = self.P.cond
        self.P.cond = blk.get("cond") if isinstance(blk, dict) else None
        if isinstance(blk, dict):
            if blk.get("pre") is not None:
                fn, reads = blk["pre"]
                self.P.op("pool", fn, reads, ())
            parts = blk["parts"]
        else:
            parts = blk
        for part in parts:
            off, n, split, src = part[:4]
            prefn = part[4] if len(part) > 4 else None
            dst = self.view(s, off, off + n).rearrange("p (a b) -> p a b", a=split)
            wbufs = (self.bufs[s],) + tuple(self.first_extra.pop(s, ()))
            if prefn is None:
                self.P.dma("pool", dst, src, (), wbufs)
            else:
                self.P.op("pool", (lambda prefn=prefn, dst=dst, src=src: self._dyn_dma(prefn, dst, src)),
                          (), wbufs, dma=True)
        self.P.cond = saved_cond
        self.next_issue += 1

    def _dyn_dma(self, prefn, dst, srcfn):
        g = self.P.nc.gpsimd
        rt = prefn()
        ins = g.dma_start(out=dst, in_=srcfn())
        m = re.search(r"R\[(Pool_tmp_(\d+))\]", str(ins.ins))
        if m:
            RH = type(rt)
            n = int(m.group(2))
            names = [m.group(1)] + [f"Pool_{rt.name}_snap_{n - k}" for k in range(1, 5)]
            for nm in names:
                try:
                    g.free_register(RH(nm, rt.engine))
                except ValueError:
                    pass
        return ins

    def _fill(self):
        while self.next_issue < len(self.blocks) and self.next_issue - self.n_released < self.ns:
            blk = self.blocks[self.next_issue]
            if isinstance(blk, dict) and blk.get("hold") and not self.unheld:
                return
            self._issue()

    def start(self):
        if self.plan:
            return
        self._fill()

    def go(self):
        if self.plan:
            self.go_at = self.next_get
            return
        assert self.next_issue == self.n_released == self.next_get, "ring must be drained when go() is called"
        self.unheld = True
        self.base = self.next_issue
        self.ns = len(self.bufs)
        self._fill()

    def get(self, parts):
        i = self.next_get
        self.next_get += 1
        if self.plan:
            self.blocks.append(parts)
            return 0, self.bufs[0]
        sl = (i - self.base) % self.ns
        return sl, self.bufs[sl]

    def release(self):
        if self.plan:
            return
        self.n_released += 1
        self._fill()


def build(stage=99):
    nc = bass.Bass("TRN2", target_bir_lowering=False)
    P = Prog(nc)

    def din(name, shape):
        return nc.dram_tensor(name, shape, F32, kind="ExternalInput").ap()

    x_d = din("x", [S, D])
    params_d = din("params", [128, NPC])
    lam_d = din("lam", [1, 256])
    win_d = din("conv_w_in", [D, 3 * D])
    wout_d = din("conv_w_out", [D, D])
    wkv_d = din("w_kv", [D, 2 * D])
    wq_d = din("attn_w_q", [D, D])
    wo_d = din("attn_w_o", [D, D])
    wgu_d = din("ffn_w_gu", [D, 2 * DFF])
    wdn_d = din("ffn_w_down", [DFF, D])
    rw_d = din("router_w", [D, NE])
    if stage >= 5:
        mgu_t = nc.dram_tensor("moe_w_gu", [NE, D, 2 * DFE], F32, kind="ExternalInput")
        mdn_t = nc.dram_tensor("moe_w_down", [NE, DFE, D], F32, kind="ExternalInput")
    else:
        mgu_t = nc.dram_tensor("moe_w_gu", [NE, 1, 1], F32, kind="ExternalInput")
        mdn_t = nc.dram_tensor("moe_w_down", [NE, 1, 1], F32, kind="ExternalInput")
    mgu_d = mgu_t.ap()
    mdn_d = mdn_t.ap()
    tri_d = din("tri", [128, 128])
    HG = nc.dram_tensor("hg_scratch", [NSLOT, D], BF16, kind="Internal").ap()
    YD = nc.dram_tensor("y_scratch", [NSLOT, D], F32, kind="Internal").ap()
    ident_d = din("ident", [128, 128])
    masks_d = din("masks", [128, 4 * 512])
    out_d = nc.dram_tensor("out", [S, D], F32, kind="ExternalOutput").ap()

    xT = nc.alloc_sbuf_tensor("xT", [128, KT, S], F32)
    hT = nc.alloc_sbuf_tensor("hT", [128, KT, S], BF16)
    AB = nc.alloc_sbuf_tensor("actbuf", [128, KT, S], BF16)
    ring = nc.alloc_sbuf_tensor("wring", [128, NS, SLOT], BF16)
    SCR = nc.alloc_sbuf_tensor("scr", [128, 10240], F32)
    prm = nc.alloc_sbuf_tensor("prm", [128, NPC], F32)
    ident = nc.alloc_sbuf_tensor("ident_sb", [128, 128], F32)
    masks = nc.alloc_sbuf_tensor("masks_sb", [128, 4, 512], BF16)
    ones_f = nc.alloc_sbuf_tensor("ones_f", [8, 128], F32)
    tri_bf = nc.alloc_sbuf_tensor("tri_bf", [128, 128], BF16)
    ident_bf = nc.alloc_sbuf_tensor("ident_bf", [128, 128], BF16)
    rw = nc.alloc_sbuf_tensor("rw_sb", [128, KT, NE], F32)
    ones_bf = nc.alloc_sbuf_tensor("ones_bf", [128, 128], BF16)
    blk_bf = nc.alloc_sbuf_tensor("blk_bf", [128, 128], BF16)
    small = nc.alloc_sbuf_tensor("small", [128, 64], F32)

    xB = [[Buf(f"x{k}_{t}") for t in range(NTC)] for k in range(KT)]
    hB = [[Buf(f"h{k}_{t}") for t in range(NTC)] for k in range(KT)]
    aB = [[Buf(f"a{k}_{t}") for t in range(NTC)] for k in range(KT)]
    wB = [Buf(f"w{s}") for s in range(NS + 3)]
    ABf = AB[:].rearrange("p k t -> p (k t)")

    def RW(sid, lo, hi):
        if sid < NS:
            return ring[:, sid, lo:hi]
        o = SLOT * (sid - NS + 1)
        return ABf[:, o + lo:o + hi]
    sB = [Buf(f"scr{i}") for i in range(40)]
    cB = Buf("consts")
    hgB = [Buf(f"hg_dram{i}") for i in range(16)]
    ydB = Buf("y_dram")
    smB = Buf("small")
    psB = [Buf(f"ps{i}") for i in range(8)]
    ps = [nc.alloc_psum_tensor(f"ps{i}", [128, 512], F32) for i in range(8)]

    def scr(kb_off, kb, dtype=F32):
        lo = int(kb_off * 256)
        hi = int((kb_off + kb) * 256)
        ap = SCR[:, lo:hi]
        if dtype == BF16:
            ap = ap.bitcast(BF16)
        b0 = int(math.floor(kb_off))
        b1 = int(math.ceil(kb_off + kb))
        return ap, sB[b0:b1]

    def tcs(t):
        return slice(t * 512, (t + 1) * 512)

    def pcol(c, n=1):
        return prm[:, c:c + n]

    class Rot:
        def __init__(self, idx):
            self.idx = idx
            self.i = 0

        def next(self):
            b = self.idx[self.i % len(self.idx)]
            self.i += 1
            return ps[b], psB[b]

    regs = {}
    for p_ in range(2):
        regs[p_] = (nc.gpsimd.alloc_register(f"re{p_}"), nc.gpsimd.alloc_register(f"rgu{p_}"),
                    nc.gpsimd.alloc_register(f"rdn{p_}"))
    tmpr = [nc.gpsimd.alloc_register(f"rtmp{i}") for i in range(4)]

    def body(P, W):
        P.dma("pool", prm[:], params_d[:], (), (cB,))
        P.dma("pool", ident[:], ident_d[:], (), (cB,))
        P.dma("pool", masks[:].rearrange("p a b -> p (a b)"), masks_d[:], (), (cB,))
        P.dma("pool", tri_bf[:], tri_d[:], (), (cB,))
        P.dma("pool", ident_bf[:], ident_d[:], (), (cB,))
        P.dma("pool", rw[:], rw_d.rearrange("(kt p) e -> p kt e", p=128), (), (cB,))
        W.start()
        P.memset("dve", ones_bf[:], 1.0, (cB,))
        P.memset("dve", blk_bf[:], 0.0, (cB,))
        P.memset("dve", blk_bf[0:64, 0:64], 1.0, (cB,))
        P.memset("dve", blk_bf[64:128, 64:128], 1.0, (cB,))
        P.memset("dve", ones_f[:], 1.0, (cB,))

        rot = Rot([0, 1, 2, 3])
        for g in range(NTC):
            stg, stgB = scr(16 * (g % 2), 16)
            stg3 = stg.rearrange("p (t d) -> p t d", t=4)
            P.dma("sp", stg3, x_d[g * 512:(g + 1) * 512, :].rearrange("(t p) d -> p t d", p=128), (), stgB)
            for kt in range(KT):
                pt, ptB = rot.next()
                for t in range(4):
                    P.transpose(pt[:, t * 128:(t + 1) * 128], stg3[:, t, kt * 128:(kt + 1) * 128], ident[:],
                                list(stgB) + [cB], (ptB,))
                P.copy("act" if kt % 2 else "dve", xT[:, kt, tcs(g)], pt[:], (ptB,), (xB[kt][g],))

        if stage >= 5:
            zsrc = ABf[:, 12288:16384].rearrange("p (s f) -> p s f", s=4)
            zB = [aB[k][t] for k in (6, 7) for t in range(NTC)]
            P.memset("dve", ABf[:, 12288:16384], 0.0, zB)
            for t in range(NTILE):
                P.dma("sp", HG[t * 512:(t + 1) * 512, :].rearrange("(s p) d -> p s d", p=128), zsrc, zB, (hgB[t % 16],))

        def rmsnorm(gcol, post_rs=None, dst=None, post_tc=None):
            banks = [4, 5, 6, 7]
            for tc in range(NTC):
                sq, sqB = scr(8 * (tc % 2), 8, BF16)
                sq3 = sq.rearrange("p (k t) -> p k t", k=KT)
                P.act(sq3, xT[:, :, tcs(tc)], AF.Square, [xB[k][tc] for k in range(KT)], sqB)
                for kt in range(KT):
                    P.matmul(ps[banks[tc]][:], ones_bf[:], sq3[:, kt, :], kt == 0, kt == KT - 1,
                             list(sqB) + [cB], (psB[banks[tc]],))
            for tc in range(NTC):
                sd, sdB = scr(16 + 4 * (tc % 2), 2)
                rs, rsB = scr(18 + 4 * (tc % 2), 2)
                P.act(sd, ps[banks[tc]][:], AF.Ln, (psB[banks[tc]], smB), sdB, bias=small[:, 0:1], scale=1.0 / D)
                P.act(rs, sd, AF.Exp, sdB, rsB, scale=-0.5)
                if post_rs is not None:
                    post_rs(tc, rs, rsB)
                for kt in range(KT):
                    if dst is None:
                        o_ap, o_b = hT[:, kt, tcs(tc)], (hB[kt][tc],)
                    else:
                        o_ap, o_b = dst(kt, tc)
                    if gcol is None:
                        P.tt("dve", o_ap, xT[:, kt, tcs(tc)], rs, ALU.mult,
                             [xB[kt][tc]] + list(rsB), o_b)
                    else:
                        P.stt("dve", o_ap, xT[:, kt, tcs(tc)], pcol(gcol + kt), rs,
                              ALU.mult, ALU.mult, [xB[kt][tc], cB] + list(rsB), o_b)
                if post_tc is not None:
                    post_tc(tc)

        P.memset("dve", small[:, 0:1], EPS, (smB,))

        def add_residual(m, tc, pt, ptB):
            P.tt("dve", xT[:, m, tcs(tc)], xT[:, m, tcs(tc)], pt[:], ALU.add, (xB[m][tc], ptB), (xB[m][tc],))

        def linear_fm(inp, inB, nk, wslot, woff, wcols, wbuf, m_list, rot, epilogue):
            for m in m_list:
                for tc in range(NTC):
                    pt, ptB = rot.next()
                    for k in range(nk):
                        c0 = woff + k * wcols + m * 128
                        P.matmul(pt[:], RW(wslot, c0, c0 + 128), inp[:, k, tcs(tc)], k == 0, k == nk - 1,
                                 (wbuf, inB[k][tc]), (ptB,))
                    epilogue(m, tc, pt, ptB)

        def wcolblock(src2d, c0, ncols):
            return [(0, KT * ncols, KT, src2d[:, c0:c0 + ncols].rearrange("(kt p) f -> p kt f", p=128))]

        def wrowblock(src2d, r0, nch):
            return [(0, nch * D, nch, src2d[r0:r0 + nch * 128, :].rearrange("(c p) f -> p c f", p=128))]

        if stage >= 2:
            rmsnorm(PC_LNMIX0)
            rot = Rot([0, 1, 2, 3, 4, 5])
            for G in range(2):
                sb_, bb_ = W.get(wcolblock(win_d, G * 512, 512))
                sc_, bc_ = W.get(wcolblock(win_d, D + G * 512, 512))
                sv_, bv_ = W.get(wcolblock(win_d, 2 * D + G * 512, 512))
                for jj in range(4):
                    j = 4 * G + jj
                    ub, ubB = scr(8 * (j % 2), 8)
                    bbuf, bbB = scr(16 + 8 * (j % 2), 8)
                    for tc in range(NTC):
                        pc, pcB = rot.next()
                        pv, pvB = rot.next()
                        pb, pbB = rot.next()
                        for (pt, ptB, slot, wb) in ((pc, pcB, sc_, bc_), (pv, pvB, sv_, bv_), (pb, pbB, sb_, bb_)):
                            for kt in range(KT):
                                c0 = kt * 512 + jj * 128
                                P.matmul(pt[:], RW(slot, c0, c0 + 128), hT[:, kt, tcs(tc)], kt == 0, kt == KT - 1,
                                         (wb, hB[kt][tc]), (ptB,))
                        csb, csbB = scr(32 + 2 * (tc % 2), 2)
                        z, zB = scr(36 + 2 * (tc % 2), 2)
                        P.copy("act", csb, pc[:], (pcB,), csbB)
                        P.tt("dve", ub[:, tcs(tc)], csb, pv[:], ALU.mult, list(csbB) + [pvB], ubB)
                        P.copy("act", bbuf[:, tcs(tc)], pb[:], (pbB,), bbB)
                        lo = tc * 512
                        P.ts("dve", z, ub[:, lo:lo + 512], pcol(PC_CW + 16 + j), None, ALU.mult, None,
                             list(ubB) + [cB], zB)
                        for sh, tap in ((1, 1), (2, 0)):
                            a = sh if tc == 0 else 0
                            P.stt("dve", z[:, a:512], ub[:, lo + a - sh:lo + 512 - sh], pcol(PC_CW + 8 * tap + j),
                                  z[:, a:512], ALU.mult, ALU.add, list(ubB) + list(zB) + [cB], zB)
                        P.tt("dve", AB[:, j, tcs(tc)], bbuf[:, tcs(tc)], z, ALU.mult, list(bbB) + list(zB), (aB[j][tc],))
                W.release(); W.release(); W.release()
            rot = Rot([0, 1, 2, 3, 4, 5, 6, 7])
            for half in range(2):
                so_, bo_ = W.get(wcolblock(wout_d, half * 512, 512))
                linear_fm(AB, aB, KT, so_, 0, 512, bo_, range(4), rot,
                          lambda m, tc, pt, ptB, half=half: add_residual(4 * half + m, tc, pt, ptB))
                W.release()

        def ffn(gu_d, dn_d, nchunks, dff, cT=None):
            rot_gu = Rot([0, 1, 2, 3])
            rot_dn = Rot([4, 5, 6, 7])
            ngrp = (nchunks + 3) // 4
            for gi in range(ngrp):
                c0 = gi * 4
                nch = min(4, nchunks - c0)
                sg_, bg_ = W.get(wcolblock(gu_d, c0 * 128, nch * 128))
                su_, bu_ = W.get(wcolblock(gu_d, dff + c0 * 128, nch * 128))
                sd_, bd_ = W.get(wrowblock(dn_d, c0 * 128, nch))
                base = 4 * (gi % 2)
                for jj in range(nch):
                    a = base + jj
                    for tc in range(NTC):
                        pg, pgB = rot_gu.next()
                        pu, puB = rot_gu.next()
                        for (pt, ptB, slot, wb) in ((pg, pgB, sg_, bg_), (pu, puB, su_, bu_)):
                            for kt in range(KT):
                                cc = kt * nch * 128 + jj * 128
                                P.matmul(pt[:], RW(slot, cc, cc + 128), hT[:, kt, tcs(tc)], kt == 0, kt == KT - 1,
                                         (wb, hB[kt][tc]), (ptB,))
                        sl, slB = scr(32 + 2 * (tc % 2), 2)
                        P.act(sl, pg[:], AF.Silu, (pgB,), slB)
                        if cT is not None:
                            P.tt("dve", sl, sl, cT[0][:, tcs(tc)], ALU.mult, list(slB) + list(cT[1]), slB)
                        P.tt("dve", AB[:, a, tcs(tc)], sl, pu[:], ALU.mult, list(slB) + [puB], (aB[a][tc],))
                W.release(); W.release()
                for m in range(KT):
                    for tc in range(NTC):
                        pt, ptB = rot_dn.next()
                        for jj in range(nch):
                            cc = jj * D + m * 128
                            P.matmul(pt[:], RW(sd_, cc, cc + 128), AB[:, base + jj, tcs(tc)], jj == 0, jj == nch - 1,
                                     (bd_, aB[base + jj][tc]), (ptB,))
                        add_residual(m, tc, pt, ptB)
                W.release()

        if stage >= 3:
            rmsnorm(PC_LNFFN0)
            if stage >= 5:
                zy, zyB = scr(36, 4)
                P.memset("dve", zy, 0.0, zyB)
                for t in range(8, NTILE):
                    for q4 in range(4):
                        P.dma("sp", YD[t * 512 + q4 * 128:t * 512 + (q4 + 1) * 128, :], zy, zyB, (ydB,))
            ffn(wgu_d, wdn_d, NFF, DFF)

        if stage >= 4:
            rmsnorm(None)
            tmp, tmpB = scr(0, 1)
            lamp, lampB = scr(1, 1)
            P.dma("sp", lamp, lam_d.broadcast_to([128, 256]), (), lampB)
            P.tt("dve", tmp[:, 0:64], lamp[:, 0:64], lamp[:, 64:128], ALU.mult, lampB, tmpB)
            P.reduce(small[:, 8:9], tmp[:, 0:64], ALU.add, tmpB, (smB,))
            P.tt("dve", tmp[:, 64:128], lamp[:, 128:192], lamp[:, 192:256], ALU.mult, lampB, tmpB)
            P.reduce(small[:, 9:10], tmp[:, 64:128], ALU.add, tmpB, (smB,))
            P.act(small[:, 10:12], small[:, 8:10], AF.Exp, (smB,), (smB,))
            P.stt("dve", small[:, 1:2], small[:, 11:12], -LAM_INIT, small[:, 10:11], ALU.add, ALU.subtract, (smB,), (smB,))
            P.ts("dve", small[:, 2:3], pcol(PC_GQ), 0.125, None, ALU.mult, None, (cB,), (smB,))
            P.ts("dve", small[:, 3:4], pcol(PC_GS), 1.0 - LAM_INIT, None, ALU.mult, None, (cB,), (smB,))

            kT0, kT0B = scr(0, 4, BF16)
            kT1, kT1B = scr(4, 4, BF16)
            qTh, qThB = scr(8, 4, BF16)
            Vh, VhB = scr(12, 4, BF16)
            Vh3 = Vh.rearrange("p (t e) -> p t e", t=NT)
            P.ts("dve", masks[:].rearrange("p a b -> p (a b)"), masks[:].rearrange("p a b -> p (a b)"), 30000.0, -30000.0,
                 ALU.mult, ALU.add, (cB,), (cB,))
            P.memset("dve", kT0[64:128, :], 0.0, kT0B)
            P.memset("dve", kT1[0:64, :], 0.0, kT1B)
            rot_s = Rot([0, 1, 2, 3])
            rot_p = rot_s
            gkv3 = prm[:, PC_LNKV:PC_LNKV + KT].unsqueeze(2)
            gm13 = prm[:, PC_LNMIX1:PC_LNMIX1 + KT].unsqueeze(2)
            estep = [0]
            for h in range(8):
                parts = [
                    (0, KT * 128, KT, wkv_d[:, h * 128:(h + 1) * 128].rearrange("(kt p) f -> p kt f", p=128)),
                    (KT * 128, KT * 128, KT, wkv_d[:, D + h * 128:D + (h + 1) * 128].rearrange("(kt p) f -> p kt f", p=128)),
                    (2 * KT * 128, KT * 128, KT, wq_d[:, h * 128:(h + 1) * 128].rearrange("(kt p) f -> p kt f", p=128)),
                ]
                sw_, bw_ = W.get(parts)
                for part, g3 in ((0, gkv3), (1, gkv3), (2, gm13)):
                    wv = RW(sw_, part * 1024, (part + 1) * 1024).rearrange("p (k f) -> p k f", k=KT)
                    P.tt("dve", wv, wv, g3.broadcast_to([128, KT, 128]), ALU.mult, (bw_, cB), (bw_,))
                fin_prev = [None]
                for which in range(2):
                    woff = 0 if which == 0 else 2048
                    for tc in range(NTC):
                        pt, ptB = rot_p.next()
                        for kt in range(KT):
                            c0 = woff + kt * 128
                            P.matmul(pt[:], RW(sw_, c0, c0 + 128), hT[:, kt, tcs(tc)], kt == 0, kt == KT - 1,
                                     (bw_, hB[kt][tc]), (ptB,))
                        slot2 = (4 * which + tc) % 2
                        kqc, kqcB = scr(30 + 2 * slot2, 2)
                        sqh, sqhB = scr(24 + slot2, 1, BF16)
                        P.copy("dve", kqc, pt[:], (ptB,), kqcB)
                        P.tt("dve", sqh, kqc, kqc, ALU.mult, kqcB, sqhB)

                        def fin(which=which, tc=tc, kqc=kqc, kqcB=kqcB, sqh=sqh, sqhB=sqhB):
                            pst, pstB = rot_p.next()
                            P.matmul(pst[:], blk_bf[:], sqh, True, True, list(sqhB) + [cB], (pstB,))
                            lnt, lntB = scr(26, 2)
                            rsh, rshB = scr(28, 2)
                            P.act(lnt, pst[:], AF.Ln, (pstB, smB), lntB, bias=small[:, 0:1], scale=1.0 / 64)
                            P.act(rsh, lnt, AF.Exp, lntB, rshB, scale=-0.5)
                            if which == 0:
                                P.stt("dve", kT0[0:64, tcs(tc)], kqc[0:64, :], prm[0:64, PC_GK:PC_GK + 1], rsh[0:64, :],
                                      ALU.mult, ALU.mult, list(kqcB) + list(rshB) + [cB], kT0B)
                                P.stt("dve", kT1[64:128, tcs(tc)], kqc[64:128, :], prm[64:128, PC_GK:PC_GK + 1], rsh[64:128, :],
                                      ALU.mult, ALU.mult, list(kqcB) + list(rshB) + [cB], kT1B)
                            else:
                                P.stt("dve", qTh[:, tcs(tc)], kqc, small[:, 2:3], rsh,
                                      ALU.mult, ALU.mult, list(kqcB) + list(rshB) + [smB], qThB)
                        if fin_prev[0] is not None:
                            fin_prev[0]()
                        fin_prev[0] = fin
                for tg in range(4):
                    pt, ptB = rot_p.next()
                    for t in range(4):
                        tok0 = (4 * tg + t) * 128
                        for kt in range(KT):
                            c0 = 1024 + kt * 128
                            P.matmul(pt[:, t * 128:(t + 1) * 128], hT[:, kt, tok0:tok0 + 128], RW(sw_, c0, c0 + 128),
                                     kt == 0, kt == KT - 1, (bw_, hB[kt][tg]), (ptB,))
                    P.copy("dve" if tg % 2 else "act", Vh[:, tg * 512:(tg + 1) * 512], pt[:], (ptB,), VhB)
                    if tg == 0:
                        fin_prev[0]()
                W.release()
                LA = 3
                pending = []
                for qc in range(NTC):
                    nk = 4 * qc + 4
                    seq = [(c, ki) for ki in range(nk) for c in (0, 1)]
                    ets = {}
                    acc = [(ps[4], psB[4], ps[6], psB[6]), (ps[5], psB[5], ps[7], psB[7])]
                    for step in range(len(seq) + LA):
                        if step < len(seq):
                            c, ki = seq[step]
                            kTc, kTcB = (kT0, kT0B) if c == 0 else (kT1, kT1B)
                            d = ki - 4 * qc
                            q0 = max(d, 0) * 128
                            pst, pstB = rot_s.next()
                            P.matmul(pst[:, q0:512], kTc[:, ki * 128:(ki + 1) * 128], qTh[:, qc * 512 + q0:(qc + 1) * 512],
                                     True, d < 0, list(kTcB) + list(qThB), (pstB,))
                            if d >= 0:
                                P.matmul(pst[:, q0:512], ident_bf[:], masks[:, d, q0:512], False, True, (cB,), (pstB,))
                            et, etB = scr(16 + (estep[0] % 8), 1, BF16)
                            estep[0] += 1
                            P.act(et[:, q0:512], pst[:, q0:512], AF.Exp, (pstB,), etB)
                            ets[step] = (et, etB, c, ki, q0)
                            if pending and step >= 1:
                                pending.pop(0)()
                        if step >= LA:
                            et, etB, c, ki, q0 = ets[step - LA]
                            po, poB, pz, pzB = acc[c]
                            P.matmul(po[:, q0:512], Vh3[:, ki, :], et[:, q0:512], ki == 0, ki == nk - 1,
                                     list(VhB) + list(etB), (poB,))
                            P.matmul(pz[:, q0:512], ones_bf[:], et[:, q0:512], ki == 0, ki == nk - 1,
                                     list(etB) + [cB], (pzB,))
                    o_aps = []
                    rzs = []
                    for c in range(2):
                        po, poB, pz, pzB = acc[c]
                        rz, rzB = scr(30 + 2 * c, 2)
                        oc, ocB = scr(36 + 2 * c, 2)
                        P.act(rz, pz[:], AF.Ln, (pzB,), rzB)
                        P.copy("dve", oc, po[:], (poB,), ocB)
                        o_aps.append((oc, ocB))
                        rzs.append((rz, rzB))
                    def tail(h=h, qc=qc, rzs=rzs, o_aps=o_aps):
                        (o0, o0B), (o1, o1B) = o_aps
                        osq, osqB = scr(24, 1, BF16)
                        oln, olnB = scr(26, 2)
                        ors, orsB = scr(28, 2)
                        st = {}

                        def stat_mm():
                            st["p"] = rot_s.next()
                            P.matmul(st["p"][0][:], ones_bf[:], osq, True, True, list(osqB) + [cB], (st["p"][1],))
                        ops_ = []
                        for c in range(2):
                            rz, rzB = rzs[c]
                            oc, ocB = o_aps[c]
                            ops_.append(lambda rz=rz, rzB=rzB: P.act(rz, rz, AF.Exp, rzB, rzB, scale=-1.0))
                            ops_.append(lambda oc=oc, ocB=ocB, rz=rz, rzB=rzB: P.tt("dve", oc, oc, rz, ALU.mult, list(ocB) + list(rzB), ocB))
                        ops_.append(lambda: P.stt("dve", o0, o1, small[:, 1:2], o0, ALU.mult, ALU.add,
                                                  list(o0B) + list(o1B) + [smB], o0B))
                        ops_.append(lambda: P.tt("dve", osq, o0, o0, ALU.mult, o0B, osqB))
                        ops_.append(stat_mm)
                        ops_.append(lambda: P.act(oln, st["p"][0][:], AF.Ln, (st["p"][1], smB), olnB, bias=small[:, 0:1], scale=1.0 / 128))
                        ops_.append(lambda: P.act(ors, oln, AF.Exp, olnB, orsB, scale=-0.5))
                        ops_.append(lambda: P.stt("dve", AB[:, h, tcs(qc)], o0, small[:, 3:4], ors, ALU.mult, ALU.mult,
                                                  list(o0B) + list(orsB) + [smB], (aB[h][qc],)))
                        return ops_
                    pending.extend(tail())
                while pending:
                    pending.pop(0)()
            rot = Rot([0, 1, 2, 3, 4, 5, 6, 7])
            for half in range(2):
                so_, bo_ = W.get(wcolblock(wo_d, half * 512, 512))
                linear_fm(AB, aB, KT, so_, 0, 512, bo_, range(4), rot,
                          lambda m, tc, pt, ptB, half=half: add_residual(4 * half + m, tc, pt, ptB))
                W.release()

        if stage >= 5:
            rtr_ap, rtr_bufs = scr(24, 8)
            rtr = rtr_ap.rearrange("p (a b) -> p a b", a=16)
            rtB = rtr_bufs[0]

            def R(i):
                return rtr[:, i, :]

            def R3(i):
                return rtr[:, i, :].rearrange("p (t e) -> p t e", t=NT)
            rstd_tok = rtr[:, 6, 0:16]
            m1 = rtr[:, 6, 16:32]
            m2 = rtr[:, 6, 32:48]
            den = rtr[:, 6, 48:64]
            rden = rtr[:, 6, 64:80]
            P1p = rtr[:, 6, 80:96]
            P2 = rtr[:, 6, 96:112]
            sp = rtr[:, 6, 112:128]
            gw = rtr[:, 7, 0:KT * NE].rearrange("p (k e) -> p k e", k=KT)
            ne_ = rtr[:, 7, 64:72]
            ntl = rtr[:, 7, 72:80]
            tb = rtr[:, 7, 80:88]
            base = rtr[:, 7, 88:96]
            P1 = rtr[:, 7, 96:112]
            G1 = rtr[:, 11, 0:16]
            G2 = rtr[:, 11, 16:32]
            etf = rtr[:, 11, 32:48]
            idx1 = rtr[:, 12, 0:16].bitcast(mybir.dt.int32)
            idx2 = rtr[:, 12, 16:32].bitcast(mybir.dt.int32)
            eid = rtr[:, 12, 32:48].bitcast(mybir.dt.int32)
            selb = rtr[:, 13, 0:64].bitcast(BF16)
            P.memset("dve", rtr_ap, 0.0, rtr_bufs)

            htok = hT[:].rearrange("p k t -> p (k t)").rearrange("p (a b) -> p a b", a=NT)

            def htokB(tt):
                return [hB[tt // 2][2 * (tt % 2)], hB[tt // 2][2 * (tt % 2) + 1]]
            hnc, hncB = scr(32, 8, BF16)
            hnc3 = hnc.rearrange("p (k t) -> p k t", k=KT)
            rot_t = Rot([0, 1, 2])

            def post_rs(tc, rs, rsB):
                pt, ptB = ps[3], psB[3]
                for t in range(4):
                    P.transpose(pt[:, t * 128:(t + 1) * 128], rs[:, t * 128:(t + 1) * 128], ident[:], list(rsB) + [cB], (ptB,))
                P.copy("dve", rstd_tok[:, 4 * tc:4 * tc + 4], pt[:].rearrange("p (t c) -> p t c", c=128)[:, :, 0], (ptB,), (rtB,))

            def post_tc(tc):
                for t in range(4):
                    tt = 4 * tc + t
                    pt, ptB = rot_t.next()
                    ptb = pt[:].bitcast(BF16)
                    for kt in range(KT):
                        P.transpose(ptb[:, kt * 128:(kt + 1) * 128], hnc3[:, kt, t * 128:(t + 1) * 128], ident_bf[:],
                                    list(hncB) + [cB], (ptB,))
                    P.copy("act" if t % 2 else "dve", htok[:, tt, :], ptb, (ptB,), htokB(tt))
            rmsnorm(PC_LNFFN1, post_rs, dst=lambda kt, tc: (hnc3[:, kt, :], hncB), post_tc=post_tc)

            P.tt("dve", gw, rw[:], prm[:, PC_LNFFN1:PC_LNFFN1 + KT].unsqueeze(2).broadcast_to([128, KT, NE]), ALU.mult,
                 (cB, rtB), (rtB,))
            pl, plB = ps[0], psB[0]
            for t in range(NT):
                for kt in range(KT):
                    P.matmul(pl[:, t * NE:(t + 1) * NE], xT[:, kt, t * 128:(t + 1) * 128], gw[:, kt, :], kt == 0, kt == KT - 1,
                             (xB[kt][t // 4], rtB), (plB,))
            bc = lambda v: v.unsqueeze(2).broadcast_to([128, NT, NE])
            DV = lambda *a: P.tt("dve", *a, (rtB,), (rtB,))
            P.tt("dve", R3(0), pl[:, 0:NT * NE].rearrange("p (t e) -> p t e", t=NT), bc(rstd_tok), ALU.mult, (plB, rtB), (rtB,))
            def first_one(src, ta, tb_):
                cur = src
                for sh, dst in ((1, ta), (2, tb_), (4, ta)):
                    P.copy("dve", R3(dst)[:, :, 0:sh], R3(cur)[:, :, 0:sh], (rtB,), (rtB,))
                    DV(R3(dst)[:, :, sh:NE], R3(cur)[:, :, sh:NE], R3(cur)[:, :, 0:NE - sh], ALU.add)
                    cur = dst
                P.ts("dve", R(tb_), R(cur), 1.0, None, ALU.is_equal, None, (rtB,), (rtB,))
                DV(R(src), R(src), R(tb_), ALU.mult)
            P.reduce(m1, R3(0), ALU.max, (rtB,), (rtB,))
            DV(R3(1), R3(0), bc(m1), ALU.is_equal)
            first_one(1, 8, 9)
            P.stt("dve", R(2), R(1), -1e30, R(0), ALU.mult, ALU.add, (rtB,), (rtB,))
            P.reduce(m2, R3(2), ALU.max, (rtB,), (rtB,))
            DV(R3(3), R3(2), bc(m2), ALU.is_equal)
            first_one(3, 8, 9)
            DV(R(3), R(3), R(1), ALU.add)
            DV(R3(4), R3(0), bc(m1), ALU.subtract)
            P.act(R(4), R(4), AF.Exp, (rtB,), (rtB,))
            DV(R(4), R(4), R(3), ALU.mult)
            P.reduce(den, R3(4), ALU.add, (rtB,), (rtB,))
            P.recip(rden, den, (rtB,), (rtB,))
            DV(R3(5), R3(4), bc(rden), ALU.mult)
            P.copy("dve", selb, R(3), (rtB,), (rtB,))
            pa, paB = ps[1], psB[1]
            pb, pbB = ps[2], psB[2]
            P.matmul(pa[:, 0:128], tri_bf[:], selb, True, True, (rtB, cB), (paB,))
            P.matmul(pb[:, 0:128], ones_bf[:], selb, True, True, (rtB, cB), (pbB,))
            P.copy("dve", R(8), pa[:, 0:128], (paB,), (rtB,))
            P.copy("dve", R(9), pb[:, 0:128], (pbB,), (rtB,))
            src_, dst_ = 9, 10
            for sh in (1, 2, 4, 8):
                P.copy("dve", R3(dst_)[:, 0:sh, :], R3(src_)[:, 0:sh, :], (rtB,), (rtB,))
                DV(R3(dst_)[:, sh:NT, :], R3(src_)[:, sh:NT, :], R3(src_)[:, 0:NT - sh, :], ALU.add)
                src_, dst_ = dst_, (14 if dst_ == 10 else 10)
            P.copy("dve", ne_, R3(src_)[:, NT - 1, :], (rtB,), (rtB,))
            if src_ != 10:
                DV(R(10), R(src_), R(9), ALU.subtract)
            else:
                DV(R(14), R(10), R(9), ALU.subtract)
                P.copy("dve", R(10), R(14), (rtB,), (rtB,))
            P.ts("dve", ntl, ne_, 0.0, None, ALU.is_gt, None, (rtB,), (rtB,))
            for thr in (512.0, 1024.0, 1536.0):
                P.stt("dve", ntl, ne_, thr, ntl, ALU.is_gt, ALU.add, (rtB,), (rtB,))
            P.memset("dve", tb[:, 0:1], 0.0, (rtB,))
            for e in range(1, NE):
                DV(tb[:, e:e + 1], tb[:, e - 1:e], ntl[:, e - 1:e], ALU.add)
            P.ts("dve", base, tb, 512.0, None, ALU.mult, None, (rtB,), (rtB,))
            DV(R(8), R(8), R(10), ALU.add)
            DV(R3(8), R3(8), base.unsqueeze(1).broadcast_to([128, NT, NE]), ALU.add)
            P.stt("dve", R(2), R(8), 1.0, R(3), ALU.add, ALU.mult, (rtB,), (rtB,))
            P.reduce(P1p, R3(2), ALU.max, (rtB,), (rtB,))
            P.reduce(sp, R3(2), ALU.add, (rtB,), (rtB,))
            DV(R3(1), R3(2), bc(P1p), ALU.is_equal)
            DV(R(1), R(1), R(5), ALU.mult)
            P.reduce(G1, R3(1), ALU.add, (rtB,), (rtB,))
            P.ts("dve", G2, G1, -1.0, 1.0, ALU.mult, ALU.add, (rtB,), (rtB,))
            P.ts("dve", P1, P1p, -1.0, None, ALU.add, None, (rtB,), (rtB,))
            P.stt("dve", P2, sp, -1.0, P1p, ALU.add, ALU.subtract, (rtB,), (rtB,))
            P.copy("dve", idx1, P1, (rtB,), (rtB,))
            P.copy("dve", idx2, P2, (rtB,), (rtB,))
            DV(R3(15), prm[:, PC_IOTA:PC_IOTA + 16].unsqueeze(2).broadcast_to([128, NT, NE]),
               tb.unsqueeze(1).broadcast_to([128, NT, NE]), ALU.is_ge)
            P.reduce(etf, R3(15), ALU.add, (rtB, cB), (rtB,))
            P.ts("dve", etf, etf, -1.0, None, ALU.add, None, (rtB,), (rtB,))
            P.copy("dve", eid, etf, (rtB,), (rtB,))
            nused = rtr[:, 11, 48:49]
            flagf = rtr[:, 11, 64:80]
            flags = rtr[:, 12, 48:64].bitcast(mybir.dt.int32)
            P.reduce(nused, ntl, ALU.add, (rtB,), (rtB,))
            P.ts("dve", flagf, prm[:, PC_IOTA:PC_IOTA + 16], nused, None, ALU.is_lt, None, (rtB, cB), (rtB,))
            if not P.plan:
                P.flag_op = len(P.ops)
                P.flag_ap = flags
            P.copy("dve", flags, flagf, (rtB,), (rtB,))
            g_ = nc.gpsimd
            for tt in range(NT):
                for ix in (idx1, idx2):
                    P.op("pool", (lambda ix=ix, tt=tt: g_.indirect_dma_start(
                        out=HG[:, :], out_offset=bass.IndirectOffsetOnAxis(ap=ix[:, tt:tt + 1], axis=0),
                        in_=htok[:, tt, :], in_offset=None)), list(htokB(tt)) + [rtB],
                        (hgB[(2 * tt + (0 if ix is idx1 else 1)) % 16],), dma=True)

            W.go()
            tcnt = [0]
            yacc = hT[:, 0:4, :].rearrange("p k t -> p (k t)").bitcast(F32).rearrange("p (s f) -> p s f", s=4)
            yaccB = lambda s_, fh: [hB[s_][2 * fh], hB[s_][2 * fh + 1]]
            hgs_l, hgsB_l, hgT_l, hgTB_l = [], [], [], []
            hgs_l.append(hT[:, 4:6, :].rearrange("p k t -> p (k t)").rearrange("p (s f) -> p s f", s=4))
            hgsB_l.append([hB[k][t] for k in (4, 5) for t in range(NTC)])
            hgT_l.append(hT[:, 6:8, :].rearrange("p k t -> p (k t)").rearrange("p (k s) -> p k s", k=KT))
            hgTB_l.append(lambda kt: [hB[6 + kt // 4][kt % 4]])
            a_, b_ = scr(8, 8, BF16)
            hgs_l.append(a_.rearrange("p (s f) -> p s f", s=4))
            hgsB_l.append(list(b_))
            a_, b2_ = scr(0, 8, BF16)
            hgT_l.append(a_.rearrange("p (k s) -> p k s", k=KT))
            hgTB_l.append(lambda kt, b2_=b2_: [b2_[kt]])
            actT = lambda a: ABf[:, a * 512:(a + 1) * 512]
            actB = lambda a: aB[a // 4][a % 4]
            rot_gu = Rot([0, 1, 2, 3])
            rot_dn = Rot([4, 5, 6, 7])

            def dyn_part(t, kind, gi):
                re_, rgu_, rdn_ = regs[t % 2]
                if kind == "d":
                    const = gi * 512 * D
                    pat = [[D, 128], [128 * D, 4], [1, D]]
                    th, rb, split = mdn_t, rdn_, 4
                else:
                    const = gi * 512 + (DFE if kind == "u" else 0)
                    pat = [[2 * DFE, 128], [128 * 2 * DFE, KT], [1, 512]]
                    th, rb, split = mgu_t, rgu_, KT
                rt_ = tmpr[tcnt[0] % 4]
                tcnt[0] += 1
                src = (lambda th=th, rt_=rt_, pat=pat: bass.AP(th, rt_, pat))
                return (0, SLOT, split, src, (lambda rt_=rt_, rb=rb, const=const: (g_.reg_add(rt_, rb, const), rt_)[1]))

            def tile_pre(t):
                re_, rgu_, rdn_ = regs[t % 2]

                def fn():
                    g_.reg_load(re_, eid[0:1, t:t + 1])
                    g_.reg_mul(rgu_, re_, D * 2 * DFE)
                    return g_.reg_mul(rdn_, re_, DFE * D)
                return (fn, (rtB,))

            def prep_tile(t):
                par = t % 2
                hgs, hgsB, hgT, hgTB = hgs_l[par], hgsB_l[par], hgT_l[par], hgTB_l[par]
                P.dma("sp", hgs, HG[t * 512:(t + 1) * 512, :].rearrange("(s p) d -> p s d", p=128), hgB, hgsB)
                for kp in range(4):
                    pt, ptB = rot_dn.next()
                    ptb = pt[:].bitcast(BF16)
                    for k2 in range(2):
                        kt = 2 * kp + k2
                        for s_ in range(4):
                            P.transpose(ptb[:, k2 * 512 + s_ * 128:k2 * 512 + (s_ + 1) * 128], hgs[:, s_, kt * 128:(kt + 1) * 128],
                                        ident_bf[:], list(hgsB) + [cB], (ptB,))
                    P.copy("act" if kp % 2 else "dve", hgT[:, 2 * kp:2 * kp + 2, :].rearrange("p k s -> p (k s)"), ptb,
                           (ptB,), hgTB(2 * kp) + hgTB(2 * kp + 1))

            ctag = lambda t: (t if t >= 8 else None)
            prep_tile(0)
            for t in range(NTILE):
                P.cond = ctag(t)
                hgT, hgTB = hgT_l[t % 2], hgTB_l[t % 2]
                for gi in range(7):
                    sg_, bg_ = W.get({"parts": [dyn_part(t, "g", gi)], "pre": tile_pre(t) if gi == 0 else None, "hold": True, "cond": ctag(t)})
                    su_, bu_ = W.get({"parts": [dyn_part(t, "u", gi)], "pre": None, "hold": True, "cond": ctag(t)})
                    sd_, bd_ = W.get({"parts": [dyn_part(t, "d", gi)], "pre": None, "hold": True, "cond": ctag(t)})
                    abase = 4 * (gi % 2)
                    for jj in range(4):
                        a = abase + jj
                        pg, pgB = rot_gu.next()
                        pu, puB = rot_gu.next()
                        for (pt, ptB, slot, wb) in ((pg, pgB, sg_, bg_), (pu, puB, su_, bu_)):
                            for kt in range(KT):
                                cc = kt * 512 + jj * 128
                                P.matmul(pt[:], RW(slot, cc, cc + 128), hgT[:, kt, :], kt == 0, kt == KT - 1,
                                         [wb] + hgTB(kt), (ptB,))
                        sl, slB = scr(32 + 2 * (jj % 2), 2)
                        P.act(sl, pg[:], AF.Silu, (pgB,), slB)
                        P.tt("dve", actT(a), sl, pu[:], ALU.mult, list(slB) + [puB], (actB(a),))
                    W.release(); W.release()
                    if gi == 3 and t + 1 < NTILE:
                        P.cond = ctag(t + 1)
                        prep_tile(t + 1)
                        P.cond = ctag(t)
                    for s_ in range(4):
                        for fh in range(2):
                            pt, ptB = rot_dn.next()
                            for jj in range(4):
                                cc = jj * D + fh * 512
                                P.matmul(pt[:], actT(abase + jj)[:, s_ * 128:(s_ + 1) * 128], RW(sd_, cc, cc + 512),
                                         jj == 0, jj == 3, (bd_, actB(abase + jj)), (ptB,))
                            ya = yacc[:, s_, fh * 512:(fh + 1) * 512]
                            if gi == 0:
                                P.copy("act", ya, pt[:], (ptB,), yaccB(s_, fh))
                            else:
                                P.tt("dve", ya, ya, pt[:], ALU.add, [ptB] + yaccB(s_, fh), yaccB(s_, fh))
                    W.release()
                P.dma("sp", YD[t * 512:(t + 1) * 512, :].rearrange("(s p) f -> p s f", p=128), yacc,
                      [b for s_ in range(4) for fh in range(2) for b in yaccB(s_, fh)], (ydB,))
            P.cond = None

            rot = Rot([0, 1, 2, 3])
            for t in range(NT):
                o3 = (0, 8, 16, 32)[t % 4]
                b1, b1B = scr(o3, 4)
                b2, b2B = scr(o3 + 4, 4)
                for (bb_, bbB_, ix) in ((b1, b1B, idx1), (b2, b2B, idx2)):
                    P.op("pool", (lambda bb_=bb_, ix=ix, t=t: g_.indirect_dma_start(
                        out=bb_, out_offset=None, in_=YD[:, :],
                        in_offset=bass.IndirectOffsetOnAxis(ap=ix[:, t:t + 1], axis=0))), (ydB, rtB), bbB_, dma=True)
                for hf in range(2):
                    pt, ptB = rot.next()
                    for k4 in range(4):
                        kt = 4 * hf + k4
                        P.transpose(pt[:, k4 * 128:(k4 + 1) * 128], xT[:, kt, t * 128:(t + 1) * 128], ident[:],
                                    (xB[kt][t // 4], cB), (ptB,))
                    hs = slice(hf * 512, (hf + 1) * 512)
                    P.stt("dve", b1[:, hs], b1[:, hs], G1[:, t:t + 1], pt[:], ALU.mult, ALU.add,
                          list(b1B) + [ptB, rtB], b1B)
                    P.stt("dve", b1[:, hs], b2[:, hs], G2[:, t:t + 1], b1[:, hs], ALU.mult, ALU.add,
                          list(b2B) + list(b1B) + [rtB], b1B)
                P.dma("sp", out_d[t * 128:(t + 1) * 128, :], b1, b1B, ())
        else:
            rot = Rot([0, 1, 2, 3])
            for t in range(NT):
                ob, obB = scr(4 * (t % 4), 4)
                for hf in range(2):
                    pt, ptB = rot.next()
                    for k4 in range(4):
                        kt = 4 * hf + k4
                        P.transpose(pt[:, k4 * 128:(k4 + 1) * 128], xT[:, kt, t * 128:(t + 1) * 128], ident[:],
                                    (xB[kt][t // 4], cB), (ptB,))
                    P.copy("act" if hf else "dve", ob[:, hf * 512:(hf + 1) * 512], pt[:], (ptB,), obB)
                P.dma("sp", out_d[t * 128:(t + 1) * 128, :], ob, obB, ())

    P.plan = True
    Wp = WStream(P, RW, wB, None)
    body(P, Wp)
    P.plan = False
    first_extra = {NS + j: [aB[2 + 2 * j + k][t] for k in range(2) for t in range(NTC)] for j in range(3)}
    W = WStream(P, RW, wB, Wp.blocks, first_extra)
    body(P, W)
    sems = []

    def sem_alloc(name):
        s = nc.alloc_semaphore(name)
        sems.append(s)
        return s
    n = P.finalize(sem_alloc)
    return nc, n


def host_inputs(inp):
    f = lambda a: np.ascontiguousarray(np.asarray(a, dtype=np.float32))
    prm = np.zeros((128, NPC), np.float32)

    def cols(v):
        return np.asarray(v, np.float32).reshape(KT, 128).T
    prm[:, PC_LNMIX0:PC_LNMIX0 + 8] = cols(inp["ln_mix"][0])
    prm[:, PC_LNFFN0:PC_LNFFN0 + 8] = cols(inp["ln_ffn"][0])
    prm[:, PC_LNKV:PC_LNKV + 8] = cols(inp["ln_kv"])
    prm[:, PC_LNMIX1:PC_LNMIX1 + 8] = cols(inp["ln_mix"][1])
    prm[:, PC_LNFFN1:PC_LNFFN1 + 8] = cols(inp["ln_ffn"][1])
    for j in range(3):
        prm[:, PC_CW + 8 * j:PC_CW + 8 * j + 8] = cols(inp["conv_w"][0][j])
    prm[:, PC_GK] = np.tile(np.asarray(inp["k_norm"], np.float32), 2)
    prm[:, PC_GQ] = np.tile(np.asarray(inp["q_norm"][0], np.float32), 2)
    prm[:, PC_GS] = np.asarray(inp["sub_norm"][0], np.float32)
    prm[:, PC_IOTA:PC_IOTA + 16] = np.arange(16, dtype=np.float32)[None, :]
    ident = np.eye(128, dtype=np.float32)
    k = np.arange(128)[:, None]
    q = np.arange(512)[None, :]
    masks = np.concatenate([(q >= d * 128 + k).astype(np.float32) for d in range(4)], axis=1)
    shared = {
        "params": prm,
        "lam": f(inp["lam_params"]).reshape(1, 256),
        "conv_w_in": f(inp["conv_w_in"][0]),
        "conv_w_out": f(inp["conv_w_out"][0]),
        "w_kv": f(inp["w_kv"]),
        "attn_w_q": f(inp["attn_w_q"][0]),
        "attn_w_o": f(inp["attn_w_o"][0]),
        "ffn_w_gu": f(inp["ffn_w_gu"][0]),
        "ffn_w_down": f(inp["ffn_w_down"][0]),
        "router_w": f(inp["router_w"][0]),
        "moe_w_gu": f(inp["moe_w_gu"][0]),
        "moe_w_down": f(inp["moe_w_down"][0]),
        "ident": ident,
        "tri": np.triu(np.ones((128, 128), np.float32), 1),
        "masks": np.ascontiguousarray(masks),
    }
    x = f(inp["x"])
    return [dict(shared, x=np.ascontiguousarray(x[b])) for b in range(8)]


_CACHE = {}


def kernel(**inputs):
    if "nc" not in _CACHE:
        _CACHE["nc"] = build()[0]
    nc = _CACHE["nc"]
    in_maps = host_inputs(inputs)
    res = run_bass_kernel_spmd(nc, in_maps, core_ids=list(range(8)))
    return np.stack([np.asarray(r["out"], dtype=np.float32) for r in res.results], axis=0)
```
